# Optimizing a Trainium2 kernel written in Bass

```python
import math
import jax, jax.numpy as jnp
from jax import lax
import numpy as np

D_MODEL = 1024
BATCH = 8
SEQ = 4096
DEPTH = 2

CTX_LEN = 256
GRID_W = 64
NORM_EPS = 1e-6
CHUNK = 64
N_BRANCHES = 3
NEG_BIG = -1e30
TINY = 1e-20

S5_GROUPS = 16
S5_GROUP_CH = 16
S5_WIDTH = S5_GROUPS * S5_GROUP_CH
S5_STATE = 64
S5_DT_MIN = 1e-3
S5_DT_MAX = 1e-1

GLA_HEADS = 4
GLA_DK = 64
GLA_DV = 128
GLA_KW = GLA_HEADS * GLA_DK
GLA_VW = GLA_HEADS * GLA_DV
GLA_GATE_RANK = 16
GLA_GATE_NORM = 16.0

HGRN_HEADS = 4
HGRN_EXPAND = 64
HGRN_DV = 64
HGRN_KW = HGRN_HEADS * HGRN_EXPAND
HGRN_VW = HGRN_HEADS * HGRN_DV

N_EXPERTS = 16
N_EXPERT_GROUPS = 4
EXPERTS_PER_GROUP = N_EXPERTS // N_EXPERT_GROUPS
TOP_K = 2
EXPERT_FF = 512

IN_SPLITS = (S5_WIDTH, GLA_KW, GLA_KW, GLA_VW, GLA_VW, 2 * GLA_GATE_RANK, HGRN_KW, 2 * HGRN_KW, HGRN_VW, HGRN_VW, N_BRANCHES * D_MODEL)
IN_WIDTH = S5_WIDTH + 2 * GLA_KW + 2 * GLA_VW + 2 * GLA_GATE_RANK + 3 * HGRN_KW + 2 * HGRN_VW + N_BRANCHES * D_MODEL

kernel_name = 'hybrid_s5_gla_hgrn2_moe_dit'


def _rmsnorm(t, g):
    tf = t.astype(jnp.float32)
    y = tf * lax.rsqrt(jnp.mean(tf * tf, axis=-1, keepdims=True) + NORM_EPS)
    return (y * g.astype(jnp.float32)).astype(t.dtype)


def _modulate(t, shift, scale):
    return t * (1 + scale) + shift


def _split_cols(t, sizes):
    out, start = [], 0
    for s in sizes:
        out.append(t[..., start:start + s])
        start += s
    return out


def _flip_parts(t, n_ctx):
    return jnp.concatenate([jnp.flip(t[:, :n_ctx], axis=1), jnp.flip(t[:, n_ctx:], axis=1)], axis=1)


def _latent_to_cols(t, n_ctx, rows):
    lat = t[:, n_ctx:]
    b, tail = lat.shape[0], lat.shape[2:]
    lat = lat.reshape((b, rows, GRID_W) + tail).swapaxes(1, 2).reshape((b, rows * GRID_W) + tail)
    return jnp.concatenate([t[:, :n_ctx], lat], axis=1)


def _latent_to_rows(t, n_ctx, rows):
    lat = t[:, n_ctx:]
    b, tail = lat.shape[0], lat.shape[2:]
    lat = lat.reshape((b, GRID_W, rows) + tail).swapaxes(1, 2).reshape((b, rows * GRID_W) + tail)
    return jnp.concatenate([t[:, :n_ctx], lat], axis=1)


def _chunk_gla(q, k, v, log_f):
    b, n, h, dk = q.shape
    dv = v.shape[-1]
    nc = n // CHUNK

    def chunks(t):
        return t.astype(jnp.float32).reshape(b, nc, CHUNK, h, t.shape[-1]).transpose(1, 0, 3, 2, 4)

    causal = jnp.tril(jnp.ones((CHUNK, CHUNK), dtype=bool))

    def step(state, inp):
        qc, kc, vc, gc = inp
        cum = jnp.cumsum(gc, axis=2)
        o_inter = jnp.einsum('bhck,bhkv->bhcv', qc * jnp.exp(cum), state)
        rel = cum[:, :, :, None, :] - cum[:, :, None, :, :]
        rel = jnp.where(causal[:, :, None], rel, NEG_BIG)
        scores = jnp.einsum('bhtk,bhsk,bhtsk->bhts', qc, kc, jnp.exp(rel))
        o_intra = jnp.einsum('bhts,bhsv->bhtv', scores, vc)
        last = cum[:, :, -1:, :]
        new_state = (jnp.exp(last[:, :, 0, :, None]) * state
                     + jnp.einsum('bhck,bhcv->bhkv', kc * jnp.exp(last - cum), vc))
        return new_state, o_inter + o_intra

    s0 = jnp.zeros((b, h, dk, dv), jnp.float32)
    _, o = lax.scan(step, s0, (chunks(q), chunks(k), chunks(v), chunks(log_f)))
    return o.transpose(1, 0, 3, 2, 4).reshape(b, n, h, dv)


def _bidir_gla(q, k_f, k_b, v, g_f, g_b, n_ctx):
    o_f = _chunk_gla(q, k_f, v, g_f)
    o_b = _chunk_gla(_flip_parts(q, n_ctx), _flip_parts(k_b, n_ctx), _flip_parts(v, n_ctx), _flip_parts(g_b, n_ctx))
    return o_f + _flip_parts(o_b, n_ctx)


def _s5_discretize(lam_re, lam_im, log_step, b_re, b_im):
    f32 = jnp.float32
    lam_re, lam_im = lam_re.astype(f32), lam_im.astype(f32)
    dt = jnp.exp(log_step.astype(f32))[:, None]
    mag = jnp.exp(lam_re * dt)
    ab_re, ab_im = mag * jnp.cos(lam_im * dt), mag * jnp.sin(lam_im * dt)
    den = lam_re * lam_re + lam_im * lam_im
    nr, ni = ab_re - 1.0, ab_im
    fr = (nr * lam_re + ni * lam_im) / den
    fi = (ni * lam_re - nr * lam_im) / den
    b_re, b_im = b_re.astype(f32), b_im.astype(f32)
    bb_re = fr[..., None] * b_re - fi[..., None] * b_im
    bb_im = fr[..., None] * b_im + fi[..., None] * b_re
    return ab_re, ab_im, bb_re, bb_im


def _s5_scan(u, ab_re, ab_im, bb_re, bb_im):
    n = u.shape[1]
    bu_re = jnp.einsum('bngh,gph->bngp', u, bb_re)
    bu_im = jnp.einsum('bngh,gph->bngp', u, bb_im)
    a_re = jnp.broadcast_to(ab_re, (1, n) + ab_re.shape)
    a_im = jnp.broadcast_to(ab_im, (1, n) + ab_im.shape)

    def combine(e1, e2):
        ar1, ai1, br1, bi1 = e1
        ar2, ai2, br2, bi2 = e2
        return (ar2 * ar1 - ai2 * ai1, ar2 * ai1 + ai2 * ar1,
                ar2 * br1 - ai2 * bi1 + br2, ar2 * bi1 + ai2 * br1 + bi2)

    _, _, x_re, x_im = lax.associative_scan(combine, (a_re, a_im, bu_re, bu_im), axis=1)
    return x_re, x_im


def _s5_mixer(u, n_ctx, lam_re, lam_im, log_step, b_re, b_im, c_re, c_im, d_skip, glu_w, glu_b):
    f32 = jnp.float32
    bsz, n, _ = u.shape
    uf = u.astype(f32)
    ug = uf.reshape(bsz, n, S5_GROUPS, S5_GROUP_CH)
    fwd_re, fwd_im = _s5_scan(ug, *_s5_discretize(lam_re[0], lam_im[0], log_step[0], b_re, b_im))
    bwd_re, bwd_im = _s5_scan(_flip_parts(ug, n_ctx), *_s5_discretize(lam_re[1], lam_im[1], log_step[1], b_re, b_im))
    x_re = fwd_re + _flip_parts(bwd_re, n_ctx)
    x_im = fwd_im + _flip_parts(bwd_im, n_ctx)
    y = (jnp.einsum('bngp,ghp->bngh', x_re, c_re.astype(f32))
         - jnp.einsum('bngp,ghp->bngh', x_im, c_im.astype(f32)))
    y = y.reshape(bsz, n, S5_WIDTH) + d_skip.astype(f32) * uf
    y = jax.nn.gelu(y)
    y = y * jax.nn.sigmoid(y @ glu_w.astype(f32) + glu_b.astype(f32))
    return y.astype(u.dtype)


def _gla_mixer(q, k, v, r, code, gate_up, gate_b, norm_g, n_ctx):
    f32 = jnp.float32
    bsz, n, _ = q.shape

    def heads(t, d):
        return t.astype(f32).reshape(bsz, n, GLA_HEADS, d)

    def log_gate(cd, w, bias):
        return jax.nn.log_sigmoid((cd @ w + bias).astype(f32)) / GLA_GATE_NORM

    qh = heads(q, GLA_DK) * (GLA_DK ** -0.5)
    kh = heads(k, GLA_DK)
    vh = heads(v, GLA_DV)
    g_f = heads(log_gate(code[..., :GLA_GATE_RANK], gate_up[0], gate_b[0]), GLA_DK)
    g_b = heads(log_gate(code[..., GLA_GATE_RANK:], gate_up[1], gate_b[1]), GLA_DK)
    o = _bidir_gla(qh, kh, kh, vh, g_f, g_b, n_ctx)
    o = _rmsnorm(o, norm_g).reshape(bsz, n, GLA_VW)
    return (o * jax.nn.silu(r.astype(f32))).astype(q.dtype)


def _hgrn2_mixer(q_raw, f_raw, i_in, g_out, lb, norm_g, n_ctx, rows):
    f32 = jnp.float32
    bsz, n, _ = q_raw.shape
    lbf = lb.astype(f32)

    def gates(fr):
        fr = fr.astype(f32)
        f = lbf + (1.0 - lbf) * jax.nn.sigmoid(fr)
        log_f = jnp.log(jnp.maximum(f, TINY))
        k = (1.0 - lbf) * jax.nn.sigmoid(-fr)
        return log_f, k

    def prep(t, d):
        return _latent_to_cols(t.reshape(bsz, n, HGRN_HEADS, d), n_ctx, rows)

    log_f_f, k_f = gates(f_raw[..., :HGRN_KW])
    log_f_b, k_b = gates(f_raw[..., HGRN_KW:])
    q = jax.nn.silu(q_raw.astype(f32))
    o = _bidir_gla(prep(q, HGRN_EXPAND), prep(k_f, HGRN_EXPAND), prep(k_b, HGRN_EXPAND),
                   prep(i_in.astype(f32), HGRN_DV), prep(log_f_f, HGRN_EXPAND), prep(log_f_b, HGRN_EXPAND), n_ctx)
    o = _latent_to_rows(o, n_ctx, rows)
    o = _rmsnorm(o, norm_g).reshape(bsz, n, HGRN_VW)
    return (o * jax.nn.silu(g_out.astype(f32))).astype(q_raw.dtype)


def _moe(h, router_w, router_b, w_up, w_down):
    f32 = jnp.float32
    probs = jax.nn.softmax((h @ router_w).astype(f32), axis=-1)
    sel = probs + router_b.astype(f32)
    grouped = sel.reshape(sel.shape[:-1] + (N_EXPERT_GROUPS, EXPERTS_PER_GROUP))
    group_score = lax.top_k(grouped, TOP_K)[0].sum(axis=-1)
    best = jnp.argmax(group_score, axis=-1)
    in_best = jnp.arange(N_EXPERT_GROUPS) == best[..., None]
    masked = jnp.where(in_best[..., None], grouped, NEG_BIG).reshape(sel.shape)
    _, idx = lax.top_k(masked, TOP_K)
    w_sel = jnp.take_along_axis(probs, idx, axis=-1)
    w_sel = w_sel / jnp.sum(w_sel, axis=-1, keepdims=True)
    gates = jnp.einsum('bnk,bnke->bne', w_sel, jax.nn.one_hot(idx, N_EXPERTS, dtype=f32))
    out = jnp.zeros(h.shape, f32)
    for e in range(N_EXPERTS):
        gu = h @ w_up[e]
        hid = jax.nn.silu(gu[..., :EXPERT_FF]) * gu[..., EXPERT_FF:]
        out = out + gates[..., e:e + 1] * (hid @ w_down[e]).astype(f32)
    return out.astype(h.dtype)


def setup_inputs(seed: int = 0) -> dict:
    key = jax.random.key(seed)
    ks = iter(jax.random.split(key, 40))
    f32 = jnp.float32
    D = D_MODEL

    def nrm(shape, scale):
        return jax.random.normal(next(ks), shape, f32) * scale

    x = nrm((BATCH, SEQ, D), 1.0)
    c = nrm((BATCH, D), 1.0)
    ctx = nrm((BATCH, CTX_LEN, D), 1.0)
    c_ctx = nrm((D,), 1.0)
    w_mod = nrm((DEPTH, D, 6 * D), 0.5 * D ** -0.5)
    b_mod = nrm((DEPTH, 6 * D), 0.02)
    norm_mix_g = 1.0 + nrm((DEPTH, D), 0.02)
    norm_ffn_g = 1.0 + nrm((DEPTH, D), 0.02)
    w_in = nrm((DEPTH, D, IN_WIDTH), D ** -0.5)
    n_idx = jnp.arange(S5_STATE, dtype=f32)
    s5_lam_re = -0.5 + nrm((DEPTH, 2, S5_GROUPS, S5_STATE), 0.01)
    s5_lam_im = math.pi * n_idx + nrm((DEPTH, 2, S5_GROUPS, S5_STATE), 0.01)
    s5_log_step = jax.random.uniform(next(ks), (DEPTH, 2, S5_GROUPS), f32,
                                     minval=math.log(S5_DT_MIN), maxval=math.log(S5_DT_MAX))
    s5_b_re = nrm((DEPTH, S5_GROUPS, S5_STATE, S5_GROUP_CH), (2 * S5_GROUP_CH) ** -0.5)
    s5_b_im = nrm((DEPTH, S5_GROUPS, S5_STATE, S5_GROUP_CH), (2 * S5_GROUP_CH) ** -0.5)
    s5_c_re = nrm((DEPTH, S5_GROUPS, S5_GROUP_CH, S5_STATE), 2.0 * S5_STATE ** -0.5)
    s5_c_im = nrm((DEPTH, S5_GROUPS, S5_GROUP_CH, S5_STATE), 2.0 * S5_STATE ** -0.5)
    s5_d = nrm((DEPTH, S5_WIDTH), 0.5)
    s5_glu_w = nrm((DEPTH, S5_WIDTH, S5_WIDTH), S5_WIDTH ** -0.5)
    s5_glu_b = nrm((DEPTH, S5_WIDTH), 0.02)
    gla_gate_up = nrm((DEPTH, 2, GLA_GATE_RANK, GLA_KW), GLA_GATE_RANK ** -0.5)
    gla_gate_b = nrm((DEPTH, 2, GLA_KW), 0.1)
    gla_norm_g = 1.0 + nrm((DEPTH, GLA_DV), 0.02)
    hgrn_lower = nrm((DEPTH, HGRN_KW), 0.5)
    hgrn_norm_g = 1.0 + nrm((DEPTH, HGRN_DV), 0.02)
    w_branch_s5 = nrm((DEPTH, S5_WIDTH, D), S5_WIDTH ** -0.5)
    w_branch_gla = nrm((DEPTH, GLA_VW, D), GLA_VW ** -0.5)
    w_branch_hgrn = nrm((DEPTH, HGRN_VW, D), HGRN_VW ** -0.5)
    w_out = nrm((DEPTH, D, D), D ** -0.5)
    router_w = nrm((D, N_EXPERTS), D ** -0.5)
    router_b = nrm((N_EXPERTS,), 0.01)
    moe_w_up = nrm((DEPTH, N_EXPERTS, D, 2 * EXPERT_FF), D ** -0.5)
    moe_w_down = nrm((DEPTH, N_EXPERTS, EXPERT_FF, D), EXPERT_FF ** -0.5)
    final_norm_g = 1.0 + nrm((D,), 0.02)
    return {'x': x, 'c': c, 'ctx': ctx, 'c_ctx': c_ctx, 'w_mod': w_mod, 'b_mod': b_mod,
            'norm_mix_g': norm_mix_g, 'norm_ffn_g': norm_ffn_g, 'w_in': w_in,
            's5_lam_re': s5_lam_re, 's5_lam_im': s5_lam_im, 's5_log_step': s5_log_step,
            's5_b_re': s5_b_re, 's5_b_im': s5_b_im, 's5_c_re': s5_c_re, 's5_c_im': s5_c_im,
            's5_d': s5_d, 's5_glu_w': s5_glu_w, 's5_glu_b': s5_glu_b,
            'gla_gate_up': gla_gate_up, 'gla_gate_b': gla_gate_b, 'gla_norm_g': gla_norm_g,
            'hgrn_lower': hgrn_lower, 'hgrn_norm_g': hgrn_norm_g,
            'w_branch_s5': w_branch_s5, 'w_branch_gla': w_branch_gla, 'w_branch_hgrn': w_branch_hgrn,
            'w_out': w_out, 'router_w': router_w, 'router_b': router_b,
            'moe_w_up': moe_w_up, 'moe_w_down': moe_w_down, 'final_norm_g': final_norm_g}


def reference(x, c, ctx, c_ctx, w_mod, b_mod, norm_mix_g, norm_ffn_g, w_in,
              s5_lam_re, s5_lam_im, s5_log_step, s5_b_re, s5_b_im, s5_c_re, s5_c_im,
              s5_d, s5_glu_w, s5_glu_b, gla_gate_up, gla_gate_b, gla_norm_g,
              hgrn_lower, hgrn_norm_g, w_branch_s5, w_branch_gla, w_branch_hgrn, w_out,
              router_w, router_b, moe_w_up, moe_w_down, final_norm_g):
    n_ctx = ctx.shape[1]
    rows = x.shape[1] // GRID_W
    p_lb = jax.nn.softmax(hgrn_lower.astype(jnp.float32), axis=0)
    lower_bounds = jnp.cumsum(p_lb, axis=0) - p_lb[0]
    silu_c = jax.nn.silu(c)
    silu_cc = jax.nn.silu(c_ctx)
    h_ctx, h_lat = ctx, x
    for layer in range(DEPTH):
        last = layer == DEPTH - 1
        m_lat = jnp.split((silu_c @ w_mod[layer] + b_mod[layer])[:, None, :], 6, axis=-1)
        m_ctx = jnp.split(silu_cc @ w_mod[layer] + b_mod[layer], 6, axis=-1)
        u = jnp.concatenate([
            _modulate(_rmsnorm(h_ctx, norm_mix_g[layer]), m_ctx[0], m_ctx[1]),
            _modulate(_rmsnorm(h_lat, norm_mix_g[layer]), m_lat[0], m_lat[1])], axis=1)
        (u_s5, q_gla, k_gla, v_gla, r_gla, code_gla,
         q_hg, f_hg, i_hg, o_gate_hg, gate_logits) = _split_cols(u @ w_in[layer], IN_SPLITS)
        y_s5 = _s5_mixer(u_s5, n_ctx, s5_lam_re[layer], s5_lam_im[layer], s5_log_step[layer],
                         s5_b_re[layer], s5_b_im[layer], s5_c_re[layer], s5_c_im[layer],
                         s5_d[layer], s5_glu_w[layer], s5_glu_b[layer])
        y_gla = _gla_mixer(q_gla, k_gla, v_gla, r_gla, code_gla, gla_gate_up[layer], gla_gate_b[layer],
                           gla_norm_g[layer], n_ctx)
        y_hg = _hgrn2_mixer(q_hg, f_hg, i_hg, o_gate_hg, lower_bounds[layer], hgrn_norm_g[layer], n_ctx, rows)
        keep = n_ctx if last else 0
        gate_a, gate_b, gate_c = jnp.split(jax.nn.sigmoid(gate_logits[:, keep:]), N_BRANCHES, axis=-1)
        merged = (gate_a * (y_s5[:, keep:] @ w_branch_s5[layer])
                  + gate_b * (y_gla[:, keep:] @ w_branch_gla[layer])
                  + gate_c * (y_hg[:, keep:] @ w_branch_hgrn[layer]))
        mix = merged @ w_out[layer]
        if last:
            h_lat = h_lat + m_lat[2] * mix
            v_in = _modulate(_rmsnorm(h_lat, norm_ffn_g[layer]), m_lat[3], m_lat[4])
            h_lat = h_lat + m_lat[5] * _moe(v_in, router_w, router_b, moe_w_up[layer], moe_w_down[layer])
        else:
            h_ctx = h_ctx + m_ctx[2] * mix[:, :n_ctx]
            h_lat = h_lat + m_lat[2] * mix[:, n_ctx:]
            v_in = jnp.concatenate([
                _modulate(_rmsnorm(h_ctx, norm_ffn_g[layer]), m_ctx[3], m_ctx[4]),
                _modulate(_rmsnorm(h_lat, norm_ffn_g[layer]), m_lat[3], m_lat[4])], axis=1)
            ff = _moe(v_in, router_w, router_b, moe_w_up[layer], moe_w_down[layer])
            h_ctx = h_ctx + m_ctx[5] * ff[:, :n_ctx]
            h_lat = h_lat + m_lat[5] * ff[:, n_ctx:]
    return _rmsnorm(h_lat, final_norm_g)
```

```python
import math
import numpy as np
from contextlib import ExitStack
import concourse.bass as bass
import concourse.mybir as mybir
from concourse.bass_utils import run_bass_kernel_spmd

F32 = mybir.dt.float32
BF16 = mybir.dt.bfloat16
ALU = mybir.AluOpType
AF = mybir.ActivationFunctionType
AX = mybir.AxisListType

T = 4352
NT = 34
D = 1024
NCTX = 256
NLAT = 4096
N_DMA_SEMS = 48
EPS = 1e-6


class Buf:
    __slots__ = ("t", "writes", "reads", "name", "psum")

    def __init__(self, t, name="", psum=False):
        self.t = t
        self.writes = {}
        self.reads = {}
        self.name = name
        self.psum = psum

    def __getitem__(self, idx):
        return self.t[idx]


class MK:
    def __init__(self, nc, stack):
        self.nc = nc
        self.stack = stack
        self.eng = {"pe": nc.tensor, "act": nc.scalar, "dve": nc.vector,
                    "pool": nc.gpsimd, "sp": nc.sync}
        self.sem = {}
        self.cnt = {}
        self.seen = {}
        for e in self.eng:
            self.sem[e] = stack.enter_context(nc.semaphore("s_" + e))
            self.cnt[e] = 0
            self.seen[e] = {}
        self.dsem = [stack.enter_context(nc.semaphore("d%d" % i)) for i in range(N_DMA_SEMS)]
        self.dval = [0] * N_DMA_SEMS
        self.dnext = 0
        self.n_inst = 0
        self.n_wait = 0
        self.same_engine_sync = {"pe": False, "act": True, "dve": True, "pool": True, "sp": True}
        self.pstack = None
        self.uid = 0

    def begin_phase(self):
        self.pstack = ExitStack()

    def end_phase(self):
        self.barrier()
        self.pstack.close()
        self.pstack = None

    def sb(self, name, shape, dt=F32):
        self.uid += 1
        st = self.pstack if self.pstack is not None else self.stack
        return Buf(st.enter_context(self.nc.sbuf_tensor("%s_%d" % (name, self.uid), shape, dt)), name)

    def ps(self, name, shape, dt=F32):
        self.uid += 1
        st = self.pstack if self.pstack is not None else self.stack
        nbytes = int(np.prod(shape[1:])) * (4 if dt == F32 else 2)
        assert nbytes % 2048 == 0, ("psum tiles must be whole banks", name, shape)
        return Buf(st.enter_context(self.nc.psum_tensor("%s_%d" % (name, self.uid), shape, dt)), name, psum=True)

    def dram(self, name, shape, dt=F32, kind="Internal"):
        return Buf(self.nc.dram_tensor(name, shape, dt, kind=kind), name)

    def sub(self, buf, name=""):
        return Buf(buf.t, name)

    def _key_sem(self, key):
        if isinstance(key, str):
            return self.sem[key]
        return self.dsem[key]

    def _wait(self, e, deps):
        seen = self.seen[e]
        for key, val in deps.items():
            if key == e and not self.same_engine_sync[e]:
                continue
            if seen.get(key, 0) >= val:
                continue
            self.eng[e].wait_ge(self._key_sem(key), val)
            self.n_wait += 1
            seen[key] = val

    @staticmethod
    def _merge(dst, src):
        for k, v in src.items():
            if dst.get(k, 0) < v:
                dst[k] = v

    def _deps(self, reads, writes):
        deps = {}
        for b in reads:
            if b.psum:
                self._merge(deps, b.reads)
        for b in reads:
            self._merge(deps, b.writes)
        for b in writes:
            self._merge(deps, b.writes)
            self._merge(deps, b.reads)
        return deps

    def _commit(self, tok, reads, writes):
        k, v = tok
        for b in reads:
            if b.psum:
                b.reads = {kk: vv for kk, vv in b.reads.items() if kk == k}
            if b.reads.get(k, 0) < v:
                b.reads[k] = v
        for b in writes:
            b.writes = {k: v}
            b.reads = {}

    def op(self, e, fn, reads=(), writes=()):
        deps = self._deps(reads, writes)
        self._wait(e, deps)
        ins = fn(self.eng[e])
        self.cnt[e] += 1
        ins.then_inc(self.sem[e], 1)
        self._commit((e, self.cnt[e]), reads, writes)
        self.n_inst += 1
        return ins

    def dma(self, q, out_ap, in_ap, reads=(), writes=(), **kw):
        i = self.dnext
        self.dnext = (self.dnext + 1) % N_DMA_SEMS
        deps = self._deps(reads, writes)
        if self.dval[i] > 0:
            deps[i] = max(deps.get(i, 0), self.dval[i])
        self._wait(q, deps)
        self.dval[i] += 16
        ins = self.eng[q].dma_start(out=out_ap, in_=in_ap, **kw)
        ins.then_inc(self.dsem[i], 16)
        self._commit((i, self.dval[i]), reads, writes)
        self.n_inst += 1
        return ins

    def barrier(self):
        deps = {}
        for e in self.eng:
            if self.cnt[e] > 0:
                deps[e] = self.cnt[e]
        for i in range(N_DMA_SEMS):
            if self.dval[i] > 0:
                deps[i] = self.dval[i]
        for e in self.eng:
            d = {k: v for k, v in deps.items() if k != e}
            self._wait(e, d)

    def finish(self, bufs, e="sp"):
        deps = {}
        for b in bufs:
            self._merge(deps, b.writes)
        self._wait(e, deps)

    def mm(self, out, lhsT, rhs, start, stop, reads, writes):
        return self.op("pe", lambda e: e.matmul(out, lhsT, rhs, start=start, stop=stop), reads, writes)

    def tr(self, out, in_, ident, reads, writes):
        return self.op("pe", lambda e: e.transpose(out, in_, ident), reads, writes)


def make_ident(mk, dt, name):
    idf = mk.sb(name + "f", [128, 128], F32)
    mk.op("pool", lambda e: e.memset(idf[:], 1.0), writes=[idf])
    mk.op("pool", lambda e: e.affine_select(out=idf[:], in_=idf[:], pattern=[[-1, 128]],
                                             compare_op=ALU.is_equal, fill=0.0, base=0,
                                             channel_multiplier=1), reads=[idf], writes=[idf])
    if dt == F32:
        return idf
    idb = mk.sb(name + "b", [128, 128], dt)
    mk.op("dve", lambda e: e.tensor_copy(idb[:], idf[:]), reads=[idf], writes=[idb])
    return idb


class G:
    pass


PARAM_SHAPES = {
    "w_mod": [2, 1024, 6144], "b_mod": [2, 6144], "norm_mix_g": [2, 1024], "norm_ffn_g": [2, 1024],
    "w_in": [2, 1024, 6176], "s5_lam_re": [2, 2, 16, 64], "s5_lam_im": [2, 2, 16, 64],
    "s5_log_step": [2, 2, 16], "s5_b_re": [2, 16, 64, 16], "s5_b_im": [2, 16, 64, 16],
    "s5_c_re": [2, 16, 16, 64], "s5_c_im": [2, 16, 16, 64], "s5_d": [2, 256],
    "s5_glu_w": [2, 256, 256], "s5_glu_b": [2, 256], "gla_gate_up": [2, 2, 16, 256],
    "gla_gate_b": [2, 2, 256], "gla_norm_g": [2, 128], "hgrn_lower": [2, 256], "hgrn_norm_g": [2, 64],
    "w_branch_s5": [2, 256, 1024], "w_branch_gla": [2, 512, 1024], "w_branch_hgrn": [2, 256, 1024],
    "w_out": [2, 1024, 1024], "router_w": [1024, 16], "router_b": [16],
    "moe_w_up": [2, 16, 1024, 1024], "moe_w_down": [2, 16, 512, 1024], "final_norm_g": [1024],
}
ACT_SHAPES = {"x": [NLAT, D], "ctx": [NCTX, D], "c": [D], "c_ctx": [D]}

C_S5 = 0
C_GQ = 256
C_GK = 512
C_GV = 768
C_GR = 1280
C_GC = 1792
C_HQ = 1824
C_HF = 2080
C_HI = 2592
C_HO = 2848
C_GATE = 3104
IN_W = 6176

SCRATCH = {
    "H": ([T, D], F32),
    "UT": ([8, 128, T], BF16),
    "MOD": ([2, 6144], F32),
}


def declare(mk, g, ext_in=(), ext_out=()):
    nc = mk.nc
    for k, s in list(ACT_SHAPES.items()) + list(PARAM_SHAPES.items()):
        setattr(g, k, mk.dram(k, s, F32, kind="ExternalInput"))
    for k, (s, dt) in SCRATCH.items():
        kind = "Internal"
        if k in ext_in:
            kind = "ExternalInput"
        if k in ext_out:
            kind = "ExternalOutput"
        setattr(g, k, mk.dram(k, s, dt, kind=kind))
    g.OUT = mk.dram("out", [NLAT, D], F32, kind="ExternalOutput")


def setup_consts(mk, g):
    g.identb = make_ident(mk, BF16, "idb")
    g.identf = g.identb
    idf = mk.sb("idf32", [128, 128], F32)
    mk.op("pool", lambda e: e.memset(idf[:], 1.0), writes=[idf])
    mk.op("pool", lambda e: e.affine_select(out=idf[:], in_=idf[:], pattern=[[-1, 128]],
                                             compare_op=ALU.is_equal, fill=0.0, base=0,
                                             channel_multiplier=1), reads=[idf], writes=[idf])
    g.identf = idf
    g.eps_col = mk.sb("epsc", [128, 1], F32)
    mk.op("pool", lambda e: e.memset(g.eps_col[:], EPS), writes=[g.eps_col])
    g.one_col = mk.sb("onec", [128, 1], F32)
    mk.op("pool", lambda e: e.memset(g.one_col[:], 1.0), writes=[g.one_col])


def phase_init_h(mk, g):
    mk.dma("sp", g.H[0:NCTX, :], g.ctx[:, :], reads=[g.ctx], writes=[g.H])
    for j in range(4):
        mk.dma("sp" if j % 2 == 0 else "act", g.H[NCTX + j * 1024:NCTX + (j + 1) * 1024, :],
               g.x[j * 1024:(j + 1) * 1024, :], reads=[g.x], writes=[g.H])


def phase_mod(mk, g, layer):
    nc = mk.nc
    if not hasattr(g, "M_all"):
        g.M_all = mk.sb("M_all", [128, 48, 2], F32)
        g.A1 = mk.sb("A1", [128, 8, 2], F32)
        g.A2 = mk.sb("A2", [128, 8, 2], F32)
    mk.begin_phase()
    c0 = mk.sb("c0", [128, 8], F32)
    c1 = mk.sb("c1", [128, 8], F32)
    sc = mk.sb("sc", [128, 8, 2], F32)
    bcol = mk.sb("bcol", [128, 48], F32)
    gm = mk.sb("gm", [128, 8], F32)
    gf = mk.sb("gf", [128, 8], F32)
    with nc.allow_non_contiguous_dma(reason="tiny column loads"):
        mk.dma("sp", c0[:], g.c[:].rearrange("(j p) -> p j", p=128), reads=[g.c], writes=[c0])
        mk.dma("sp", c1[:], g.c_ctx[:].rearrange("(j p) -> p j", p=128), reads=[g.c_ctx], writes=[c1])
        mk.dma("sp", bcol[:], g.b_mod[layer, :].rearrange("(j p) -> p j", p=128), reads=[g.b_mod], writes=[bcol])
        mk.dma("sp", gm[:], g.norm_mix_g[layer, :].rearrange("(j p) -> p j", p=128), reads=[g.norm_mix_g], writes=[gm])
        mk.dma("sp", gf[:], g.norm_ffn_g[layer, :].rearrange("(j p) -> p j", p=128), reads=[g.norm_ffn_g], writes=[gf])
    mk.op("act", lambda e: e.activation(sc[:, :, 0], c0[:], AF.Silu), reads=[c0], writes=[sc])
    mk.op("act", lambda e: e.activation(sc[:, :, 1], c1[:], AF.Silu), reads=[c1, sc], writes=[sc])
    pm = mk.ps("pm", [128, 256, 2], F32)
    wb = [mk.sb("wmodblk%d" % i, [128, 8, 512], F32) for i in range(2)]
    for cb in range(12):
        w = wb[cb % 2]
        mk.dma("sp" if cb % 2 == 0 else "act", w[:],
               g.w_mod[layer, :, cb * 512:(cb + 1) * 512].rearrange("(kc p) n -> p kc n", p=128),
               reads=[g.w_mod], writes=[w])
        for ft in range(4):
            j = cb * 4 + ft
            for kc in range(8):
                mk.mm(pm[:, j, :], w[:, kc, ft * 128:(ft + 1) * 128], sc[:, kc, :],
                      start=(kc == 0), stop=(kc == 7), reads=[w, sc], writes=[pm])
    for wv in range(2):
        mk.op("dve", lambda e: e.tensor_tensor(g.M_all[:, :, wv], pm[:, 0:48, wv], bcol[:], ALU.add),
              reads=[pm, bcol, g.M_all], writes=[g.M_all])
    for wv in range(2):
        mk.op("dve", lambda e: e.scalar_tensor_tensor(g.A1[:, :, wv], g.M_all[:, 8:16, wv], 1.0, gm[:],
                                                       ALU.add, ALU.mult),
              reads=[g.M_all, gm, g.A1], writes=[g.A1])
        mk.op("dve", lambda e: e.scalar_tensor_tensor(g.A2[:, :, wv], g.M_all[:, 32:40, wv], 1.0, gf[:],
                                                       ALU.add, ALU.mult),
              reads=[g.M_all, gf, g.A2], writes=[g.A2])
    with nc.allow_non_contiguous_dma(reason="small modulation table"):
        for wv in range(2):
            mk.dma("sp", g.MOD[wv, :].rearrange("(j p) -> p j", p=128), g.M_all[:, :, wv], reads=[g.M_all], writes=[g.MOD])
    mk.end_phase()


def norm_tile(mk, g, h_t, xn, ss, rstd, junk):
    mk.op("act", lambda e: e.activation(junk[:], h_t[:], AF.Square, accum_out=ss[:, 0:1]),
          reads=[h_t], writes=[junk, ss])
    mk.op("act", lambda e: e.activation(rstd[:], ss[:], AF.Sqrt, scale=1.0 / D, bias=g.eps_col[:, 0:1]),
          reads=[ss, g.eps_col], writes=[rstd])
    mk.op("dve", lambda e: e.reciprocal(rstd[:], rstd[:]), reads=[rstd], writes=[rstd])
    mk.op("dve", lambda e: e.tensor_scalar(xn[:], h_t[:], rstd[:, 0:1], None, ALU.mult),
          reads=[h_t, rstd], writes=[xn])


def transpose_mod_tile(mk, g, xn, pT, ut, A, Bsh, wv, boff):
    for kc in range(8):
        mk.tr(pT[:, kc, :], xn[:, kc * 128:(kc + 1) * 128], g.identb[:], reads=[xn, g.identb], writes=[pT])
    tmp = Bsh if Bsh is not None else ut
    mk.op("dve", lambda e: e.tensor_tensor(tmp[:], pT[:], A[:, :, wv].unsqueeze(2).broadcast_to([128, 8, 128]), ALU.mult),
          reads=[pT, A], writes=[tmp])
    mk.op("dve", lambda e: e.tensor_tensor(ut[:], tmp[:], g.M_all[:, boff:boff + 8, wv].unsqueeze(2).broadcast_to([128, 8, 128]), ALU.add),
          reads=[tmp, g.M_all, ut], writes=[ut])


def phase_norm1(mk, g, layer):
    mk.begin_phase()
    hts = [mk.sb("h_t%d" % i, [128, D], F32) for i in range(2)]
    xns = [mk.sb("xn%d" % i, [128, D], BF16) for i in range(2)]
    uts = [mk.sb("ut%d" % i, [128, 8, 128], BF16) for i in range(2)]
    pTs = [mk.ps("pT%d" % i, [128, 8, 128], BF16) for i in range(2)]
    junk = mk.sb("junk", [128, D], BF16)
    sss = [mk.sb("ss%d" % i, [128, 1], F32) for i in range(2)]
    rstds = [mk.sb("rstd%d" % i, [128, 1], F32) for i in range(2)]
    for i in range(NT):
        b = i % 2
        wv = 1 if i < 2 else 0
        mk.dma("sp", hts[b][:], g.H[i * 128:(i + 1) * 128, :], reads=[g.H], writes=[hts[b]])
        norm_tile(mk, g, hts[b], xns[b], sss[b], rstds[b], junk)
        transpose_mod_tile(mk, g, xns[b], pTs[b], uts[b], g.A1, None, wv, 0)
        mk.dma("act", g.UT[:, :, i * 128:(i + 1) * 128].rearrange("k p t -> p k t"), uts[b][:],
               reads=[uts[b]], writes=[g.UT])
    mk.end_phase()


def push_scope(mk):
    if not hasattr(mk, "scopes"):
        mk.scopes = []
    mk.scopes.append(mk.pstack)
    mk.pstack = ExitStack()


def pop_scope(mk):
    mk.barrier()
    mk.pstack.close()
    mk.pstack = mk.scopes.pop()


SCRATCH.update({
    "U8D": ([16, 128, 544], F32),
    "Y8D": ([16, 128, 544], F32),
    "YS5T": ([2, 128, T], BF16),
})

TWO_PI = 2.0 * math.pi
S5_OFF = 64.0 * math.pi
S5_TAUS = [float(t) for t in range(-7, 9)] + [8.0 * (2 ** k) for k in range(10)]


def tau_idx(t):
    return int(t) + 7


def s5_tables(mk):
    WZ = mk.sb("s5WZ", [128, 2, 16, 2, 64], F32)
    CO = mk.sb("s5CO", [128, 2, 8, 2, 128], F32)
    TOEP = mk.sb("s5TOEP", [128, 16, 128], F32)
    EPD = mk.sb("s5EPD", [128, 2, 8, 10, 3], F32)
    return WZ, CO, TOEP, EPD


def s5_setup_gen(mk, g, layer, WZ, CO, TOEP, EPD, pz, py):
    nc = mk.nc
    NTAU = len(S5_TAUS)
    lr = mk.sb("lr", [128, 2, 8], F32)
    li = mk.sb("li", [128, 2, 8], F32)
    ls = mk.sb("ls", [128, 2, 8], F32)
    BT = [mk.sb("BT%d" % i, [128, 8, 16], F32) for i in range(2)]
    CT = [mk.sb("CT%d" % i, [128, 8, 16], F32) for i in range(2)]
    with nc.allow_non_contiguous_dma(reason="small s5 parameter tables"):
        for (dst, src) in ((lr, g.s5_lam_re), (li, g.s5_lam_im)):
            for d in range(2):
                mk.dma("pool", dst[:, d, :], src[layer, d].rearrange("(pr hf) p -> (hf p) pr", hf=2), reads=[src], writes=[dst])
        for hf in range(2):
            for d in range(2):
                mk.dma("pool", ls[64 * hf:64 * hf + 64, d, :], g.s5_log_step[layer, d, hf::2].partition_broadcast(64),
                       reads=[g.s5_log_step], writes=[ls])
            for (dst, src) in ((BT[0], g.s5_b_re), (BT[1], g.s5_b_im)):
                mk.dma("pool", dst[64 * hf:64 * hf + 64, :, :],
                       src[layer].rearrange("(pr hf) p h -> hf p pr h", hf=2)[hf], reads=[src], writes=[dst])
            for (dst, src) in ((CT[0], g.s5_c_re), (CT[1], g.s5_c_im)):
                for pr in range(8):
                    mk.dma("pool", dst[64 * hf:64 * hf + 64, pr, :],
                           src[layer, 2 * pr + hf].rearrange("h p -> p h"), reads=[src], writes=[dst])
    dtv = mk.sb("dtv", [128, 16], F32)
    zr = mk.sb("zr", [128, 16], F32)
    zi = mk.sb("zi", [128, 16], F32)
    lrf = lr[:].rearrange("p d r -> p (d r)")
    lif = li[:].rearrange("p d r -> p (d r)")
    mk.op("act", lambda e: e.activation(dtv[:], ls[:].rearrange("p d r -> p (d r)"), AF.Exp), reads=[ls], writes=[dtv])
    mk.op("dve", lambda e: e.tensor_tensor(zr[:], lrf, dtv[:], ALU.mult), reads=[lr, dtv], writes=[zr])
    mk.op("dve", lambda e: e.tensor_tensor(zi[:], lif, dtv[:], ALU.mult), reads=[li, dtv], writes=[zi])
    tau = mk.sb("tau", [128, NTAU], F32)
    for i, tv in enumerate(S5_TAUS):
        mk.op("pool", lambda e: e.memset(tau[:, i:i + 1], tv), reads=[tau], writes=[tau])
    shp = [128, 16, NTAU]
    ARG = mk.sb("ARG", shp, F32)
    MAG = mk.sb("MAG", shp, F32)
    SA = mk.sb("SA", shp, F32)
    CA = mk.sb("CA", shp, F32)
    ER = mk.sb("ER", shp, F32)
    EI = mk.sb("EI", shp, F32)
    taub = tau[:].unsqueeze(1).broadcast_to(shp)
    mk.op("dve", lambda e: e.tensor_tensor(ARG[:], zi[:].unsqueeze(2).broadcast_to(shp), taub, ALU.mult),
          reads=[zi, tau], writes=[ARG])
    mk.op("dve", lambda e: e.tensor_tensor(MAG[:], zr[:].unsqueeze(2).broadcast_to(shp), taub, ALU.mult),
          reads=[zr, tau], writes=[MAG])
    mk.op("act", lambda e: e.activation(MAG[:], MAG[:], AF.Exp), reads=[MAG], writes=[MAG])
    KI = mk.sb("KI", shp, mybir.dt.int32)
    KF = mk.sb("KF", shp, F32)

    def range_reduce(dst, addc):
        mk.op("dve", lambda e: e.tensor_scalar(dst[:], ARG[:], addc, None, ALU.add), reads=[ARG], writes=[dst])
        mk.op("dve", lambda e: e.tensor_scalar(KF[:], dst[:], 1.0 / TWO_PI, None, ALU.mult), reads=[dst], writes=[KF])
        mk.op("dve", lambda e: e.tensor_copy(KI[:], KF[:]), reads=[KF], writes=[KI])
        mk.op("dve", lambda e: e.tensor_copy(KF[:], KI[:]), reads=[KI], writes=[KF])
        mk.op("dve", lambda e: e.scalar_tensor_tensor(dst[:], KF[:], -TWO_PI, dst[:], ALU.mult, ALU.add), reads=[KF, dst], writes=[dst])
        mk.op("dve", lambda e: e.tensor_scalar(KF[:], dst[:], math.pi, TWO_PI, ALU.is_gt, ALU.mult), reads=[dst], writes=[KF])
        mk.op("dve", lambda e: e.tensor_tensor(dst[:], dst[:], KF[:], ALU.subtract), reads=[KF, dst], writes=[dst])
        mk.op("dve", lambda e: e.tensor_scalar(KF[:], dst[:], -math.pi, TWO_PI, ALU.is_lt, ALU.mult), reads=[dst], writes=[KF])
        mk.op("dve", lambda e: e.tensor_tensor(dst[:], dst[:], KF[:], ALU.add), reads=[KF, dst], writes=[dst])
        mk.op("dve", lambda e: e.tensor_scalar(dst[:], dst[:], math.pi, -math.pi, ALU.min, ALU.max), reads=[dst], writes=[dst])

    range_reduce(SA, S5_OFF)
    range_reduce(CA, S5_OFF + 0.5 * math.pi)
    mk.op("act", lambda e: e.activation(SA[:], SA[:], AF.Sin), reads=[SA], writes=[SA])
    mk.op("act", lambda e: e.activation(CA[:], CA[:], AF.Sin), reads=[CA], writes=[CA])
    mk.op("dve", lambda e: e.tensor_tensor(ER[:], MAG[:], CA[:], ALU.mult), reads=[MAG, CA], writes=[ER])
    mk.op("dve", lambda e: e.tensor_tensor(EI[:], MAG[:], SA[:], ALU.mult), reads=[MAG, SA], writes=[EI])
    EPDv = EPD[:].rearrange("p d r k c -> p (d r) k c")
    mk.op("dve", lambda e: e.tensor_copy(EPDv[:, :, :, 0], ER[:, :, 16:26]), reads=[ER, EPD], writes=[EPD])
    mk.op("dve", lambda e: e.tensor_copy(EPDv[:, :, :, 1], EI[:, :, 16:26]), reads=[EI, EPD], writes=[EPD])
    mk.op("dve", lambda e: e.tensor_scalar(EPDv[:, :, :, 2], EI[:, :, 16:26], -1.0, None, ALU.mult), reads=[EI, EPD], writes=[EPD])
    i1 = tau_idx(1)
    nr = mk.sb("nr", [128, 16], F32)
    den = mk.sb("den", [128, 16], F32)
    t1 = mk.sb("t1", [128, 16], F32)
    t2 = mk.sb("t2", [128, 16], F32)
    fr = mk.sb("fr", [128, 16], F32)
    fi = mk.sb("fi", [128, 16], F32)
    mk.op("dve", lambda e: e.tensor_scalar(nr[:], ER[:, :, i1], -1.0, None, ALU.add), reads=[ER], writes=[nr])
    mk.op("dve", lambda e: e.tensor_tensor(den[:], lrf, lrf, ALU.mult), reads=[lr], writes=[den])
    mk.op("dve", lambda e: e.tensor_tensor(t1[:], lif, lif, ALU.mult), reads=[li], writes=[t1])
    mk.op("dve", lambda e: e.tensor_tensor(den[:], den[:], t1[:], ALU.add), reads=[den, t1], writes=[den])
    mk.op("dve", lambda e: e.reciprocal(den[:], den[:]), reads=[den], writes=[den])
    mk.op("dve", lambda e: e.tensor_tensor(t1[:], nr[:], lrf, ALU.mult), reads=[nr, lr], writes=[t1])
    mk.op("dve", lambda e: e.tensor_tensor(t2[:], EI[:, :, i1], lif, ALU.mult), reads=[EI, li], writes=[t2])
    mk.op("dve", lambda e: e.tensor_tensor(t1[:], t1[:], t2[:], ALU.add), reads=[t1, t2], writes=[t1])
    mk.op("dve", lambda e: e.tensor_tensor(fr[:], t1[:], den[:], ALU.mult), reads=[t1, den], writes=[fr])
    mk.op("dve", lambda e: e.tensor_tensor(t1[:], EI[:, :, i1], lrf, ALU.mult), reads=[EI, lr], writes=[t1])
    mk.op("dve", lambda e: e.tensor_tensor(t2[:], nr[:], lif, ALU.mult), reads=[nr, li], writes=[t2])
    mk.op("dve", lambda e: e.tensor_tensor(t1[:], t1[:], t2[:], ALU.subtract), reads=[t1, t2], writes=[t1])
    mk.op("dve", lambda e: e.tensor_tensor(fi[:], t1[:], den[:], ALU.mult), reads=[t1, den], writes=[fi])
    BBR = mk.sb("BBR", [128, 2, 8, 16], F32)
    BBI = mk.sb("BBI", [128, 2, 8, 16], F32)
    tb1 = mk.sb("tb1", [128, 8, 16], F32)
    tb2 = mk.sb("tb2", [128, 8, 16], F32)
    for d in range(2):
        frb = fr[:, d * 8:(d + 1) * 8].unsqueeze(2).broadcast_to([128, 8, 16])
        fib = fi[:, d * 8:(d + 1) * 8].unsqueeze(2).broadcast_to([128, 8, 16])
        mk.op("dve", lambda e: e.tensor_tensor(tb1[:], BT[0][:], frb, ALU.mult), reads=[BT[0], fr], writes=[tb1])
        mk.op("dve", lambda e: e.tensor_tensor(tb2[:], BT[1][:], fib, ALU.mult), reads=[BT[1], fi], writes=[tb2])
        mk.op("dve", lambda e: e.tensor_tensor(BBR[:, d], tb1[:], tb2[:], ALU.subtract), reads=[tb1, tb2, BBR], writes=[BBR])
        mk.op("dve", lambda e: e.tensor_tensor(tb1[:], BT[1][:], frb, ALU.mult), reads=[BT[1], fr], writes=[tb1])
        mk.op("dve", lambda e: e.tensor_tensor(tb2[:], BT[0][:], fib, ALU.mult), reads=[BT[0], fi], writes=[tb2])
        mk.op("dve", lambda e: e.tensor_tensor(BBI[:, d], tb1[:], tb2[:], ALU.add), reads=[tb1, tb2, BBI], writes=[BBI])
    WZT = mk.sb("WZT", [128, 2, 8, 2, 8, 16], F32)
    COM = mk.sb("COM", [128, 2, 8, 2, 8, 16], F32)
    COv = CO[:].rearrange("p d r i (t h) -> p d r i t h", h=16)
    ta = mk.sb("ta", [128, 8, 16], F32)
    tb = mk.sb("tb", [128, 8, 16], F32)
    sh3 = [128, 8, 16]

    def cplx(dst_re, dst_im, xr, xi, er, ei, conj_im, dsts):
        mk.op("dve", lambda e: e.tensor_tensor(ta[:], xr, er, ALU.mult), reads=[BBR, BBI, CT[0], CT[1], ER, EI], writes=[ta])
        mk.op("dve", lambda e: e.tensor_tensor(tb[:], xi, ei, ALU.mult), reads=[BBR, BBI, CT[0], CT[1], ER, EI], writes=[tb])
        mk.op("dve", lambda e: e.tensor_tensor(dst_re, ta[:], tb[:], ALU.subtract), reads=[ta, tb] + dsts, writes=dsts)
        mk.op("dve", lambda e: e.tensor_tensor(ta[:], xr, ei, ALU.mult), reads=[BBR, BBI, CT[0], CT[1], ER, EI], writes=[ta])
        mk.op("dve", lambda e: e.tensor_tensor(tb[:], xi, er, ALU.mult), reads=[BBR, BBI, CT[0], CT[1], ER, EI], writes=[tb])
        if conj_im:
            mk.op("dve", lambda e: e.scalar_tensor_tensor(dst_im, ta[:], -1.0, tb[:], ALU.mult, ALU.subtract),
                  reads=[ta, tb] + dsts, writes=dsts)
        else:
            mk.op("dve", lambda e: e.tensor_tensor(dst_im, ta[:], tb[:], ALU.add), reads=[ta, tb] + dsts, writes=dsts)

    for d in range(2):
        for pr in range(8):
            col = d * 8 + pr
            if d == 0:
                e_wz = slice(tau_idx(7), tau_idx(0) - 1 if tau_idx(0) - 1 >= 0 else None, -1)
                e_co = slice(tau_idx(1), tau_idx(8) + 1)
                e_cm = slice(tau_idx(-7), tau_idx(0) + 1)
            else:
                e_wz = slice(tau_idx(0), tau_idx(7) + 1)
                e_co = slice(tau_idx(8), tau_idx(1) - 1, -1)
                e_cm = slice(tau_idx(0), tau_idx(-7) - 1 if tau_idx(-7) - 1 >= 0 else None, -1)

            def eb(tab, sl):
                return tab[:, col, sl].unsqueeze(2).broadcast_to(sh3)

            xbr = BBR[:, d, pr, :].unsqueeze(1).broadcast_to(sh3)
            xbi = BBI[:, d, pr, :].unsqueeze(1).broadcast_to(sh3)
            cplx(WZT[:, d, pr, 0], WZT[:, d, pr, 1], xbr, xbi, eb(ER, e_wz), eb(EI, e_wz), False, [WZT])
            xcr = CT[0][:, pr, :].unsqueeze(1).broadcast_to(sh3)
            xci = CT[1][:, pr, :].unsqueeze(1).broadcast_to(sh3)
            cplx(COv[:, d, pr, 0], COv[:, d, pr, 1], xcr, xci, eb(ER, e_co), eb(EI, e_co), True, [CO])
            cplx(COM[:, d, pr, 0], COM[:, d, pr, 1], xcr, xci, eb(ER, e_cm), eb(EI, e_cm), True, [COM])
            yield
    maskF = mk.sb("maskF", [128, 8, 16], F32)
    maskB = mk.sb("maskB", [128, 8, 16], F32)
    mk.op("pool", lambda e: e.memset(maskF[:], 1.0), writes=[maskF])
    mk.op("pool", lambda e: e.memset(maskB[:], 1.0), writes=[maskB])
    mk.op("pool", lambda e: e.affine_select(out=maskF[:], in_=maskF[:], pattern=[[16, 8], [0, 16]],
                                             compare_op=ALU.is_ge, fill=0.0, base=15, channel_multiplier=-1),
          reads=[maskF], writes=[maskF])
    mk.op("pool", lambda e: e.affine_select(out=maskB[:], in_=maskB[:], pattern=[[-16, 8], [0, 16]],
                                             compare_op=ALU.is_ge, fill=0.0, base=0, channel_multiplier=1),
          reads=[maskB], writes=[maskB])
    mF = maskF[:].rearrange("p t h -> p (t h)")
    mB = maskB[:].rearrange("p t h -> p (t h)")
    ttmp = mk.sb("ttmp", [128, 128], F32)
    for gg in range(16):
        pr, hf = gg // 2, gg % 2
        rows = slice(64 * hf, 64 * hf + 64)
        pyb = py[gg % 2]
        for d in range(2):
            for ri in range(2):
                mk.op("pe", lambda e: e.transpose(pz[gg % 2][:, d, ri * 64:(ri + 1) * 64],
                                                    WZT[rows, d, pr, ri].rearrange("p s h -> p (s h)"),
                                                    g.identf[rows, rows]),
                      reads=[WZT, g.identf], writes=[pz[gg % 2]])
                mk.mm(pyb[:, d, 0:128], WZT[rows, d, pr, ri].rearrange("p s h -> p (s h)"),
                      COM[rows, d, pr, ri].rearrange("p t h -> p (t h)"), start=(ri == 0), stop=(ri == 1),
                      reads=[WZT, COM], writes=[pyb])
        mk.op("act", lambda e: e.activation(WZ[:, :, gg].rearrange("p d i c -> p d (i c)"), pz[gg % 2][:, :, 0:128], AF.Copy),
              reads=[pz[gg % 2], WZ], writes=[WZ])
        mk.op("dve", lambda e: e.tensor_tensor(ttmp[:], pyb[:, 0, 0:128], mF, ALU.mult), reads=[pyb, maskF], writes=[ttmp])
        mk.op("dve", lambda e: e.tensor_tensor(TOEP[:, gg, :], pyb[:, 1, 0:128], mB, ALU.mult), reads=[pyb, maskB, TOEP], writes=[TOEP])
        mk.op("dve", lambda e: e.tensor_tensor(TOEP[:, gg, :], TOEP[:, gg, :], ttmp[:], ALU.add), reads=[TOEP, ttmp], writes=[TOEP])
        yield


def s5_main(mk, g, layer, WZ, CO, TOEP, EPD, pz, py):
    nc = mk.nc
    UR = mk.sb("UR", [128, 2, 8, 544], F32)
    U8 = mk.sb("U8", [128, 16, 544], F32)
    Ws5 = mk.sb("Ws5", [128, 8, 256], BF16)
    mk.dma("pool", Ws5[:], g.w_in[layer, :, C_S5:C_S5 + 256].rearrange("(kc p) n -> p kc n", p=128),
           reads=[g.w_in], writes=[Ws5])
    ubs = [mk.sb("ub%d" % i, [128, 8, 512], BF16) for i in range(2)]
    nblk = 9
    for blk in range(nblk):
        N = 512 if blk < 8 else 256
        t0 = blk * 512
        ub = ubs[blk % 2]
        mk.dma("sp", ub[:, :, 0:N], g.UT[:, :, t0:t0 + N].rearrange("k p t -> p k t"), reads=[g.UT], writes=[ub])
        for mt in range(2):
            pp = pz[mt]
            for kc in range(8):
                mk.mm(pp[:, 0, 0:N], Ws5[:, kc, mt * 128:(mt + 1) * 128], ub[:, kc, 0:N],
                      start=(kc == 0), stop=(kc == 7), reads=[Ws5, ub], writes=[pp])
            c0 = blk * 64
            ncn = N // 8
            mk.op("act" if mt == 0 else "dve",
                  (lambda e: e.activation(UR[:, mt, :, c0:c0 + ncn].rearrange("p s c -> p c s"),
                                          pp[:, 0, 0:N].rearrange("p (c s) -> p c s", s=8), AF.Copy)) if mt == 0 else
                  (lambda e: e.tensor_copy(UR[:, mt, :, c0:c0 + ncn].rearrange("p s c -> p c s"),
                                           pp[:, 0, 0:N].rearrange("p (c s) -> p c s", s=8))),
                  reads=[pp, UR], writes=[UR])
    for gg in range(16):
        mt, gl = gg // 8, gg % 8
        mk.dma("sp" if gg % 2 == 0 else "pool", g.U8D[gg].rearrange("(s h) c -> h s c", h=16),
               UR[16 * gl:16 * gl + 16, mt, :, :], reads=[UR], writes=[g.U8D])
    mk.dma("sp", U8[:], g.U8D[:, :, :].rearrange("g p c -> p g c"), reads=[g.U8D], writes=[U8])

    ZSs = [[mk.sb("ZS%d_%d" % (i, st_), [128, 2, 544], F32) for i in range(2)] for st_ in range(2)]
    ZQs = [[mk.sb("ZQ%d_%d" % (i, st_), [128, 2, 544], F32) for i in range(2)] for st_ in range(2)]
    XS = [mk.sb("XS%d" % i, [128, 2, 545], F32) for i in range(2)]
    ystage = [mk.sb("yst%d" % i, [128, 544], F32) for i in range(2)]
    dtmp = mk.sb("dtmp", [128, 544], F32)
    mk.op("pool", lambda e: e.memset(XS[0][:], 0.0), writes=[XS[0]])
    mk.op("pool", lambda e: e.memset(XS[1][:], 0.0), writes=[XS[1]])
    SEGS = [(0, 32, 0, 256), (32, 256, 0, 0), (288, 256, 1, 0)]
    nzp_box = [0]

    def zstage(pr):
        ZS = ZSs[pr % 2]
        nzp = nzp_box[0]
        for d in range(2):
            for ri in range(2):
                pzz = pz[nzp % 2]
                nzp += 1
                for hf in range(2):
                    gg = 2 * pr + hf
                    for (c0, n, bk, pc) in SEGS:
                        mk.mm(pzz[64 * hf:64 * hf + 64, bk, pc:pc + n], WZ[:, d, gg, ri, :], U8[:, gg, c0:c0 + n],
                              start=True, stop=True, reads=[WZ, U8], writes=[pzz])
                if d == 0:
                    mk.op("act", lambda e: e.activation(ZS[d][:, ri, 0:32], pzz[:, 0, 256:288], AF.Copy), reads=[pzz, ZS[d]], writes=[ZS[d]])
                    mk.op("act", lambda e: e.activation(ZS[d][:, ri, 32:544].rearrange("p (b c) -> p b c", b=2), pzz[:, :, 0:256], AF.Copy),
                          reads=[pzz, ZS[d]], writes=[ZS[d]])
                else:
                    mk.op("act", lambda e: e.activation(ZS[d][:, ri, 512:544], pzz[:, 0, 256:288], AF.Copy), reads=[pzz, ZS[d]], writes=[ZS[d]])
                    mk.op("act", lambda e: e.activation(ZS[d][:, ri, 0:512].rearrange("p (b c) -> p b c", b=2), pzz[:, :, 0:256], AF.Copy),
                          reads=[pzz, ZS[d]], writes=[ZS[d]])

        nzp_box[0] = nzp

    def rest_stage(pr):
        ZS = ZSs[pr % 2]
        ZQ = ZQs[pr % 2]
        PQ = [[ZS[0], ZQ[0]], [ZS[1], ZQ[1]]]
        KP = {id(b): mk.sub(b, "keep") for b in (ZS[0], ZQ[0], ZS[1], ZQ[1])}
        for k in range(10):
            s = 2 ** k
            for d in range(2):
                P, Q = PQ[d]
                ar = EPD[:, d, pr, k, 0:1]
                ai = EPD[:, d, pr, k, 1:2]
                nai = EPD[:, d, pr, k, 2:3]
                if d == 0:
                    dst = slice(s, 544); src = slice(0, 544 - s); keep = slice(0, s)
                else:
                    dst = slice(0, 544 - s); src = slice(s, 544); keep = slice(544 - s, 544)
                for (qo, pi_, sc_, base, bb) in ((0, 0, ar, P, 0), (1, 1, ar, P, 1)):
                    mk.op("dve", lambda e: e.scalar_tensor_tensor(Q[:, qo, dst], P[:, pi_, src], sc_, base[:, bb, dst], ALU.mult, ALU.add),
                          reads=[P, KP[id(P)], EPD, Q], writes=[Q])
                mk.op("act", lambda e: e.activation(Q[:, :, keep], P[:, :, keep], AF.Copy), reads=[P, KP[id(P)], KP[id(Q)]], writes=[KP[id(Q)]])
            for d in range(2):
                P, Q = PQ[d]
                ai = EPD[:, d, pr, k, 1:2]
                nai = EPD[:, d, pr, k, 2:3]
                if d == 0:
                    dst = slice(s, 544); src = slice(0, 544 - s)
                else:
                    dst = slice(0, 544 - s); src = slice(s, 544)
                for (qo, pi_, sc_) in ((0, 1, nai), (1, 0, ai)):
                    mk.op("dve", lambda e: e.scalar_tensor_tensor(Q[:, qo, dst], P[:, pi_, src], sc_, Q[:, qo, dst], ALU.mult, ALU.add),
                          reads=[P, KP[id(P)], EPD, Q], writes=[Q])
                PQ[d] = [Q, P]
        for d in range(2):
            P = PQ[d][0]
            if d == 0:
                mk.op("act", lambda e: e.activation(XS[d][:, :, 1:545], P[:, :, :], AF.Copy), reads=[P, KP[id(P)], XS[d]], writes=[XS[d]])
            else:
                mk.op("act", lambda e: e.activation(XS[d][:, :, 0:544], P[:, :, :], AF.Copy), reads=[P, KP[id(P)], XS[d]], writes=[XS[d]])
        for hf in range(2):
            gg = 2 * pr + hf
            rows = slice(64 * hf, 64 * hf + 64)
            pyy = py[gg % 2]
            for (c0, n, bk, pc) in SEGS:
                out = pyy[:, bk, pc:pc + n]
                mk.mm(out, TOEP[:, gg, :], U8[:, gg, c0:c0 + n], start=True, stop=False, reads=[TOEP, U8], writes=[pyy])
                for ri in range(2):
                    mk.mm(out, CO[rows, 0, pr, ri, :], XS[0][rows, ri, c0:c0 + n], start=False, stop=False,
                          reads=[CO, XS[0]], writes=[pyy])
                j0 = (c0 - 32) if c0 >= 32 else (512 + c0)
                for ri in range(2):
                    mk.mm(out, CO[rows, 1, pr, ri, :], XS[1][rows, ri, j0 + 1:j0 + 1 + n], start=False, stop=(ri == 1),
                          reads=[CO, XS[1]], writes=[pyy])
            yst = ystage[gg % 2]
            mk.op("act", lambda e: e.activation(yst[:, 0:32], pyy[:, 0, 256:288], AF.Copy), reads=[pyy, yst], writes=[yst])
            mk.op("act", lambda e: e.activation(yst[:, 32:544].rearrange("p (b c) -> p b c", b=2), pyy[:, :, 0:256], AF.Copy),
                  reads=[pyy, yst], writes=[yst])
            mk.dma("sp", g.Y8D[gg], yst[:], reads=[yst], writes=[g.Y8D])

    zstage(0)
    for pr in range(8):
        if pr + 1 < 8:
            zstage(pr + 1)
        rest_stage(pr)
    YT = U8
    YTv = U8[:].rearrange("p (m t) c -> p m t c", m=2)
    for gg in range(16):
        mt, gl = gg // 8, gg % 8
        mk.dma("sp" if gg % 2 == 0 else "pool", YTv[16 * gl:16 * gl + 16, mt, :, :],
               g.Y8D[gg].rearrange("(t h) c -> h t c", h=16), reads=[g.Y8D, U8], writes=[U8])
    dcol = mk.sb("dcol", [128, 2], F32)
    bglu = mk.sb("bglu", [128, 2], F32)
    Wglu = mk.sb("Wglu", [128, 2, 256], BF16)
    with nc.allow_non_contiguous_dma(reason="small columns"):
        mk.dma("sp", dcol[:], g.s5_d[layer].rearrange("(m p) -> p m", p=128), reads=[g.s5_d], writes=[dcol])
        mk.dma("sp", bglu[:], g.s5_glu_b[layer].rearrange("(m p) -> p m", p=128), reads=[g.s5_glu_b], writes=[bglu])
    mk.dma("pool", Wglu[:], g.s5_glu_w[layer].rearrange("(m p) n -> p m n", p=128), reads=[g.s5_glu_w], writes=[Wglu])
    y1 = mk.sb("y1", [128, 2, 512], F32)
    sq = mk.sb("sq", [128, 2, 512], F32)
    yg = mk.sb("yg", [128, 2, 512], F32)
    ygb = mk.sb("ygb", [128, 2, 512], BF16)
    sig = mk.sb("sig", [128, 512], F32)
    yos = [mk.sb("yo%d" % i, [128, 2, 512], BF16) for i in range(2)]
    for blk in range(nblk):
        N = 512 if blk < 8 else 256
        t0 = blk * 512
        c0 = blk * 64
        ncn = N // 8
        yo = yos[blk % 2]
        for mt in range(2):
            uv = UR[:, mt, :, c0:c0 + ncn].rearrange("p s c -> p c s")
            yv = YTv[:, mt, :, c0:c0 + ncn].rearrange("p t c -> p c t")
            mk.op("dve", lambda e: e.scalar_tensor_tensor(y1[:, mt, 0:N].rearrange("p (c s) -> p c s", s=8), uv,
                                                           dcol[:, mt:mt + 1], yv, ALU.mult, ALU.add),
                  reads=[UR, U8, dcol, y1], writes=[y1])
        a = y1[:, :, 0:N]
        mk.op("pool", lambda e: e.tensor_tensor(sq[:, :, 0:N], a, a, ALU.mult), reads=[y1], writes=[sq])
        mk.op("pool", lambda e: e.tensor_scalar(sq[:, :, 0:N], sq[:, :, 0:N], 0.044715, 1.0, ALU.mult, ALU.add), reads=[sq], writes=[sq])
        mk.op("dve", lambda e: e.tensor_tensor(sq[:, :, 0:N], sq[:, :, 0:N], a, ALU.mult), reads=[sq, y1], writes=[sq])
        mk.op("act", lambda e: e.activation(sq[:, :, 0:N], sq[:, :, 0:N], AF.Sigmoid, scale=1.5957691216057308), reads=[sq], writes=[sq])
        mk.op("dve", lambda e: e.tensor_tensor(yg[:, :, 0:N], sq[:, :, 0:N], a, ALU.mult), reads=[sq, y1], writes=[yg])
        mk.op("pool", lambda e: e.tensor_copy(ygb[:, :, 0:N], yg[:, :, 0:N]), reads=[yg], writes=[ygb])
        for mo in range(2):
            pp = pz[mo]
            for mi in range(2):
                mk.mm(pp[:, 0, 0:N], Wglu[:, mi, mo * 128:(mo + 1) * 128], ygb[:, mi, 0:N], start=(mi == 0), stop=(mi == 1),
                      reads=[Wglu, ygb], writes=[pp])
            mk.op("act", lambda e: e.activation(sig[:, 0:N], pp[:, 0, 0:N], AF.Sigmoid, bias=bglu[:, mo:mo + 1]),
                  reads=[pp, bglu], writes=[sig])
            mk.op("dve", lambda e: e.tensor_tensor(yo[:, mo, 0:N], yg[:, mo, 0:N], sig[:, 0:N], ALU.mult),
                  reads=[yg, sig, yo], writes=[yo])
        mk.dma("sp", g.YS5T[:, :, t0:t0 + N].rearrange("m p t -> p m t"), yo[:, :, 0:N], reads=[yo], writes=[g.YS5T])


def run_gens(gens):
    alive = [True] * len(gens)
    while any(alive):
        for i_, gi in enumerate(gens):
            if alive[i_]:
                try:
                    next(gi)
                except StopIteration:
                    alive[i_] = False


def phase_s5(mk, g, layer):
    mk.begin_phase()
    WZ, CO, TOEP, EPD = s5_tables(mk)
    pz = [mk.ps("s5pz%d" % i, [128, 2, 512], F32) for i in range(2)]
    py = [mk.ps("s5py%d" % i, [128, 2, 512], F32) for i in range(2)]
    push_scope(mk)
    run_gens([s5_setup_gen(mk, g, layer, WZ, CO, TOEP, EPD, pz, py)])
    pop_scope(mk)
    push_scope(mk)
    s5_main(mk, g, layer, WZ, CO, TOEP, EPD, pz, py)
    pop_scope(mk)
    mk.end_phase()


def norm_chain(mk, g, scan):
    sfx = "_s" if scan else "_n"
    dst = g.UTH if scan else g.UT
    hts = [mk.sb("h_t%d" % i + sfx, [128, D], F32) for i in range(2)]
    xns = [mk.sb("xn%d" % i + sfx, [128, D], BF16) for i in range(2)]
    uts = [mk.sb("ut%d" % i + sfx, [128, 8, 128], BF16) for i in range(2)]
    pTs = [mk.ps("pT%d" % i + sfx, [128, 8, 128], BF16) for i in range(2)]
    junk = mk.sb("junk" + sfx, [128, D], BF16)
    utf = mk.sb("utf" + sfx, [128, 8, 128], F32)
    sss = [mk.sb("ss%d" % i + sfx, [128, 1], F32) for i in range(2)]
    rstds = [mk.sb("rstd%d" % i + sfx, [128, 1], F32) for i in range(2)]
    q1, q2 = ("sp", "pool")
    for i in range(NT):
        b = i % 2
        wv = 1 if i < 2 else 0
        if not scan:
            mk.dma(q1, hts[b][:], g.H[i * 128:(i + 1) * 128, :], reads=[g.H], writes=[hts[b]])
        else:
            po = 0
            for (r0, rs, n) in scan_rows(i):
                src = g.H[r0:r0 + n, :] if rs == 1 else g.H[r0:r0 + (n - 1) * rs + 1:rs, :]
                mk.dma(q1, hts[b][po:po + n, :], src, reads=[g.H], writes=[hts[b]])
                po += n
        norm_tile(mk, g, hts[b], xns[b], sss[b], rstds[b], junk)
        yield
        transpose_mod_tile(mk, g, xns[b], pTs[b], uts[b], g.A1, utf, wv, 0)
        mk.dma(q2, dst[:, :, i * 128:(i + 1) * 128].rearrange("k p t -> p k t"), uts[b][:],
               reads=[uts[b]], writes=[dst])
        yield


def phase_norm_s5(mk, g, layer):
    mk.begin_phase()
    WZ, CO, TOEP, EPD = s5_tables(mk)
    push_scope(mk)
    pzs = mk.ps("s5pzs", [128, 2, 512], F32)
    pys = mk.ps("s5pys", [128, 2, 512], F32)
    run_gens([norm_chain(mk, g, False), norm_chain(mk, g, True),
              s5_setup_gen(mk, g, layer, WZ, CO, TOEP, EPD, [pzs, pzs], [pys, pys])])
    pop_scope(mk)
    push_scope(mk)
    pz = [mk.ps("s5pz%d" % i, [128, 2, 512], F32) for i in range(2)]
    py = [mk.ps("s5py%d" % i, [128, 2, 512], F32) for i in range(2)]
    s5_main(mk, g, layer, WZ, CO, TOEP, EPD, pz, py)
    pop_scope(mk)
    mk.end_phase()


SCRATCH.update({
    "UTH": ([8, 128, T], BF16),
    "OFG": ([4, 128, T], F32),
    "OFH": ([2, 128, T], F32),
    "YGLAT": ([4, 128, T], BF16),
    "YHG": ([T, 256], BF16),
})


def scan_rows(tile_idx):
    if tile_idx < 2:
        return [(tile_idx * 128, 1, 128)]
    c0 = (tile_idx - 2) * 2
    return [(NCTX + c0, 64, 64), (NCTX + c0 + 1, 64, 64)]


def phase_norm1_scan(mk, g, layer):
    mk.begin_phase()
    hts = [mk.sb("h_t%d" % i, [128, D], F32) for i in range(2)]
    xns = [mk.sb("xn%d" % i, [128, D], BF16) for i in range(2)]
    uts = [mk.sb("ut%d" % i, [128, 8, 128], BF16) for i in range(2)]
    pTs = [mk.ps("pT%d" % i, [128, 8, 128], BF16) for i in range(2)]
    junk = mk.sb("junk", [128, D], BF16)
    sss = [mk.sb("ss%d" % i, [128, 1], F32) for i in range(2)]
    rstds = [mk.sb("rstd%d" % i, [128, 1], F32) for i in range(2)]
    for i in range(NT):
        b = i % 2
        wv = 1 if i < 2 else 0
        po = 0
        for (r0, rs, n) in scan_rows(i):
            if rs == 1:
                src = g.H[r0:r0 + n, :]
            else:
                src = g.H[r0:r0 + (n - 1) * rs + 1:rs, :]
            mk.dma("sp", hts[b][po:po + n, :], src, reads=[g.H], writes=[hts[b]])
            po += n
        norm_tile(mk, g, hts[b], xns[b], sss[b], rstds[b], junk)
        transpose_mod_tile(mk, g, xns[b], pTs[b], uts[b], g.A1, None, wv, 0)
        mk.dma("act", g.UTH[:, :, i * 128:(i + 1) * 128].rearrange("k p t -> p k t"), uts[b][:],
               reads=[uts[b]], writes=[g.UTH])
    mk.end_phase()


def phase_linattn(mk, g, layer, kind):
    nc = mk.nc
    gla = (kind == "gla")
    DV = 128 if gla else 64
    VW = 4 * DV
    esc = (-1.0 / 16.0) if gla else 1.0
    qscale = 0.125 if gla else 1.0
    UTsrc = g.UT if gla else g.UTH
    OF = g.OFG if gla else g.OFH
    NPO = 4 if gla else 2
    cq, ck, cv = (C_GQ, C_GK, C_GV) if gla else (C_HQ, None, C_HI)
    mk.begin_phase()
    ones32 = mk.sb("ones32", [128, 32], F32)
    mk.op("pool", lambda e: e.memset(ones32[:], 1.0), writes=[ones32])
    masks = []
    for d in range(2):
        m = mk.sb("mask%d" % d, [128, 128], F32)
        mk.op("pool", lambda e: e.memset(m[:], 1.0), writes=[m])
        if d == 0:
            mk.op("pool", lambda e: e.affine_select(out=m[:], in_=m[:], pattern=[[1, 128]], compare_op=ALU.is_ge,
                                                     fill=0.0, base=0, channel_multiplier=-1), reads=[m], writes=[m])
        else:
            mk.op("pool", lambda e: e.affine_select(out=m[:], in_=m[:], pattern=[[-1, 128]], compare_op=ALU.is_ge,
                                                     fill=0.0, base=0, channel_multiplier=1), reads=[m], writes=[m])
        for c in range(4):
            cs = slice(32 * c, 32 * c + 32)
            if d == 0:
                mk.op("pool", lambda e: e.affine_select(out=m[:, cs], in_=m[:, cs], pattern=[[0, 32]], compare_op=ALU.is_ge,
                                                         fill=0.0, base=-32 * c, channel_multiplier=1), reads=[m], writes=[m])
            else:
                mk.op("pool", lambda e: e.affine_select(out=m[:, cs], in_=m[:, cs], pattern=[[0, 32]], compare_op=ALU.is_ge,
                                                         fill=0.0, base=32 * c + 31, channel_multiplier=-1), reads=[m], writes=[m])
        masks.append(m)
    rowmask = mk.sb("rowmask", [128, 4], F32)
    mk.op("pool", lambda e: e.memset(rowmask[:], 1.0), writes=[rowmask])
    for c in range(4):
        mk.op("pool", lambda e: e.affine_select(out=rowmask[:, c:c + 1], in_=rowmask[:, c:c + 1], pattern=[[0, 1]],
                                                 compare_op=ALU.is_ge, fill=0.0, base=-32 * c, channel_multiplier=1),
              reads=[rowmask], writes=[rowmask])
        mk.op("pool", lambda e: e.affine_select(out=rowmask[:, c:c + 1], in_=rowmask[:, c:c + 1], pattern=[[0, 1]],
                                                 compare_op=ALU.is_ge, fill=0.0, base=32 * c + 31, channel_multiplier=-1),
              reads=[rowmask], writes=[rowmask])
    def loadw(name, c0, n):
        w = mk.sb(name, [128, 8, n], BF16)
        mk.dma("pool", w[:], g.w_in[layer, :, c0:c0 + n].rearrange("(kc p) n -> p kc n", p=128), reads=[g.w_in], writes=[w])
        return w
    Wq = loadw("Wq", cq, 256)
    Wv = loadw("Wv", cv, VW)
    if gla:
        Wk = loadw("Wk", ck, 256)
        Wr = loadw("Wr", C_GR, 512)
        Wdec = [loadw("Wc%d" % d, C_GC + 16 * d, 16) for d in range(2)]
        GU = [mk.sb("GU%d" % d, [16, 256], F32) for d in range(2)]
        nbcol = mk.sb("nbcol", [128, 2, 2], F32)
        gn = mk.sb("gncol", [128, 1], F32)
        with nc.allow_non_contiguous_dma(reason="small"):
            for d in range(2):
                mk.dma("sp", GU[d][:], g.gla_gate_up[layer, d], reads=[g.gla_gate_up], writes=[GU[d]])
                mk.dma("sp", nbcol[:, d, :], g.gla_gate_b[layer, d].rearrange("(m p) -> p m", p=128), reads=[g.gla_gate_b], writes=[nbcol])
            mk.dma("sp", gn[:], g.gla_norm_g[layer].rearrange("(p o) -> p o", o=1), reads=[g.gla_norm_g], writes=[gn])
        mk.op("dve", lambda e: e.tensor_scalar(nbcol[:], nbcol[:], -1.0, None, ALU.mult), reads=[nbcol], writes=[nbcol])
        ones_bf = mk.sb("ones_bf", [128, 128], BF16)
        mk.op("pool", lambda e: e.memset(ones_bf[:], 1.0), writes=[ones_bf])
    else:
        Wdec = [loadw("Wf%d" % d, C_HF + 256 * d, 256) for d in range(2)]
        Wog = loadw("Wog", C_HO, 256)
        lbc = mk.sb("lbc", [128, 2], F32)
        omlb = mk.sb("omlb", [128, 2], F32)
        l0 = mk.sb("l0", [128, 2], F32)
        gnrow = mk.sb("gnrow", [128, 4, 64], F32)
        with nc.allow_non_contiguous_dma(reason="small"):
            mk.dma("sp", lbc[:], g.hgrn_lower[1].rearrange("(m p) -> p m", p=128), reads=[g.hgrn_lower], writes=[lbc])
            mk.dma("sp", l0[:], g.hgrn_lower[0].rearrange("(m p) -> p m", p=128), reads=[g.hgrn_lower], writes=[l0])
            for h in range(4):
                mk.dma("sp", gnrow[:, h, :], g.hgrn_norm_g[layer].partition_broadcast(128), reads=[g.hgrn_norm_g], writes=[gnrow])
        if layer == 0:
            mk.op("dve", lambda e: e.memset(lbc[:], 0.0), reads=[lbc], writes=[lbc])
        else:
            mk.op("dve", lambda e: e.tensor_tensor(lbc[:], lbc[:], l0[:], ALU.subtract), reads=[lbc, l0], writes=[lbc])
            mk.op("act", lambda e: e.activation(lbc[:], lbc[:], AF.Sigmoid), reads=[lbc], writes=[lbc])
        mk.op("dve", lambda e: e.tensor_scalar(omlb[:], lbc[:], -1.0, 1.0, ALU.mult, ALU.add), reads=[lbc], writes=[omlb])

    pproj = [mk.ps("pproj%d" % i, [128, 512], F32) for i in range(2)]
    psc = mk.ps("psc", [128, 2, 512], F32)
    po = mk.ps("po", [128, 2, 512], F32)
    pds = mk.ps("pds", [128, 2, 256], F32)
    ptr = mk.ps("ptr", [128, 1024], BF16)
    pss = Buf(pds.t, "pss", psum=True)
    pss = pds
    pssv = pds[:].rearrange("p a b -> p (a b)")
    npj = [0]

    def nextp():
        npj[0] += 1
        return pproj[npj[0] % 2]

    ubs = [mk.sb("ub%d" % i, [128, 8, 512], BF16) for i in range(2)]
    qT = mk.sb("qT", [128, 2, 512], F32)
    kT = mk.sb("kT", [128, 2, 512], F32)
    LG = mk.sb("LG", [128, 2, 512], F32)
    vtok = mk.sb("vtok", [128, 4, VW], BF16)
    if gla:
        codeT = mk.sb("codeT", [16, 512], F32)
        rsil = mk.sb("rsil", [128, 4, 512], F32)
    else:
        sg = mk.sb("sg", [128, 2, 512], F32)
        ogs = mk.sb("ogs", [128, 4, 256], F32)
    cum = mk.sb("cum", [128, 2, 128], F32)
    eq = mk.sb("eq", [128, 2, 128], F32)
    ek = mk.sb("ek", [128, 2, 128], F32)
    elast = mk.sb("elast", [128, 2, 4], F32)
    qtil = mk.sb("qtil", [128, 2, 128], BF16)
    ktil = mk.sb("ktil", [128, 2, 128], F32)
    ktilb = mk.sb("ktilb", [128, 2, 128], BF16)
    khT = mk.sb("khT", [128, 2, 128], BF16)
    khat4 = mk.sb("khat4", [128, 4, 256], BF16)
    PT = mk.sb("PT", [128, 4, 128], BF16)
    S = [mk.sb("S%d" % i, [128, DV], F32) for i in range(2)]
    Sb = [mk.sb("Sb%d" % i, [128, DV], BF16) for i in range(2)]
    ost = [mk.sb("ost%d" % i, [128, NPO, 128], F32) for i in range(2)]
    ofl = [mk.sb("ofl%d" % i, [128, NPO, 128], F32) for i in range(2)]
    osum = mk.sb("osum", [128, NPO, 128], F32)
    if gla:
        osq = mk.sb("osq", [128, 4, 128], BF16)
        rstd = mk.sb("rstdg", [128, 512], F32)
        yst = [mk.sb("ystg%d" % i, [128, 4, 128], BF16) for i in range(2)]
    else:
        otok = mk.sb("otok", [128, 256], F32)
        junkh = mk.sb("junkh", [128, 64], F32)
        ssh = mk.sb("ssh", [128, 4], F32)
        rsth = mk.sb("rsth", [128, 4], F32)
        ptf = pds
        ptfv = pssv
        ysth = [mk.sb("ysth%d" % i, [128, 256], BF16) for i in range(2)]
    ntile = [0]

    def po_v():
        if gla:
            return po[:, :, 0:256].rearrange("p r (m t) -> p r m t", m=2)
        return None

    def hv(buf):
        return buf[:].rearrange("p (m r) t -> p r m t", r=2)

    def ost_v(par):
        return hv(ost[par]) if gla else None

    def ofl_v(par):
        return hv(ofl[par]) if gla else None

    def osum_v():
        return hv(osum) if gla else None


    blocks = [(0, 2)] + [(2 + 4 * b, 4) for b in range(8)]
    for d in range(2):
        last_pass = (d == 1)
        for mt in range(2):
            mk.op("pool", lambda e: e.memset(S[mt][:], 0.0), reads=[S[mt]], writes=[S[mt]])
            mk.op("pool", lambda e: e.memset(Sb[mt][:], 0.0), reads=[Sb[mt]], writes=[Sb[mt]])
        order = blocks if d == 0 else [blocks[0]] + blocks[:0:-1]
        for bi, (tile0, ntl) in enumerate(order):
            N = ntl * 128
            t0 = tile0 * 128
            ub = ubs[bi % 2]
            mk.dma("sp", ub[:, :, 0:N], UTsrc[:, :, t0:t0 + N].rearrange("k p t -> p k t"), reads=[UTsrc], writes=[ub])

            def proj(W, c0, m, evac):
                pp = nextp()
                for kc in range(8):
                    mk.mm(pp[0:m, 0:N], W[:, kc, c0:c0 + m], ub[:, kc, 0:N], start=(kc == 0), stop=(kc == 7),
                          reads=[W, ub], writes=[pp])
                evac(pp)

            for mt in range(2):
                if gla:
                    proj(Wq, mt * 128, 128, lambda pp: mk.op("act", lambda e: e.activation(qT[:, mt, 0:N], pp[:, 0:N], AF.Copy),
                                                              reads=[pp, qT], writes=[qT]))
                else:
                    proj(Wq, mt * 128, 128, lambda pp: mk.op("act", lambda e: e.activation(qT[:, mt, 0:N], pp[:, 0:N], AF.Silu),
                                                              reads=[pp, qT], writes=[qT]))
            if gla:
                for mt in range(2):
                    proj(Wk, mt * 128, 128, lambda pp: mk.op("dve", lambda e: e.tensor_copy(kT[:, mt, 0:N], pp[:, 0:N]),
                                                              reads=[pp, kT], writes=[kT]))
                proj(Wdec[d], 0, 16, lambda pp: mk.op("act", lambda e: e.activation(codeT[:, 0:N], pp[0:16, 0:N], AF.Copy),
                                                       reads=[pp], writes=[codeT]))
                for mt in range(2):
                    pp = nextp()
                    mk.mm(pp[:, 0:N], GU[d][:, mt * 128:(mt + 1) * 128], codeT[:, 0:N], start=True, stop=True,
                          reads=[GU[d], codeT], writes=[pp])
                    mk.op("act", lambda e: e.activation(LG[:, mt, 0:N], pp[:, 0:N], AF.Exp, scale=-1.0, bias=nbcol[:, d, mt:mt + 1]),
                          reads=[pp, nbcol, LG], writes=[LG])
                    mk.op("act", lambda e: e.activation(LG[:, mt, 0:N], LG[:, mt, 0:N], AF.Ln, bias=g.one_col[:, 0:1]),
                          reads=[LG, g.one_col], writes=[LG])
            else:
                for mt in range(2):
                    def ev(pp):
                        mk.op("act", lambda e: e.activation(sg[:, mt, 0:N], pp[:, 0:N], AF.Sigmoid), reads=[pp, sg], writes=[sg])
                        mk.op("act", lambda e: e.activation(LG[:, mt, 0:N], sg[:, mt, 0:N], AF.Ln, scale=omlb[:, mt:mt + 1],
                                                             bias=lbc[:, mt:mt + 1]), reads=[sg, omlb, lbc, LG], writes=[LG])
                        mk.op("dve", lambda e: e.tensor_scalar(kT[:, mt, 0:N], sg[:, mt, 0:N], -1.0, 1.0, ALU.mult, ALU.add),
                              reads=[sg, kT], writes=[kT])
                        mk.op("dve", lambda e: e.tensor_scalar(kT[:, mt, 0:N], kT[:, mt, 0:N], omlb[:, mt:mt + 1], None, ALU.mult),
                              reads=[kT, omlb], writes=[kT])
                    proj(Wdec[d], mt * 128, 128, ev)
            for tl in range(ntl):
                pp = nextp()
                for kc in range(8):
                    mk.mm(pp[:, 0:VW], ub[:, kc, tl * 128:(tl + 1) * 128], Wv[:, kc, :], start=(kc == 0), stop=(kc == 7),
                          reads=[Wv, ub], writes=[pp])
                mk.op("dve", lambda e: e.tensor_copy(vtok[:, tl, :], pp[:, 0:VW]), reads=[pp, vtok], writes=[vtok])
                if last_pass and not gla:
                    pp = nextp()
                    for kc in range(8):
                        mk.mm(pp[:, 0:256], ub[:, kc, tl * 128:(tl + 1) * 128], Wog[:, kc, :], start=(kc == 0), stop=(kc == 7),
                              reads=[Wog, ub], writes=[pp])
                    mk.op("act", lambda e: e.activation(ogs[:, tl, :], pp[:, 0:256], AF.Silu), reads=[pp, ogs], writes=[ogs])
            if last_pass and gla:
                for h in range(4):
                    proj(Wr, h * 128, 128, lambda pp: mk.op("act", lambda e: e.activation(rsil[:, h, 0:N], pp[:, 0:N], AF.Silu),
                                                             reads=[pp, rsil], writes=[rsil]))
            tls = list(range(ntl)) if d == 0 else list(range(ntl - 1, -1, -1))
            for tl in tls:
                ti = tile0 + tl
                tsl = slice(tl * 128, (tl + 1) * 128)
                ntile[0] += 1
                par = ntile[0] % 2
                if last_pass:
                    mk.dma("act", ofl[par][:], OF[:, :, ti * 128:(ti + 1) * 128].rearrange("m p t -> p m t"),
                           reads=[OF], writes=[ofl[par]])
                for mt in range(2):
                    for c in range(4):
                        lo, hi = 32 * c, 32 * c + 32
                        if d == 0:
                            osl = slice(lo, hi)
                            isl = slice(tl * 128 + lo, tl * 128 + hi)
                        else:
                            osl = slice(hi - 1, lo - 1 if lo > 0 else None, -1)
                            ilo, ihi = tl * 128 + lo, tl * 128 + hi
                            isl = slice(ihi - 1, ilo - 1 if ilo > 0 else None, -1)
                        mk.op("dve", lambda e: e.tensor_tensor_scan(cum[:, mt, osl], ones32[:, :], LG[:, mt, isl], 0.0,
                                                                     ALU.mult, ALU.add), reads=[LG, ones32, cum], writes=[cum])
                lidx = slice(31, 128, 32) if d == 0 else slice(0, 128, 32)
                mk.op("act", lambda e: e.activation(eq[:], cum[:], AF.Exp, scale=esc), reads=[cum], writes=[eq])
                mk.op("act", lambda e: e.activation(ek[:], cum[:], AF.Exp, scale=-esc), reads=[cum], writes=[ek])
                mk.op("act", lambda e: e.activation(elast[:], cum[:, :, lidx], AF.Exp, scale=esc), reads=[cum], writes=[elast])
                mk.op("dve", lambda e: e.scalar_tensor_tensor(qtil[:], qT[:, :, tsl], qscale, eq[:], ALU.mult, ALU.mult),
                      reads=[qT, eq], writes=[qtil])
                mk.op("dve", lambda e: e.tensor_tensor(ktil[:], kT[:, :, tsl], ek[:], ALU.mult), reads=[kT, ek], writes=[ktil])
                mk.op("pool", lambda e: e.tensor_copy(ktilb[:], ktil[:]), reads=[ktil], writes=[ktilb])
                mk.op("dve", lambda e: e.tensor_tensor(khT[:].rearrange("p m (c j) -> p m c j", j=32),
                                                        ktil[:].rearrange("p m (c j) -> p m c j", j=32),
                                                        elast[:].unsqueeze(3).broadcast_to([128, 2, 4, 32]), ALU.mult),
                      reads=[ktil, elast], writes=[khT])
                for mt in range(2):
                    mk.tr(ptr[:, mt * 128:(mt + 1) * 128], khT[:, mt, :], g.identb[:], reads=[khT, g.identb], writes=[ptr])
                for c in range(4):
                    if c % 2 == 0:
                        mk.op("act", lambda e: e.activation(khat4[:, c, :], ptr[:, 0:256], AF.Identity, scale=rowmask[:, c:c + 1]),
                              reads=[ptr, rowmask, khat4], writes=[khat4])
                    else:
                        mk.op("dve", lambda e: e.tensor_scalar(khat4[:, c, :], ptr[:, 0:256], rowmask[:, c:c + 1], None, ALU.mult),
                              reads=[ptr, rowmask, khat4], writes=[khat4])
                for h in range(4):
                    mt, rows = h // 2, slice(64 * (h % 2), 64 * (h % 2) + 64)
                    mk.mm(psc[:, h % 2, mt * 128:(mt + 1) * 128], ktilb[rows, mt, :], qtil[rows, mt, :], start=True, stop=True,
                          reads=[ktilb, qtil], writes=[psc])
                mk.op("dve", lambda e: e.tensor_tensor(PT[:].rearrange("p (m r) t -> p r m t", r=2),
                                                        psc[:, :, 0:256].rearrange("p r (m t) -> p r m t", m=2),
                                                        masks[d][:].unsqueeze(1).unsqueeze(1).broadcast_to([128, 2, 2, 128]), ALU.mult),
                      reads=[psc, masks[d]], writes=[PT])

                def po_ap(h, cols):
                    c0_ = (h // 2) * 128
                    csl = slice(c0_ + cols.start, c0_ + cols.stop)
                    if gla:
                        return po[:, h % 2, csl]
                    return po[64 * (h % 2):64 * (h % 2) + 64, h % 2, csl]

                chunks = list(range(4)) if d == 0 else [3, 2, 1, 0]
                for ci, c in enumerate(chunks):
                    cs = slice(32 * c, 32 * c + 32)
                    for h in range(4):
                        mt, rows = h // 2, slice(64 * (h % 2), 64 * (h % 2) + 64)
                        mk.mm(po_ap(h, cs), vtok[:, tl, h * DV:(h + 1) * DV], PT[:, h, cs], start=True, stop=False,
                              reads=[vtok, PT], writes=[po])
                        mk.mm(po_ap(h, cs), Sb[mt][rows, :], qtil[rows, mt, cs], start=False, stop=True,
                              reads=[Sb[mt], qtil], writes=[po])
                    for h in range(4):
                        mt, rows = h // 2, slice(64 * (h % 2), 64 * (h % 2) + 64)
                        mk.mm(pds[rows, mt, 0:DV], khat4[:, c, h * 64:(h + 1) * 64], vtok[:, tl, h * DV:(h + 1) * DV], start=True, stop=True,
                              reads=[khat4, vtok], writes=[pds])
                    for mt in range(2):
                        mk.op("dve", lambda e: e.scalar_tensor_tensor(S[mt][:], S[mt][:], elast[:, mt, c:c + 1], pds[:, mt, 0:DV],
                                                                       ALU.mult, ALU.add), reads=[S[mt], elast, pds], writes=[S[mt]])
                        mk.op("pool" if mt == 0 else "act",
                              (lambda e: e.tensor_copy(Sb[mt][:], S[mt][:])) if mt == 0 else
                              (lambda e: e.activation(Sb[mt][:], S[mt][:], AF.Copy)), reads=[S[mt]], writes=[Sb[mt]])
                if not last_pass:
                    if gla:
                        mk.op("act", lambda e: e.activation(ost_v(par), po_v(), AF.Copy), reads=[po], writes=[ost[par]])
                    else:
                        for r in range(2):
                            rr = slice(64 * r, 64 * r + 64)
                            mk.op("act", lambda e: e.activation(ost[par][rr, :, :], po[rr, r, 0:256].rearrange("p (m t) -> p m t", m=2), AF.Copy),
                                  reads=[po, ost[par]], writes=[ost[par]])
                    mk.dma("sp", OF[:, :, ti * 128:(ti + 1) * 128].rearrange("m p t -> p m t"), ost[par][:],
                           reads=[ost[par]], writes=[OF])
                    continue
                if gla:
                    mk.op("dve", lambda e: e.tensor_tensor(osum_v(), po_v(), ofl_v(par), ALU.add), reads=[po, ofl[par]], writes=[osum])
                else:
                    for r in range(2):
                        rr = slice(64 * r, 64 * r + 64)
                        mk.op("dve", lambda e: e.tensor_tensor(osum[rr, :, :], po[rr, r, 0:256].rearrange("p (m t) -> p m t", m=2),
                                                                ofl[par][rr, :, :], ALU.add), reads=[po, ofl[par], osum], writes=[osum])
                if gla:
                    mk.op("act", lambda e: e.activation(osq[:], osum[:], AF.Square), reads=[osum], writes=[osq])
                    mk.mm(pssv, ones_bf[:], osq[:].rearrange("p h t -> p (h t)"), start=True, stop=True,
                          reads=[ones_bf, osq], writes=[pss])
                    mk.op("act", lambda e: e.activation(rstd[:], pssv, AF.Sqrt, scale=1.0 / 128.0, bias=g.eps_col[:, 0:1]),
                          reads=[pss, g.eps_col], writes=[rstd])
                    mk.op("dve", lambda e: e.reciprocal(rstd[:], rstd[:]), reads=[rstd], writes=[rstd])
                    mk.op("dve", lambda e: e.scalar_tensor_tensor(osum[:], osum[:], gn[:, 0:1], rstd[:].rearrange("p (h t) -> p h t", h=4),
                                                                   ALU.mult, ALU.mult), reads=[osum, gn, rstd], writes=[osum])
                    mk.op("dve", lambda e: e.tensor_tensor(yst[par][:], osum[:], rsil[:, :, tsl], ALU.mult),
                          reads=[osum, rsil], writes=[yst[par]])
                    mk.dma("sp", g.YGLAT[:, :, ti * 128:(ti + 1) * 128].rearrange("m p t -> p m t"), yst[par][:],
                           reads=[yst[par]], writes=[g.YGLAT])
                else:
                    for mt in range(2):
                        mk.tr(ptfv[:, mt * 128:(mt + 1) * 128], osum[:, mt, :], g.identf[:], reads=[osum, g.identf], writes=[ptf])
                    mk.op("act", lambda e: e.activation(otok[:], ptfv[:, 0:256], AF.Copy), reads=[ptf], writes=[otok])
                    for h in range(4):
                        mk.op("act", lambda e: e.activation(junkh[:], otok[:, h * 64:(h + 1) * 64], AF.Square, accum_out=ssh[:, h:h + 1]),
                              reads=[otok, junkh, ssh], writes=[junkh, ssh])
                    mk.op("act", lambda e: e.activation(rsth[:], ssh[:], AF.Sqrt, scale=1.0 / 64.0, bias=g.eps_col[:, 0:1]),
                          reads=[ssh, g.eps_col], writes=[rsth])
                    mk.op("dve", lambda e: e.reciprocal(rsth[:], rsth[:]), reads=[rsth], writes=[rsth])
                    ov = otok[:].rearrange("p (h v) -> p h v", h=4)
                    mk.op("dve", lambda e: e.tensor_tensor(ov, ov, rsth[:].unsqueeze(2).broadcast_to([128, 4, 64]), ALU.mult),
                          reads=[otok, rsth], writes=[otok])
                    mk.op("dve", lambda e: e.tensor_tensor(ov, ov, gnrow[:], ALU.mult), reads=[otok, gnrow], writes=[otok])
                    mk.op("dve", lambda e: e.tensor_tensor(ysth[par][:], otok[:], ogs[:, tl, :], ALU.mult),
                          reads=[otok, ogs], writes=[ysth[par]])
                    pofs = 0
                    for (r0, rs, n) in scan_rows(ti):
                        dst = g.YHG[r0:r0 + n, :] if rs == 1 else g.YHG[r0:r0 + (n - 1) * rs + 1:rs, :]
                        mk.dma("sp", dst, ysth[par][pofs:pofs + n, :], reads=[ysth[par]], writes=[g.YHG])
                        pofs += n
    mk.end_phase()


SCRATCH.update({
    "VT": ([8, 128, T], BF16),
    "GATESD": ([128, NT, 16], F32),
})

BIG = 1.0e30


def phase_merge(mk, g, layer):
    nc = mk.nc
    last = (layer == 1)
    if not hasattr(g, "gates"):
        g.gates = mk.sb("gates", [128, NT, 16], F32)
    mk.begin_phase()

    def loadw(name, src_ap, kc, n):
        w = mk.sb(name, [128, kc, n], BF16)
        mk.dma("pool", w[:], src_ap.rearrange("(kc p) n -> p kc n", p=128), reads=[g.w_in], writes=[w])
        return w
    Wg = mk.sb("Wg", [128, 8, 3072], BF16)
    for br in range(3):
        mk.dma("pool", Wg[:, :, br * 1024:(br + 1) * 1024],
               g.w_in[layer, :, C_GATE + br * 1024:C_GATE + (br + 1) * 1024].rearrange("(kc p) n -> p kc n", p=128),
               reads=[g.w_in], writes=[Wg])
    Wb = [loadw("Wb0", g.w_branch_s5[layer], 2, 1024), loadw("Wb1", g.w_branch_gla[layer], 4, 1024),
          loadw("Wb2", g.w_branch_hgrn[layer], 2, 1024)]
    Wout = loadw("Wout", g.w_out[layer], 8, 1024)
    RW = mk.sb("RW", [128, 8, 16], F32)
    rb = mk.sb("rb", [128, 16], F32)
    G2row = mk.sb("G2row", [128, 1024], F32)
    with nc.allow_non_contiguous_dma(reason="small"):
        mk.dma("sp", RW[:], g.router_w[:, :].rearrange("(kc p) e -> p kc e", p=128), reads=[g.router_w], writes=[RW])
        mk.dma("sp", rb[:], g.router_b[:].partition_broadcast(128), reads=[g.router_b], writes=[rb])
    rot = [mk.ps("rot%d" % i, [128, 512], F32) for i in range(2)]
    pmix = mk.ps("pmix", [128, 2, 512], F32)
    pT32 = mk.ps("pT32", [128, 8, 128], F32)
    plg = mk.ps("plg", [128, 512], F32)
    ptb = mk.ps("ptb", [128, 1024], BF16)
    nrot = [0]

    def nextrot():
        nrot[0] += 1
        return rot[nrot[0] % 2]

    ubs = [mk.sb("ub%d" % i, [128, 8, 512], BF16) for i in range(2)]
    ys5 = mk.sb("ys5", [128, 2, 512], BF16)
    ygl = mk.sb("ygl", [128, 4, 512], BF16)
    yhtok = mk.sb("yhtok", [128, 4, 256], BF16)
    yhT = mk.sb("yhT", [128, 2, 512], BF16)
    sig = [mk.sb("sig%d" % i, [128, 512], F32) for i in range(2)]
    acc = mk.sb("acc", [128, 512], F32)
    tmpm = mk.sb("tmpm", [128, 512], F32)
    mergedT = mk.sb("mergedT", [128, 8, 512], BF16)
    hts = [mk.sb("h_t%d" % i, [128, D], F32) for i in range(2)]
    tmph = mk.sb("tmph", [128, D], F32)
    xnf = mk.sb("xnf", [128, D], F32)
    junk = mk.sb("junk", [128, D], BF16)
    ss = mk.sb("ss", [128, 1], F32)
    rstd = mk.sb("rstd", [128, 1], F32)
    vts = [mk.sb("vt%d" % i, [128, 8, 128], BF16) for i in range(2)]
    vT32 = mk.sb("vT32", [128, 8, 128], F32)
    LGT = mk.sb("LGT", [128, NT, 16], F32)
    R = {k: mk.sb("r_" + k, [128, 16], F32) for k in ("ex", "pr", "sel", "msk", "m1", "m2", "w")}
    r4 = {k: mk.sb("r4_" + k, [128, 4], F32) for k in ("a", "gs", "inb", "t2")}
    r1 = {k: mk.sb("r1_" + k, [128, 1], F32) for k in ("mx", "sum", "best", "top1", "top2", "ws")}

    blocks = [(0, 2)] + [(2 + 4 * b, 4) for b in range(8)]
    if last:
        blocks = blocks[1:]
    cur_w = [None]
    nt_ = [0]
    mergedTs = [mergedT, mk.sb("mergedT2", [128, 8, 512], BF16)]

    def dtloop(bi, tile0, ntl):
            N = ntl * 128
            t0 = tile0 * 128
            wv = 1 if tile0 < 2 else 0
            ub = ubs[bi % 2]
            mk.dma("sp", ub[:, :, 0:N], g.UT[:, :, t0:t0 + N].rearrange("k p t -> p k t"), reads=[g.UT], writes=[ub])
            mk.dma("pool", ys5[:, :, 0:N], g.YS5T[:, :, t0:t0 + N].rearrange("m p t -> p m t"), reads=[g.YS5T], writes=[ys5])
            mk.dma("pool", ygl[:, :, 0:N], g.YGLAT[:, :, t0:t0 + N].rearrange("m p t -> p m t"), reads=[g.YGLAT], writes=[ygl])
            mk.dma("sp", yhtok[:, 0:ntl, :], g.YHG[t0:t0 + N, :].rearrange("(a p) c -> p a c", p=128), reads=[g.YHG], writes=[yhtok])
            for tl in range(ntl):
                for m in range(2):
                    mk.tr(ptb[:, m * 128:(m + 1) * 128], yhtok[:, tl, m * 128:(m + 1) * 128], g.identb[:],
                          reads=[yhtok, g.identb], writes=[ptb])
                mk.op("act", lambda e: e.activation(yhT[:, :, tl * 128:(tl + 1) * 128], ptb[:, 0:256].rearrange("p (m t) -> p m t", m=2), AF.Copy),
                      reads=[ptb, yhT], writes=[yhT])
            ybr = [(ys5, 2), (ygl, 4), (yhT, 2)]
            for dt in range(8):
                for br in range(3):
                    pg = nextrot()
                    for kc in range(8):
                        mk.mm(pg[:, 0:N], Wg[:, kc, br * 1024 + dt * 128:br * 1024 + (dt + 1) * 128], ub[:, kc, 0:N],
                              start=(kc == 0), stop=(kc == 7), reads=[Wg, ub], writes=[pg])
                    sg = sig[br % 2]
                    mk.op("act", lambda e: e.activation(sg[:, 0:N], pg[:, 0:N], AF.Sigmoid), reads=[pg], writes=[sg])
                    pp = nextrot()
                    yb, nk = ybr[br]
                    for kc in range(nk):
                        mk.mm(pp[:, 0:N], Wb[br][:, kc, dt * 128:(dt + 1) * 128], yb[:, kc, 0:N],
                              start=(kc == 0), stop=(kc == nk - 1), reads=[Wb[br], yb], writes=[pp])
                    if br == 0:
                        mk.op("dve", lambda e: e.tensor_tensor(acc[:, 0:N], pp[:, 0:N], sg[:, 0:N], ALU.mult), reads=[pp, sg], writes=[acc])
                    elif br == 1:
                        mk.op("dve", lambda e: e.tensor_tensor(tmpm[:, 0:N], pp[:, 0:N], sg[:, 0:N], ALU.mult), reads=[pp, sg], writes=[tmpm])
                        mk.op("dve", lambda e: e.tensor_tensor(acc[:, 0:N], acc[:, 0:N], tmpm[:, 0:N], ALU.add), reads=[acc, tmpm], writes=[acc])
                    else:
                        mk.op("dve", lambda e: e.tensor_tensor(tmpm[:, 0:N], pp[:, 0:N], sg[:, 0:N], ALU.mult), reads=[pp, sg], writes=[tmpm])
                        mk.op("dve", lambda e: e.tensor_tensor(mergedTs[bi % 2][:, dt, 0:N], acc[:, 0:N], tmpm[:, 0:N], ALU.add),
                              reads=[acc, tmpm, mergedTs[bi % 2]], writes=[mergedTs[bi % 2]])
                    yield


    def tails(bi, tile0, ntl):
            N = ntl * 128
            t0 = tile0 * 128
            wv = 1 if tile0 < 2 else 0
            if cur_w[0] != wv:
                cur_w[0] = wv
                mk.dma("sp", G2row[:], g.MOD[wv, 2048:3072].partition_broadcast(128), reads=[g.MOD], writes=[G2row])
            for tl in range(ntl):
                ti = tile0 + tl
                nt_[0] += 1
                par = nt_[0] % 2
                ht = hts[par]
                mk.dma("sp", ht[:], g.H[ti * 128:(ti + 1) * 128, :], reads=[g.H], writes=[ht])
                for hf in range(2):
                    for kc in range(8):
                        mk.mm(pmix[:, hf, :], mergedTs[bi % 2][:, kc, tl * 128:(tl + 1) * 128], Wout[:, kc, hf * 512:(hf + 1) * 512],
                              start=(kc == 0), stop=(kc == 7), reads=[mergedTs[bi % 2], Wout], writes=[pmix])
                mk.op("dve", lambda e: e.tensor_tensor(tmph[:], pmix[:].rearrange("p a b -> p (a b)"), G2row[:], ALU.mult),
                      reads=[pmix, G2row], writes=[tmph])
                mk.op("dve", lambda e: e.tensor_tensor(ht[:], ht[:], tmph[:], ALU.add), reads=[ht, tmph], writes=[ht])
                mk.dma("sp", g.H[ti * 128:(ti + 1) * 128, :], ht[:], reads=[ht], writes=[g.H])
                yield
                mk.op("act", lambda e: e.activation(junk[:], ht[:], AF.Square, accum_out=ss[:, 0:1]), reads=[ht], writes=[junk, ss])
                mk.op("act", lambda e: e.activation(rstd[:], ss[:], AF.Sqrt, scale=1.0 / D, bias=g.eps_col[:, 0:1]),
                      reads=[ss, g.eps_col], writes=[rstd])
                mk.op("dve", lambda e: e.reciprocal(rstd[:], rstd[:]), reads=[rstd], writes=[rstd])
                mk.op("dve", lambda e: e.tensor_scalar(xnf[:], ht[:], rstd[:, 0:1], None, ALU.mult), reads=[ht, rstd], writes=[xnf])
                for kc in range(8):
                    mk.tr(pT32[:, kc, :], xnf[:, kc * 128:(kc + 1) * 128], g.identf[:], reads=[xnf, g.identf], writes=[pT32])
                yield
                vt = vts[par]
                mk.op("dve", lambda e: e.tensor_tensor(vT32[:], pT32[:], g.A2[:, :, wv].unsqueeze(2).broadcast_to([128, 8, 128]), ALU.mult),
                      reads=[pT32, g.A2], writes=[vT32])
                mk.op("dve", lambda e: e.tensor_tensor(vT32[:], vT32[:], g.M_all[:, 24:32, wv].unsqueeze(2).broadcast_to([128, 8, 128]), ALU.add),
                      reads=[vT32, g.M_all], writes=[vT32])
                mk.op("act", lambda e: e.activation(vt[:], vT32[:], AF.Copy), reads=[vT32], writes=[vt])
                mk.dma("pool", g.VT[:, :, ti * 128:(ti + 1) * 128].rearrange("k p t -> p k t"), vt[:], reads=[vt], writes=[g.VT])
                yield
                for kc in range(8):
                    mk.mm(plg[:, 0:16], vT32[:, kc, :], RW[:, kc, :], start=(kc == 0), stop=(kc == 7), reads=[vT32, RW], writes=[plg])
                mk.op("act", lambda e: e.activation(LGT[:, ti, :], plg[:, 0:16], AF.Copy), reads=[plg, LGT], writes=[LGT])
                yield


    def run_gens(gens):
        alive = [True] * len(gens)
        while any(alive):
            for i_, gi in enumerate(gens):
                if alive[i_]:
                    try:
                        next(gi)
                    except StopIteration:
                        alive[i_] = False

    run_gens([dtloop(0, *blocks[0])])
    for bi in range(len(blocks)):
        gl = [tails(bi, *blocks[bi])]
        if bi + 1 < len(blocks):
            gl.append(dtloop(bi + 1, *blocks[bi + 1]))
        run_gens(gl)
    V = "dve"
    tsel = slice(2, NT) if last else slice(0, NT)
    ntv = NT - 2 if last else NT
    L3 = LGT[:, tsel, :]
    sh = [128, ntv, 16]
    sh4 = [128, ntv, 4]
    Rb = {k: mk.sb("rb_" + k, [128, NT, 16], F32) for k in ("ex", "pr", "sel", "msk", "m1", "m2")}
    Rg = {k: mk.sb("rg_" + k, [128, NT, 4], F32) for k in ("a", "gs", "inb", "t2")}
    Rs = {k: mk.sb("rs_" + k, [128, NT], F32) for k in ("mx", "sum", "best", "top1", "top2", "ws")}
    X = lambda k: Rb[k][:, tsel, :]
    X4 = lambda k: Rg[k][:, tsel, :]
    X1 = lambda k: Rs[k][:, tsel]
    B16 = lambda k: Rs[k][:, tsel].unsqueeze(2).broadcast_to(sh)
    mk.op(V, lambda e: e.tensor_reduce(X1("mx"), L3, AX.X, ALU.max), reads=[LGT], writes=[Rs["mx"]])
    mk.op(V, lambda e: e.tensor_tensor(X("ex"), L3, B16("mx"), ALU.subtract), reads=[LGT, Rs["mx"]], writes=[Rb["ex"]])
    mk.op("act", lambda e: e.activation(X("ex"), X("ex"), AF.Exp), reads=[Rb["ex"]], writes=[Rb["ex"]])
    mk.op(V, lambda e: e.tensor_reduce(X1("sum"), X("ex"), AX.X, ALU.add), reads=[Rb["ex"]], writes=[Rs["sum"]])
    mk.op(V, lambda e: e.reciprocal(X1("sum"), X1("sum")), reads=[Rs["sum"]], writes=[Rs["sum"]])
    mk.op(V, lambda e: e.tensor_tensor(X("pr"), X("ex"), B16("sum"), ALU.mult), reads=[Rb["ex"], Rs["sum"]], writes=[Rb["pr"]])
    mk.op(V, lambda e: e.tensor_tensor(X("sel"), X("pr"), rb[:].unsqueeze(1).broadcast_to(sh), ALU.add), reads=[Rb["pr"], rb], writes=[Rb["sel"]])
    selg = lambda j: Rb["sel"][:, tsel, :].rearrange("p t (g j) -> p t g j", j=4)[:, :, :, j]
    first = True
    for (a_, b_) in ((0, 1), (0, 2), (0, 3), (1, 2), (1, 3), (2, 3)):
        if first:
            mk.op(V, lambda e: e.tensor_tensor(X4("gs"), selg(a_), selg(b_), ALU.add), reads=[Rb["sel"]], writes=[Rg["gs"]])
            first = False
        else:
            mk.op(V, lambda e: e.tensor_tensor(X4("a"), selg(a_), selg(b_), ALU.add), reads=[Rb["sel"]], writes=[Rg["a"]])
            mk.op(V, lambda e: e.tensor_tensor(X4("gs"), X4("gs"), X4("a"), ALU.max), reads=[Rg["gs"], Rg["a"]], writes=[Rg["gs"]])
    mk.op(V, lambda e: e.tensor_reduce(X1("best"), X4("gs"), AX.X, ALU.max), reads=[Rg["gs"]], writes=[Rs["best"]])
    mk.op(V, lambda e: e.tensor_tensor(X4("inb"), X4("gs"), Rs["best"][:, tsel].unsqueeze(2).broadcast_to(sh4), ALU.is_equal),
          reads=[Rg["gs"], Rs["best"]], writes=[Rg["inb"]])
    mk.op(V, lambda e: e.tensor_scalar(X4("t2"), X4("inb"), BIG, -BIG, ALU.mult, ALU.add), reads=[Rg["inb"]], writes=[Rg["t2"]])
    for j in range(4):
        mskj = Rb["msk"][:, tsel, :].rearrange("p t (g j) -> p t g j", j=4)[:, :, :, j]
        mk.op(V, lambda e: e.tensor_tensor(mskj, selg(j), X4("inb"), ALU.mult), reads=[Rb["sel"], Rg["inb"], Rb["msk"]], writes=[Rb["msk"]])
        mk.op(V, lambda e: e.tensor_tensor(mskj, mskj, X4("t2"), ALU.add), reads=[Rb["msk"], Rg["t2"]], writes=[Rb["msk"]])
    mk.op(V, lambda e: e.tensor_reduce(X1("top1"), X("msk"), AX.X, ALU.max), reads=[Rb["msk"]], writes=[Rs["top1"]])
    mk.op(V, lambda e: e.tensor_tensor(X("m1"), X("msk"), B16("top1"), ALU.is_equal), reads=[Rb["msk"], Rs["top1"]], writes=[Rb["m1"]])
    mk.op(V, lambda e: e.scalar_tensor_tensor(X("msk"), X("m1"), -BIG, X("msk"), ALU.mult, ALU.add), reads=[Rb["m1"], Rb["msk"]], writes=[Rb["msk"]])
    mk.op(V, lambda e: e.tensor_reduce(X1("top2"), X("msk"), AX.X, ALU.max), reads=[Rb["msk"]], writes=[Rs["top2"]])
    mk.op(V, lambda e: e.tensor_tensor(X("m2"), X("msk"), B16("top2"), ALU.is_equal), reads=[Rb["msk"], Rs["top2"]], writes=[Rb["m2"]])
    mk.op(V, lambda e: e.tensor_tensor(X("m1"), X("m1"), X("m2"), ALU.add), reads=[Rb["m1"], Rb["m2"]], writes=[Rb["m1"]])
    mk.op(V, lambda e: e.tensor_tensor(X("ex"), X("pr"), X("m1"), ALU.mult), reads=[Rb["pr"], Rb["m1"]], writes=[Rb["ex"]])
    mk.op(V, lambda e: e.tensor_reduce(X1("ws"), X("ex"), AX.X, ALU.add), reads=[Rb["ex"]], writes=[Rs["ws"]])
    mk.op(V, lambda e: e.reciprocal(X1("ws"), X1("ws")), reads=[Rs["ws"]], writes=[Rs["ws"]])
    mk.op(V, lambda e: e.tensor_tensor(g.gates[:, tsel, :], X("ex"), B16("ws"), ALU.mult), reads=[Rb["ex"], Rs["ws"], g.gates], writes=[g.gates])
    if getattr(g, 'debug_gates', False):
        mk.dma("sp", g.GATESD[:, :, :], g.gates[:], reads=[g.gates], writes=[g.GATESD])
    mk.end_phase()


def phase_moe(mk, g, layer):
    nc = mk.nc
    last = (layer == 1)
    mk.begin_phase()
    tiles = list(range(2, NT)) if last else list(range(NT))
    nsb = 4
    per = (len(tiles) + nsb - 1) // nsb
    sbs = [tiles[i * per:(i + 1) * per] for i in range(nsb)]
    sbs = [x for x in sbs if x]
    MAXT = max(len(x) for x in sbs)
    accs = [mk.sb("macc%d" % i, [128, MAXT, D], F32) for i in range(2)]
    vts = [mk.sb("mvt%d" % i, [128, 8, MAXT * 128], BF16) for i in range(2)]

    def load_vt(si):
        sb_ = sbs[si]
        mk.dma("sp", vts[si % 2][:, :, 0:len(sb_) * 128], g.VT[:, :, sb_[0] * 128:sb_[0] * 128 + len(sb_) * 128].rearrange("k p t -> p k t"),
               reads=[g.VT], writes=[vts[si % 2]])
    load_vt(0)
    Wup = [mk.sb("Wup%d" % i, [128, 8, 1024], BF16) for i in range(2)]
    Wdn = [mk.sb("Wdn%d" % i, [128, 4, 1024], BF16) for i in range(2)]
    sgs = [mk.sb("msg%d" % i, [128, 512], F32) for i in range(2)]
    hid = [mk.sb("hid%d" % i, [128, 4, 512], BF16) for i in range(2)]
    hts = [mk.sb("mh%d" % i, [128, D], F32) for i in range(2)]
    tmph = mk.sb("mtmp", [128, D], F32)
    G5row = mk.sb("G5row", [128, D], F32)
    pup = [mk.ps("pup%d" % i, [128, 512], F32) for i in range(4)]
    pdn = [mk.ps("pdn%d" % i, [128, 2, 512], F32) for i in range(2)]
    if last:
        fng = mk.sb("fng", [128, D], F32)
        mk.dma("sp", fng[:], g.final_norm_g[:].partition_broadcast(128), reads=[g.final_norm_g], writes=[fng])
        junk = mk.sb("mjunk", [128, D], BF16)
        ss = mk.sb("mss", [128, 1], F32)
        rstd = mk.sb("mrstd", [128, 1], F32)
    cur_w = [None]
    nw = 0
    nup = 0
    nh = 0
    ndn = 0
    def tail_fn(sb, acc):
        for j, ti in enumerate(sb):
            wv = 1 if ti < 2 else 0
            if cur_w[0] != wv:
                cur_w[0] = wv
                mk.dma("sp", G5row[:], g.MOD[wv, 5120:6144].partition_broadcast(128), reads=[g.MOD], writes=[G5row])
            ht = hts[j % 2]
            mk.dma("sp", ht[:], g.H[ti * 128:(ti + 1) * 128, :], reads=[g.H], writes=[ht])
            mk.op("pool", lambda e: e.tensor_tensor(tmph[:], acc[:, j, :], G5row[:], ALU.mult), reads=[acc, G5row], writes=[tmph])
            mk.op("pool", lambda e: e.tensor_tensor(ht[:], ht[:], tmph[:], ALU.add), reads=[ht, tmph], writes=[ht])
            if not last:
                mk.dma("sp", g.H[ti * 128:(ti + 1) * 128, :], ht[:], reads=[ht], writes=[g.H])
            else:
                mk.op("act", lambda e: e.activation(junk[:], ht[:], AF.Square, accum_out=ss[:, 0:1]), reads=[ht], writes=[junk, ss])
                mk.op("act", lambda e: e.activation(rstd[:], ss[:], AF.Sqrt, scale=1.0 / D, bias=g.eps_col[:, 0:1]),
                      reads=[ss, g.eps_col], writes=[rstd])
                mk.op("dve", lambda e: e.reciprocal(rstd[:], rstd[:]), reads=[rstd], writes=[rstd])
                mk.op("dve", lambda e: e.scalar_tensor_tensor(ht[:], ht[:], rstd[:, 0:1], fng[:], ALU.mult, ALU.mult),
                      reads=[ht, rstd, fng], writes=[ht])
                r0 = ti * 128 - NCTX
                mk.dma("sp", g.OUT[r0:r0 + 128, :], ht[:], reads=[ht], writes=[g.OUT])


    items = []
    for si, sb in enumerate(sbs):
        nts = len(sb)
        subs = []
        o = 0
        while o < nts:
            n = min(4, nts - o)
            subs.append((o, n))
            o += n
        for e_ in range(16):
            for k_, (o, n) in enumerate(subs):
                items.append((si, e_, k_, o, n))
    st = {"nw": 0, "nup": 0, "nh": 0, "ndn": 0, "w": {}}

    def up_stage(it):
        si, e_, k_, o, n = it
        sb = sbs[si]
        vt = vts[si % 2]
        if e_ == 0 and k_ == 0 and si + 1 < len(sbs):
            load_vt(si + 1)
        if k_ == 0:
            if e_ == 2 and si > 0:
                tail_fn(sbs[si - 1], accs[(si - 1) % 2])
            wu = Wup[st["nw"] % 2]
            wd = Wdn[st["nw"] % 2]
            st["nw"] += 1
            st["w"][(si, e_)] = (wu, wd)
            mk.dma("pool", wu[:], g.moe_w_up[layer, e_].rearrange("(kc p) n -> p kc n", p=128), reads=[g.moe_w_up], writes=[wu])
            mk.dma("pool", wd[:], g.moe_w_down[layer, e_].rearrange("(kc p) n -> p kc n", p=128), reads=[g.moe_w_down], writes=[wd])
        wu, wd = st["w"][(si, e_)]
        N = n * 128
        c0 = o * 128
        hd = hid[st["nh"] % 2]
        st["nh"] += 1
        for ft in range(4):
            pg = pup[st["nup"] % 4]
            pu = pup[(st["nup"] + 1) % 4]
            st["nup"] += 2
            for kc in range(8):
                mk.mm(pg[:, 0:N], wu[:, kc, ft * 128:(ft + 1) * 128], vt[:, kc, c0:c0 + N], start=(kc == 0), stop=(kc == 7),
                      reads=[wu, vt], writes=[pg])
            for kc in range(8):
                mk.mm(pu[:, 0:N], wu[:, kc, 512 + ft * 128:512 + (ft + 1) * 128], vt[:, kc, c0:c0 + N], start=(kc == 0), stop=(kc == 7),
                      reads=[wu, vt], writes=[pu])
            sg = sgs[ft % 2]
            mk.op("act", lambda e: e.activation(sg[:, 0:N], pg[:, 0:N], AF.Silu), reads=[pg], writes=[sg])
            mk.op("dve", lambda e: e.tensor_tensor(hd[:, ft, 0:N], pu[:, 0:N], sg[:, 0:N], ALU.mult), reads=[pu, sg, hd], writes=[hd])
        return hd

    def down_stage(it, hd):
        si, e_, k_, o, n = it
        sb = sbs[si]
        acc = accs[si % 2]
        wu, wd = st["w"][(si, e_)]
        for tl in range(n):
            ti = sb[o + tl]
            pd = pdn[st["ndn"] % 2]
            st["ndn"] += 1
            for hf in range(2):
                for ft in range(4):
                    mk.mm(pd[:, hf, :], hd[:, ft, tl * 128:(tl + 1) * 128], wd[:, ft, hf * 512:(hf + 1) * 512],
                          start=(ft == 0), stop=(ft == 3), reads=[hd, wd], writes=[pd])
            pdv = pd[:].rearrange("p a b -> p (a b)")
            if e_ == 0:
                mk.op("dve", lambda e: e.tensor_scalar(acc[:, o + tl, :], pdv, g.gates[:, ti, e_:e_ + 1], None, ALU.mult),
                      reads=[pd, g.gates, acc], writes=[acc])
            else:
                mk.op("dve", lambda e: e.scalar_tensor_tensor(acc[:, o + tl, :], pdv, g.gates[:, ti, e_:e_ + 1], acc[:, o + tl, :],
                                                               ALU.mult, ALU.add), reads=[pd, g.gates, acc], writes=[acc])

    hd_cur = up_stage(items[0])
    for k in range(len(items)):
        hd_next = up_stage(items[k + 1]) if k + 1 < len(items) else None
        down_stage(items[k], hd_cur)
        hd_cur = hd_next
    tail_fn(sbs[-1], accs[(len(sbs) - 1) % 2])
    mk.end_phase()


def build_program():
    nc = bass.Bass("TRN2", target_bir_lowering=False)
    with ExitStack() as st:
        mk = MK(nc, st)
        g = G()
        declare(mk, g)
        st.enter_context(nc.Block())
        setup_consts(mk, g)
        phase_init_h(mk, g)
        for layer in range(2):
            phase_mod(mk, g, layer)
            phase_norm_s5(mk, g, layer)
            phase_linattn2(mk, g, layer, "gla")
            phase_linattn2(mk, g, layer, "hgrn")
            phase_merge(mk, g, layer)
            phase_moe(mk, g, layer)
        mk.finish([g.OUT], "sp")
        for e in ("act", "pool", "dve", "pe"):
            mk.finish([g.OUT], e)
    return nc


_NC_CACHE = {}


def kernel(**inputs):
    n = 8
    if "nc" not in _NC_CACHE:
        _NC_CACHE["nc"] = build_program()
    nc = _NC_CACHE["nc"]
    params = {k: np.ascontiguousarray(np.asarray(inputs[k], dtype=np.float32)) for k in PARAM_SHAPES}
    x = np.asarray(inputs["x"], dtype=np.float32)
    ctx = np.asarray(inputs["ctx"], dtype=np.float32)
    c = np.asarray(inputs["c"], dtype=np.float32)
    c_ctx = np.ascontiguousarray(np.asarray(inputs["c_ctx"], dtype=np.float32))
    in_maps = []
    for b in range(n):
        m = dict(params)
        m["x"] = np.ascontiguousarray(x[b])
        m["ctx"] = np.ascontiguousarray(ctx[b])
        m["c"] = np.ascontiguousarray(c[b])
        m["c_ctx"] = c_ctx
        in_maps.append(m)
    res = run_bass_kernel_spmd(nc, in_maps, core_ids=list(range(n)))
    out = np.stack([np.asarray(res.results[b]["out"], dtype=np.float32) for b in range(n)], axis=0)
    return out


SCRATCH.update({
    "OBG": ([4, 128, T], F32),
    "OBH": ([2, 128, T], F32),
})


def phase_linattn2(mk, g, layer, kind):
    nc = mk.nc
    gla = (kind == "gla")
    DV = 128 if gla else 64
    VW = 4 * DV
    esc = (-1.0 / 16.0) if gla else 1.0
    qscale = 0.125 if gla else 1.0
    UTsrc = g.UT if gla else g.UTH
    OFs = [g.OFG, g.OBG] if gla else [g.OFH, g.OBH]
    NPO = 4 if gla else 2
    cq, ck, cv = (C_GQ, C_GK, C_GV) if gla else (C_HQ, None, C_HI)
    mk.begin_phase()
    rst = mk.sb("rst", [128, 128], F32)
    mk.op("pool", lambda e: e.memset(rst[:], 1.0), writes=[rst])
    for c in range(4):
        mk.op("pool", lambda e: e.memset(rst[:, 32 * c:32 * c + 1], 0.0), reads=[rst], writes=[rst])
    masks = []
    for d in range(2):
        m = mk.sb("mask%d" % d, [128, 128], F32)
        mk.op("pool", lambda e: e.memset(m[:], 1.0), writes=[m])
        if d == 0:
            mk.op("pool", lambda e: e.affine_select(out=m[:], in_=m[:], pattern=[[1, 128]], compare_op=ALU.is_ge,
                                                     fill=0.0, base=0, channel_multiplier=-1), reads=[m], writes=[m])
        else:
            mk.op("pool", lambda e: e.affine_select(out=m[:], in_=m[:], pattern=[[-1, 128]], compare_op=ALU.is_ge,
                                                     fill=0.0, base=0, channel_multiplier=1), reads=[m], writes=[m])
        for c in range(4):
            cs = slice(32 * c, 32 * c + 32)
            if d == 0:
                mk.op("pool", lambda e: e.affine_select(out=m[:, cs], in_=m[:, cs], pattern=[[0, 32]], compare_op=ALU.is_ge,
                                                         fill=0.0, base=-32 * c, channel_multiplier=1), reads=[m], writes=[m])
            else:
                mk.op("pool", lambda e: e.affine_select(out=m[:, cs], in_=m[:, cs], pattern=[[0, 32]], compare_op=ALU.is_ge,
                                                         fill=0.0, base=32 * c + 31, channel_multiplier=-1), reads=[m], writes=[m])
        masks.append(m)
    rowmask = mk.sb("rowmask", [128, 4], F32)
    mk.op("pool", lambda e: e.memset(rowmask[:], 1.0), writes=[rowmask])
    for c in range(4):
        mk.op("pool", lambda e: e.affine_select(out=rowmask[:, c:c + 1], in_=rowmask[:, c:c + 1], pattern=[[0, 1]],
                                                 compare_op=ALU.is_ge, fill=0.0, base=-32 * c, channel_multiplier=1),
              reads=[rowmask], writes=[rowmask])
        mk.op("pool", lambda e: e.affine_select(out=rowmask[:, c:c + 1], in_=rowmask[:, c:c + 1], pattern=[[0, 1]],
                                                 compare_op=ALU.is_ge, fill=0.0, base=32 * c + 31, channel_multiplier=-1),
              reads=[rowmask], writes=[rowmask])
    rowsel = mk.sb("rowsel", [128, 2], F32)
    mk.op("pool", lambda e: e.memset(rowsel[:], 1.0), writes=[rowsel])
    mk.op("pool", lambda e: e.affine_select(out=rowsel[:, 0:1], in_=rowsel[:, 0:1], pattern=[[0, 1]], compare_op=ALU.is_ge,
                                             fill=0.0, base=63, channel_multiplier=-1), reads=[rowsel], writes=[rowsel])
    mk.op("pool", lambda e: e.affine_select(out=rowsel[:, 1:2], in_=rowsel[:, 1:2], pattern=[[0, 1]], compare_op=ALU.is_ge,
                                             fill=0.0, base=-64, channel_multiplier=1), reads=[rowsel], writes=[rowsel])

    lnsel = mk.sb("lnsel", [128, 2], F32)
    mk.op("dve", lambda e: e.tensor_scalar(lnsel[:], rowsel[:], -1.0, 30000.0, ALU.add, ALU.mult), reads=[rowsel], writes=[lnsel])

    def loadw(name, c0, n):
        w = mk.sb(name, [128, 8, n], BF16)
        mk.dma("pool", w[:], g.w_in[layer, :, c0:c0 + n].rearrange("(kc p) n -> p kc n", p=128), reads=[g.w_in], writes=[w])
        return w
    Wq = loadw("Wq", cq, 256)
    Wv = loadw("Wv", cv, VW)
    if gla:
        Wk = loadw("Wk", ck, 256)
        Wdec = [loadw("Wc%d" % d, C_GC + 16 * d, 16) for d in range(2)]
        GU = [mk.sb("GU%d" % d, [16, 256], F32) for d in range(2)]
        nbcol = mk.sb("nbcol", [128, 2, 2], F32)
        with nc.allow_non_contiguous_dma(reason="small"):
            for d in range(2):
                mk.dma("sp", GU[d][:], g.gla_gate_up[layer, d], reads=[g.gla_gate_up], writes=[GU[d]])
                mk.dma("sp", nbcol[:, d, :], g.gla_gate_b[layer, d].rearrange("(m p) -> p m", p=128), reads=[g.gla_gate_b], writes=[nbcol])
        mk.op("dve", lambda e: e.tensor_scalar(nbcol[:], nbcol[:], -1.0, None, ALU.mult), reads=[nbcol], writes=[nbcol])
    else:
        Wdec = [loadw("Wf%d" % d, C_HF + 256 * d, 256) for d in range(2)]
        lbc = mk.sb("lbc", [128, 2], F32)
        omlb = mk.sb("omlb", [128, 2], F32)
        l0 = mk.sb("l0", [128, 2], F32)
        with nc.allow_non_contiguous_dma(reason="small"):
            mk.dma("sp", lbc[:], g.hgrn_lower[1].rearrange("(m p) -> p m", p=128), reads=[g.hgrn_lower], writes=[lbc])
            mk.dma("sp", l0[:], g.hgrn_lower[0].rearrange("(m p) -> p m", p=128), reads=[g.hgrn_lower], writes=[l0])
        if layer == 0:
            mk.op("dve", lambda e: e.memset(lbc[:], 0.0), reads=[lbc], writes=[lbc])
        else:
            mk.op("dve", lambda e: e.tensor_tensor(lbc[:], lbc[:], l0[:], ALU.subtract), reads=[lbc, l0], writes=[lbc])
            mk.op("act", lambda e: e.activation(lbc[:], lbc[:], AF.Sigmoid), reads=[lbc], writes=[lbc])
        mk.op("dve", lambda e: e.tensor_scalar(omlb[:], lbc[:], -1.0, 1.0, ALU.mult, ALU.add), reads=[lbc], writes=[omlb])
        nomlb = mk.sb("nomlb", [128, 2], F32)
        mk.op("dve", lambda e: e.tensor_scalar(nomlb[:], omlb[:], -1.0, None, ALU.mult), reads=[omlb], writes=[nomlb])

    blocks = [(0, 2)] + [(2 + 4 * b, 4) for b in range(8)]

    def chain(d):
        sfx = "_%d" % d
        pA = mk.ps("pA" + sfx, [128, 512], F32)
        pB = mk.ps("pB" + sfx, [128, 512], F32)
        pds = mk.ps("pds" + sfx, [128, 2, 256], F32)
        ptr = mk.ps("ptr" + sfx, [128, 1024], BF16)
        pscv = pA[:].rearrange("p (h t) -> p h t", h=4)
        rotl = [pA, pB]
        npj = [0]

        def nextp():
            npj[0] += 1
            return rotl[npj[0] % 2]

        ubs = [mk.sb("ub%d" % i + sfx, [128, 8, 512], BF16) for i in range(2)]
        qT = mk.sb("qT" + sfx, [128, 2, 512], F32)
        kT = mk.sb("kT" + sfx, [128, 2, 512], F32)
        LG = mk.sb("LG" + sfx, [128, 2, 512], F32)
        vtok = mk.sb("vtok" + sfx, [128, 4, VW], BF16)
        if gla:
            codeT = mk.sb("codeT" + sfx, [16, 512], F32)
        else:
            sg = mk.sb("sg" + sfx, [128, 2, 512], F32)
        cum = mk.sb("cum" + sfx, [128, 2, 128], F32)
        eq = mk.sb("eq" + sfx, [128, 2, 128], F32)
        eqm = mk.sb("eqm" + sfx, [128, 2, 2, 128], F32)
        ek = mk.sb("ek" + sfx, [128, 2, 128], F32)
        elast = mk.sb("elast" + sfx, [128, 2, 4], F32)
        qtil4 = mk.sb("qtil4" + sfx, [128, 4, 128], F32)
        ktil = mk.sb("ktil" + sfx, [128, 2, 128], F32)
        ktilb = mk.sb("ktilb" + sfx, [128, 2, 128], BF16)
        khT = mk.sb("khT" + sfx, [128, 2, 128], BF16)
        khat4 = mk.sb("khat4" + sfx, [128, 4, 256], BF16)
        PT = mk.sb("PT" + sfx, [128, 4, 128], BF16)
        S = [mk.sb("S%d" % i + sfx, [128, DV], F32) for i in range(2)]
        Sb4 = [[mk.sb("Sb4_%d_%d" % (pp_, i) + sfx, [128, 5, DV], F32) for i in range(2)] for pp_ in range(2)]
        SbT = [[[mk.sub(Sb4[pp_][i], "slot") for _ in range(5)] for i in range(2)] for pp_ in range(2)]
        ptrF = ptr[:].bitcast(F32).rearrange("p (m x) -> p m x", m=2)
        ost = [mk.sb("ost%d" % i + sfx, [128, NPO, 128], F32) for i in range(2)]
        for mt in range(2):
            mk.op("pool", lambda e: e.memset(S[mt][:], 0.0), reads=[S[mt]], writes=[S[mt]])
            for pp_ in range(2):
                mk.op("pool", lambda e: e.memset(Sb4[pp_][mt][:], 0.0), reads=SbT[pp_][mt], writes=SbT[pp_][mt])
        ntile = 0
        order = blocks if d == 0 else [blocks[0]] + blocks[:0:-1]
        for bi, (tile0, ntl) in enumerate(order):
            N = ntl * 128
            t0 = tile0 * 128
            ub = ubs[bi % 2]
            if bi == 0:
                mk.dma("sp", ub[:, :, 0:N], UTsrc[:, :, t0:t0 + N].rearrange("k p t -> p k t"), reads=[UTsrc], writes=[ub])

            def proj(W, c0, m, evac):
                pp = nextp()
                for kc in range(8):
                    mk.mm(pp[0:m, 0:N], W[:, kc, c0:c0 + m], ub[:, kc, 0:N], start=(kc == 0), stop=(kc == 7),
                          reads=[W, ub], writes=[pp])
                evac(pp)

            for mt in range(2):
                fq = AF.Copy if gla else AF.Silu
                proj(Wq, mt * 128, 128, lambda pp: mk.op("act", lambda e: e.activation(qT[:, mt, 0:N], pp[:, 0:N], fq),
                                                          reads=[pp, qT], writes=[qT]))
                yield
            if gla:
                for mt in range(2):
                    proj(Wk, mt * 128, 128, lambda pp: mk.op("dve", lambda e: e.tensor_copy(kT[:, mt, 0:N], pp[:, 0:N]),
                                                              reads=[pp, kT], writes=[kT]))
                    yield
                proj(Wdec[d], 0, 16, lambda pp: mk.op("act", lambda e: e.activation(codeT[:, 0:N], pp[0:16, 0:N], AF.Copy),
                                                       reads=[pp], writes=[codeT]))
                for mt in range(2):
                    pp = nextp()
                    mk.mm(pp[:, 0:N], GU[d][:, mt * 128:(mt + 1) * 128], codeT[:, 0:N], start=True, stop=True,
                          reads=[GU[d], codeT], writes=[pp])
                    mk.op("act", lambda e: e.activation(LG[:, mt, 0:N], pp[:, 0:N], AF.Exp, scale=-1.0, bias=nbcol[:, d, mt:mt + 1]),
                          reads=[pp, nbcol, LG], writes=[LG])
                    mk.op("act", lambda e: e.activation(LG[:, mt, 0:N], LG[:, mt, 0:N], AF.Ln, bias=g.one_col[:, 0:1]),
                          reads=[LG, g.one_col], writes=[LG])
                    yield
            else:
                for mt in range(2):
                    def ev(pp):
                        mk.op("act", lambda e: e.activation(sg[:, mt, 0:N], pp[:, 0:N], AF.Sigmoid), reads=[pp, sg], writes=[sg])
                        mk.op("act", lambda e: e.activation(LG[:, mt, 0:N], sg[:, mt, 0:N], AF.Ln, scale=omlb[:, mt:mt + 1],
                                                             bias=lbc[:, mt:mt + 1]), reads=[sg, omlb, lbc, LG], writes=[LG])
                        mk.op("act", lambda e: e.activation(kT[:, mt, 0:N], sg[:, mt, 0:N], AF.Identity, scale=nomlb[:, mt:mt + 1],
                                                             bias=omlb[:, mt:mt + 1]), reads=[sg, kT, omlb, nomlb], writes=[kT])
                    proj(Wdec[d], mt * 128, 128, ev)
                    yield
            for tl in range(ntl):
                pp = nextp()
                for kc in range(8):
                    mk.mm(pp[:, 0:VW], ub[:, kc, tl * 128:(tl + 1) * 128], Wv[:, kc, :], start=(kc == 0), stop=(kc == 7),
                          reads=[Wv, ub], writes=[pp])
                mk.op("dve", lambda e: e.tensor_copy(vtok[:, tl, :], pp[:, 0:VW]), reads=[pp, vtok], writes=[vtok])
                yield
            if bi + 1 < len(order):
                tile0n, ntln = order[bi + 1]
                mk.dma("sp", ubs[(bi + 1) % 2][:, :, 0:ntln * 128],
                       UTsrc[:, :, tile0n * 128:(tile0n + ntln) * 128].rearrange("k p t -> p k t"), reads=[UTsrc], writes=[ubs[(bi + 1) % 2]])
            tls = list(range(ntl)) if d == 0 else list(range(ntl - 1, -1, -1))
            for tl in tls:
                ti = tile0 + tl
                tsl = slice(tl * 128, (tl + 1) * 128)
                ntile += 1
                par = ntile % 2
                for mt in range(2):
                    if d == 0:
                        osl = slice(0, 128)
                        isl = slice(tl * 128, tl * 128 + 128)
                    else:
                        osl = slice(127, None, -1)
                        ilo, ihi = tl * 128, tl * 128 + 128
                        isl = slice(ihi - 1, ilo - 1 if ilo > 0 else None, -1)
                    mk.op("dve", lambda e: e.tensor_tensor_scan(cum[:, mt, osl], rst[:, :], LG[:, mt, isl], 0.0,
                                                                 ALU.mult, ALU.add), reads=[LG, rst, cum], writes=[cum])
                yield
                lidx = slice(31, 128, 32) if d == 0 else slice(0, 128, 32)
                for r in range(2):
                    mk.op("act", lambda e: e.activation(eqm[:, r], cum[:], AF.Exp, scale=esc, bias=lnsel[:, r:r + 1]),
                          reads=[cum, lnsel, eqm], writes=[eqm])
                mk.op("act", lambda e: e.activation(ek[:], cum[:], AF.Exp, scale=-esc), reads=[cum], writes=[ek])
                mk.op("act", lambda e: e.activation(elast[:], cum[:, :, lidx], AF.Exp, scale=esc), reads=[cum], writes=[elast])
                for r in range(2):
                    mk.op("dve", lambda e: e.scalar_tensor_tensor(qtil4[:].rearrange("p (m r) t -> p r m t", r=2)[:, r],
                                                                   qT[:, :, tsl], qscale, eqm[:, r], ALU.mult, ALU.mult),
                          reads=[qT, eqm, qtil4], writes=[qtil4])
                mk.op("dve", lambda e: e.tensor_tensor(ktil[:], kT[:, :, tsl], ek[:], ALU.mult), reads=[kT, ek], writes=[ktil])
                mk.op("dve", lambda e: e.tensor_tensor(khT[:].rearrange("p m (c j) -> p m c j", j=32),
                                                        ktil[:].rearrange("p m (c j) -> p m c j", j=32),
                                                        elast[:].unsqueeze(3).broadcast_to([128, 2, 4, 32]), ALU.mult),
                      reads=[ktil, elast], writes=[khT])
                yield
                for mt in range(2):
                    mk.tr(ptr[:, mt * 128:(mt + 1) * 128], khT[:, mt, :], g.identb[:], reads=[khT, g.identb], writes=[ptr])
                for c in range(4):
                    if c % 2 == 0:
                        mk.op("act", lambda e: e.activation(khat4[:, c, :], ptr[:, 0:256], AF.Identity, scale=rowmask[:, c:c + 1]),
                              reads=[ptr, rowmask, khat4], writes=[khat4])
                    else:
                        mk.op("pool", lambda e: e.tensor_scalar(khat4[:, c, :], ptr[:, 0:256], rowmask[:, c:c + 1], None, ALU.mult),
                              reads=[ptr, rowmask, khat4], writes=[khat4]) if False else \
                            mk.op("dve", lambda e: e.tensor_scalar(khat4[:, c, :], ptr[:, 0:256], rowmask[:, c:c + 1], None, ALU.mult),
                                  reads=[ptr, rowmask, khat4], writes=[khat4])
                for h in range(4):
                    mk.mm(pscv[:, h, :], ktil[:, h // 2, :], qtil4[:, h, :], start=True, stop=True,
                          reads=[ktil, qtil4], writes=[pA])
                mk.op("dve", lambda e: e.tensor_tensor(PT[:], pscv, masks[d][:].unsqueeze(1).broadcast_to([128, 4, 128]), ALU.mult),
                      reads=[pA, masks[d]], writes=[PT])
                yield

                def po_ap(h, cols):
                    if gla:
                        return pB[:, h * 128 + cols.start:h * 128 + cols.stop]
                    mt_ = h // 2
                    return pB[64 * (h % 2):64 * (h % 2) + 64, mt_ * 128 + cols.start:mt_ * 128 + cols.stop]

                chunks = list(range(4)) if d == 0 else [3, 2, 1, 0]
                cur = ntile % 2
                prv = 1 - cur
                for ci, c in enumerate(chunks):
                    bank, bv = (pds, pds) if ci < 2 else (ptr, ptrF)
                    sl = ci % 2
                    for h in range(4):
                        mt, rows = h // 2, slice(64 * (h % 2), 64 * (h % 2) + 64)
                        mk.mm(bv[rows, mt, sl * 128:sl * 128 + DV], khat4[:, c, h * 64:(h + 1) * 64], vtok[:, tl, h * DV:(h + 1) * DV],
                              start=True, stop=True, reads=[khat4, vtok], writes=[bank])
                for ci, c in enumerate(chunks):
                    bank, bv = (pds, pds) if ci < 2 else (ptr, ptrF)
                    sl = ci % 2
                    for mt in range(2):
                        if ci == 0:
                            sin_ap, sin_buf = Sb4[prv][mt][:, 4, :], SbT[prv][mt][4]
                        else:
                            sin_ap, sin_buf = Sb4[cur][mt][:, ci, :], SbT[cur][mt][ci]
                        mk.op("dve", lambda e: e.scalar_tensor_tensor(Sb4[cur][mt][:, ci + 1, :], sin_ap, elast[:, mt, c:c + 1],
                                                                       bv[:, mt, sl * 128:sl * 128 + DV], ALU.mult, ALU.add),
                              reads=[sin_buf, elast, bank, SbT[cur][mt][ci + 1]], writes=[SbT[cur][mt][ci + 1]])
                yield
                for ci, c in enumerate(chunks):
                    cs = slice(32 * c, 32 * c + 32)
                    for h in range(4):
                        mt = h // 2
                        if ci == 0:
                            st_ap, st_buf = Sb4[prv][mt][:, 4, :], SbT[prv][mt][4]
                        else:
                            st_ap, st_buf = Sb4[cur][mt][:, ci, :], SbT[cur][mt][ci]
                        mk.mm(po_ap(h, cs), vtok[:, tl, h * DV:(h + 1) * DV], PT[:, h, cs], start=True, stop=False,
                              reads=[vtok, PT], writes=[pB])
                        mk.mm(po_ap(h, cs), st_ap, qtil4[:, h, cs], start=False, stop=True,
                              reads=[st_buf, qtil4], writes=[pB])
                yield
                if gla:
                    mk.op("act", lambda e: e.activation(ost[par][:], pB[:].rearrange("p (h t) -> p h t", h=4), AF.Copy), reads=[pB], writes=[ost[par]])
                else:
                    mk.op("act", lambda e: e.activation(ost[par][:], pB[:, 0:256].rearrange("p (m t) -> p m t", m=2), AF.Copy),
                          reads=[pB], writes=[ost[par]])
                mk.dma("pool", OFs[d][:, :, ti * 128:(ti + 1) * 128].rearrange("m p t -> p m t"), ost[par][:],
                       reads=[ost[par]], writes=[OFs[d]])
                yield

    push_scope(mk)
    gens = [chain(0), chain(1)]
    alive = [True, True]
    while any(alive):
        for i, gi in enumerate(gens):
            if alive[i]:
                try:
                    next(gi)
                except StopIteration:
                    alive[i] = False
    pop_scope(mk)

    push_scope(mk)
    pf = [mk.ps("pf%d" % i, [128, 512], F32) for i in range(2)]
    pss = mk.ps("pssf", [128, 512], F32)
    ubs = [mk.sb("fub%d" % i, [128, 8, 512], BF16) for i in range(2)]
    ofl = [mk.sb("fof%d" % i, [128, NPO, 512], F32) for i in range(2)]
    obl = [mk.sb("fob%d" % i, [128, NPO, 512], F32) for i in range(2)]
    if gla:
        Wr = loadw("Wr", C_GR, 512)
        gn = mk.sb("gncol", [128, 1], F32)
        with nc.allow_non_contiguous_dma(reason="small"):
            mk.dma("sp", gn[:], g.gla_norm_g[layer].rearrange("(p o) -> p o", o=1), reads=[g.gla_norm_g], writes=[gn])
        ones_bf = mk.sb("ones_bf", [128, 128], BF16)
        mk.op("pool", lambda e: e.memset(ones_bf[:], 1.0), writes=[ones_bf])
        rsil = mk.sb("rsil", [128, 4, 512], F32)
        osq = mk.sb("osq", [128, 512], BF16)
        rstd = mk.sb("rstdg", [128, 512], F32)
        ysts = [mk.sb("ystg%d" % i, [128, 4, 512], BF16) for i in range(2)]
    else:
        Wog = loadw("Wog", C_HO, 256)
        gnrow = mk.sb("gnrow", [128, 4, 64], F32)
        for h in range(4):
            mk.dma("sp", gnrow[:, h, :], g.hgrn_norm_g[layer].partition_broadcast(128), reads=[g.hgrn_norm_g], writes=[gnrow])
        ogs = mk.sb("ogs", [128, 256], F32)
        otok = mk.sb("otok", [128, 256], F32)
        junkh = mk.sb("junkh", [128, 64], F32)
        ssh = mk.sb("ssh", [128, 4], F32)
        rsth = mk.sb("rsth", [128, 4], F32)
        ysth = [mk.sb("ysth%d" % i, [128, 256], BF16) for i in range(2)]
    npf = 0
    for bi, (tile0, ntl) in enumerate(blocks):
        N = ntl * 128
        t0 = tile0 * 128
        ub = ubs[bi % 2]
        of_, ob_ = ofl[bi % 2], obl[bi % 2]
        mk.dma("sp", ub[:, :, 0:N], UTsrc[:, :, t0:t0 + N].rearrange("k p t -> p k t"), reads=[UTsrc], writes=[ub])
        mk.dma("pool", of_[:, :, 0:N], OFs[0][:, :, t0:t0 + N].rearrange("m p t -> p m t"), reads=[OFs[0]], writes=[of_])
        mk.dma("pool", ob_[:, :, 0:N], OFs[1][:, :, t0:t0 + N].rearrange("m p t -> p m t"), reads=[OFs[1]], writes=[ob_])
        mk.op("dve", lambda e: e.tensor_tensor(of_[:, :, 0:N], of_[:, :, 0:N], ob_[:, :, 0:N], ALU.add), reads=[of_, ob_], writes=[of_])
        if gla:
            yst = ysts[bi % 2]
            for h in range(4):
                pp = pf[npf % 2]
                npf += 1
                for kc in range(8):
                    mk.mm(pp[:, 0:N], Wr[:, kc, h * 128:(h + 1) * 128], ub[:, kc, 0:N], start=(kc == 0), stop=(kc == 7),
                          reads=[Wr, ub], writes=[pp])
                mk.op("act", lambda e: e.activation(rsil[:, h, 0:N], pp[:, 0:N], AF.Silu), reads=[pp, rsil], writes=[rsil])
                mk.op("act", lambda e: e.activation(osq[:, 0:N], of_[:, h, 0:N], AF.Square), reads=[of_], writes=[osq])
                mk.mm(pss[:, 0:N], ones_bf[:], osq[:, 0:N], start=True, stop=True, reads=[ones_bf, osq], writes=[pss])
                mk.op("act", lambda e: e.activation(rstd[:, 0:N], pss[:, 0:N], AF.Sqrt, scale=1.0 / 128.0, bias=g.eps_col[:, 0:1]),
                      reads=[pss, g.eps_col], writes=[rstd])
                mk.op("dve", lambda e: e.reciprocal(rstd[:, 0:N], rstd[:, 0:N]), reads=[rstd], writes=[rstd])
                mk.op("dve", lambda e: e.scalar_tensor_tensor(of_[:, h, 0:N], of_[:, h, 0:N], gn[:, 0:1], rstd[:, 0:N],
                                                               ALU.mult, ALU.mult), reads=[of_, gn, rstd], writes=[of_])
                mk.op("dve", lambda e: e.tensor_tensor(yst[:, h, 0:N], of_[:, h, 0:N], rsil[:, h, 0:N], ALU.mult),
                      reads=[of_, rsil, yst], writes=[yst])
            mk.dma("sp", g.YGLAT[:, :, t0:t0 + N].rearrange("m p t -> p m t"), yst[:, :, 0:N], reads=[yst], writes=[g.YGLAT])
        else:
            for tl in range(ntl):
                ti = tile0 + tl
                pp = pf[npf % 2]
                npf += 1
                for kc in range(8):
                    mk.mm(pp[:, 0:256], ub[:, kc, tl * 128:(tl + 1) * 128], Wog[:, kc, :], start=(kc == 0), stop=(kc == 7),
                          reads=[Wog, ub], writes=[pp])
                mk.op("act", lambda e: e.activation(ogs[:], pp[:, 0:256], AF.Silu), reads=[pp], writes=[ogs])
                for mt in range(2):
                    mk.tr(pss[:, mt * 128:(mt + 1) * 128], of_[:, mt, tl * 128:(tl + 1) * 128], g.identf[:], reads=[of_, g.identf], writes=[pss])
                mk.op("act", lambda e: e.activation(otok[:], pss[:, 0:256], AF.Copy), reads=[pss], writes=[otok])
                for h in range(4):
                    mk.op("act", lambda e: e.activation(junkh[:], otok[:, h * 64:(h + 1) * 64], AF.Square, accum_out=ssh[:, h:h + 1]),
                          reads=[otok, junkh, ssh], writes=[junkh, ssh])
                mk.op("act", lambda e: e.activation(rsth[:], ssh[:], AF.Sqrt, scale=1.0 / 64.0, bias=g.eps_col[:, 0:1]),
                      reads=[ssh, g.eps_col], writes=[rsth])
                mk.op("dve", lambda e: e.reciprocal(rsth[:], rsth[:]), reads=[rsth], writes=[rsth])
                ov = otok[:].rearrange("p (h v) -> p h v", h=4)
                mk.op("dve", lambda e: e.tensor_tensor(ov, ov, rsth[:].unsqueeze(2).broadcast_to([128, 4, 64]), ALU.mult),
                      reads=[otok, rsth], writes=[otok])
                mk.op("dve", lambda e: e.tensor_tensor(ov, ov, gnrow[:], ALU.mult), reads=[otok, gnrow], writes=[otok])
                yh = ysth[ti % 2]
                mk.op("dve", lambda e: e.tensor_tensor(yh[:], otok[:], ogs[:], ALU.mult), reads=[otok, ogs], writes=[yh])
                pofs = 0
                for (r0, rs, n) in scan_rows(ti):
                    dst = g.YHG[r0:r0 + n, :] if rs == 1 else g.YHG[r0:r0 + (n - 1) * rs + 1:rs, :]
                    mk.dma("sp", dst, yh[pofs:pofs + n, :], reads=[yh], writes=[g.YHG])
                    pofs += n
    pop_scope(mk)
    mk.end_phase()


def phase_norm1_both(mk, g, layer):
    mk.begin_phase()

    def chain(scan):
        sfx = "_s" if scan else "_n"
        dst = g.UTH if scan else g.UT
        hts = [mk.sb("h_t%d" % i + sfx, [128, D], F32) for i in range(2)]
        xns = [mk.sb("xn%d" % i + sfx, [128, D], BF16) for i in range(2)]
        uts = [mk.sb("ut%d" % i + sfx, [128, 8, 128], BF16) for i in range(2)]
        pTs = [mk.ps("pT%d" % i + sfx, [128, 8, 128], BF16) for i in range(2)]
        junk = mk.sb("junk" + sfx, [128, D], BF16)
        sss = [mk.sb("ss%d" % i + sfx, [128, 1], F32) for i in range(2)]
        rstds = [mk.sb("rstd%d" % i + sfx, [128, 1], F32) for i in range(2)]
        q1, q2 = ("sp", "act") if not scan else ("act", "sp")
        for i in range(NT):
            b = i % 2
            wv = 1 if i < 2 else 0
            if not scan:
                mk.dma(q1, hts[b][:], g.H[i * 128:(i + 1) * 128, :], reads=[g.H], writes=[hts[b]])
            else:
                po = 0
                for (r0, rs, n) in scan_rows(i):
                    src = g.H[r0:r0 + n, :] if rs == 1 else g.H[r0:r0 + (n - 1) * rs + 1:rs, :]
                    mk.dma(q1, hts[b][po:po + n, :], src, reads=[g.H], writes=[hts[b]])
                    po += n
            norm_tile(mk, g, hts[b], xns[b], sss[b], rstds[b], junk)
            yield
            transpose_mod_tile(mk, g, xns[b], pTs[b], uts[b], g.A1, None, wv, 0)
            mk.dma(q2, dst[:, :, i * 128:(i + 1) * 128].rearrange("k p t -> p k t"), uts[b][:],
                   reads=[uts[b]], writes=[dst])
            yield

    gens = [chain(False), chain(True)]
    alive = [True, True]
    while any(alive):
        for i, gi in enumerate(gens):
            if alive[i]:
                try:
                    next(gi)
                except StopIteration:
                    alive[i] = False
    mk.end_phase()
```

```python
import math
import numpy as np
from contextlib import ExitStack
import concourse.bass as bass
import concourse.mybir as mybir
from concourse.bass_utils import run_bass_kernel_spmd

F32 = mybir.dt.float32
BF16 = mybir.dt.bfloat16
ALU = mybir.AluOpType
AF = mybir.ActivationFunctionType
AX = mybir.AxisListType

T = 4352
NT = 34
D = 1024
NCTX = 256
NLAT = 4096
N_DMA_SEMS = 48
EPS = 1e-6


class Buf:
    __slots__ = ("t", "writes", "reads", "name", "psum")

    def __init__(self, t, name="", psum=False):
        self.t = t
        self.writes = {}
        self.reads = {}
        self.name = name
        self.psum = psum

    def __getitem__(self, idx):
        return self.t[idx]


class MK:
    def __init__(self, nc, stack):
        self.nc = nc
        self.stack = stack
        self.eng = {"pe": nc.tensor, "act": nc.scalar, "dve": nc.vector,
                    "pool": nc.gpsimd, "sp": nc.sync}
        self.sem = {}
        self.cnt = {}
        self.seen = {}
        for e in self.eng:
            self.sem[e] = stack.enter_context(nc.semaphore("s_" + e))
            self.cnt[e] = 0
            self.seen[e] = {}
        self.dsem = [stack.enter_context(nc.semaphore("d%d" % i)) for i in range(N_DMA_SEMS)]
        self.dval = [0] * N_DMA_SEMS
        self.dnext = 0
        self.n_inst = 0
        self.n_wait = 0
        self.same_engine_sync = {"pe": False, "act": True, "dve": True, "pool": True, "sp": True}
        self.pstack = None
        self.uid = 0

    def begin_phase(self):
        self.pstack = ExitStack()

    def end_phase(self):
        self.barrier()
        self.pstack.close()
        self.pstack = None

    def sb(self, name, shape, dt=F32):
        self.uid += 1
        st = self.pstack if self.pstack is not None else self.stack
        return Buf(st.enter_context(self.nc.sbuf_tensor("%s_%d" % (name, self.uid), shape, dt)), name)

    def ps(self, name, shape, dt=F32):
        self.uid += 1
        st = self.pstack if self.pstack is not None else self.stack
        nbytes = int(np.prod(shape[1:])) * (4 if dt == F32 else 2)
        assert nbytes % 2048 == 0, ("psum tiles must be whole banks", name, shape)
        return Buf(st.enter_context(self.nc.psum_tensor("%s_%d" % (name, self.uid), shape, dt)), name, psum=True)

    def dram(self, name, shape, dt=F32, kind="Internal"):
        return Buf(self.nc.dram_tensor(name, shape, dt, kind=kind), name)

    def sub(self, buf, name=""):
        return Buf(buf.t, name)

    def _key_sem(self, key):
        if isinstance(key, str):
            return self.sem[key]
        return self.dsem[key]

    def _wait(self, e, deps):
        seen = self.seen[e]
        for key, val in deps.items():
            if key == e and not self.same_engine_sync[e]:
                continue
            if seen.get(key, 0) >= val:
                continue
            self.eng[e].wait_ge(self._key_sem(key), val)
            self.n_wait += 1
            seen[key] = val

    @staticmethod
    def _merge(dst, src):
        for k, v in src.items():
            if dst.get(k, 0) < v:
                dst[k] = v

    def _deps(self, reads, writes):
        deps = {}
        for b in reads:
            if b.psum:
                self._merge(deps, b.reads)
        for b in reads:
            self._merge(deps, b.writes)
        for b in writes:
            self._merge(deps, b.writes)
            self._merge(deps, b.reads)
        return deps

    def _commit(self, tok, reads, writes):
        k, v = tok
        for b in reads:
            if b.psum:
                b.reads = {kk: vv for kk, vv in b.reads.items() if kk == k}
            if b.reads.get(k, 0) < v:
                b.reads[k] = v
        for b in writes:
            b.writes = {k: v}
            b.reads = {}

    def op(self, e, fn, reads=(), writes=()):
        deps = self._deps(reads, writes)
        self._wait(e, deps)
        ins = fn(self.eng[e])
        self.cnt[e] += 1
        ins.then_inc(self.sem[e], 1)
        self._commit((e, self.cnt[e]), reads, writes)
        self.n_inst += 1
        return ins

    def dma(self, q, out_ap, in_ap, reads=(), writes=(), **kw):
        i = self.dnext
        self.dnext = (self.dnext + 1) % N_DMA_SEMS
        deps = self._deps(reads, writes)
        if self.dval[i] > 0:
            deps[i] = max(deps.get(i, 0), self.dval[i])
        self._wait(q, deps)
        self.dval[i] += 16
        ins = self.eng[q].dma_start(out=out_ap, in_=in_ap, **kw)
        ins.then_inc(self.dsem[i], 16)
        self._commit((i, self.dval[i]), reads, writes)
        self.n_inst += 1
        return ins

    def barrier(self):
        deps = {}
        for e in self.eng:
            if self.cnt[e] > 0:
                deps[e] = self.cnt[e]
        for i in range(N_DMA_SEMS):
            if self.dval[i] > 0:
                deps[i] = self.dval[i]
        for e in self.eng:
            d = {k: v for k, v in deps.items() if k != e}
            self._wait(e, d)

    def finish(self, bufs, e="sp"):
        deps = {}
        for b in bufs:
            self._merge(deps, b.writes)
        self._wait(e, deps)

    def mm(self, out, lhsT, rhs, start, stop, reads, writes):
        return self.op("pe", lambda e: e.matmul(out, lhsT, rhs, start=start, stop=stop), reads, writes)

    def tr(self, out, in_, ident, reads, writes):
        return self.op("pe", lambda e: e.transpose(out, in_, ident), reads, writes)


def make_ident(mk, dt, name):
    idf = mk.sb(name + "f", [128, 128], F32)
    mk.op("pool", lambda e: e.memset(idf[:], 1.0), writes=[idf])
    mk.op("pool", lambda e: e.affine_select(out=idf[:], in_=idf[:], pattern=[[-1, 128]],
                                             compare_op=ALU.is_equal, fill=0.0, base=0,
                                             channel_multiplier=1), reads=[idf], writes=[idf])
    if dt == F32:
        return idf
    idb = mk.sb(name + "b", [128, 128], dt)
    mk.op("dve", lambda e: e.tensor_copy(idb[:], idf[:]), reads=[idf], writes=[idb])
    return idb


class G:
    pass


PARAM_SHAPES = {
    "w_mod": [2, 1024, 6144], "b_mod": [2, 6144], "norm_mix_g": [2, 1024], "norm_ffn_g": [2, 1024],
    "w_in": [2, 1024, 6176], "s5_lam_re": [2, 2, 16, 64], "s5_lam_im": [2, 2, 16, 64],
    "s5_log_step": [2, 2, 16], "s5_b_re": [2, 16, 64, 16], "s5_b_im": [2, 16, 64, 16],
    "s5_c_re": [2, 16, 16, 64], "s5_c_im": [2, 16, 16, 64], "s5_d": [2, 256],
    "s5_glu_w": [2, 256, 256], "s5_glu_b": [2, 256], "gla_gate_up": [2, 2, 16, 256],
    "gla_gate_b": [2, 2, 256], "gla_norm_g": [2, 128], "hgrn_lower": [2, 256], "hgrn_norm_g": [2, 64],
    "w_branch_s5": [2, 256, 1024], "w_branch_gla": [2, 512, 1024], "w_branch_hgrn": [2, 256, 1024],
    "w_out": [2, 1024, 1024], "router_w": [1024, 16], "router_b": [16],
    "moe_w_up": [2, 16, 1024, 1024], "moe_w_down": [2, 16, 512, 1024], "final_norm_g": [1024],
}
ACT_SHAPES = {"x": [NLAT, D], "ctx": [NCTX, D], "c": [D], "c_ctx": [D]}

C_S5 = 0
C_GQ = 256
C_GK = 512
C_GV = 768
C_GR = 1280
C_GC = 1792
C_HQ = 1824
C_HF = 2080
C_HI = 2592
C_HO = 2848
C_GATE = 3104
IN_W = 6176

SCRATCH = {
    "H": ([T, D], F32),
    "UT": ([8, 128, T], BF16),
    "MOD": ([2, 6144], F32),
}


def declare(mk, g, ext_in=(), ext_out=()):
    nc = mk.nc
    for k, s in list(ACT_SHAPES.items()) + list(PARAM_SHAPES.items()):
        setattr(g, k, mk.dram(k, s, F32, kind="ExternalInput"))
    for k, (s, dt) in SCRATCH.items():
        kind = "Internal"
        if k in ext_in:
            kind = "ExternalInput"
        if k in ext_out:
            kind = "ExternalOutput"
        setattr(g, k, mk.dram(k, s, dt, kind=kind))
    g.OUT = mk.dram("out", [NLAT, D], F32, kind="ExternalOutput")


def setup_consts(mk, g):
    g.identb = make_ident(mk, BF16, "idb")
    g.identf = g.identb
    idf = mk.sb("idf32", [128, 128], F32)
    mk.op("pool", lambda e: e.memset(idf[:], 1.0), writes=[idf])
    mk.op("pool", lambda e: e.affine_select(out=idf[:], in_=idf[:], pattern=[[-1, 128]],
                                             compare_op=ALU.is_equal, fill=0.0, base=0,
                                             channel_multiplier=1), reads=[idf], writes=[idf])
    g.identf = idf
    g.eps_col = mk.sb("epsc", [128, 1], F32)
    mk.op("pool", lambda e: e.memset(g.eps_col[:], EPS), writes=[g.eps_col])
    g.one_col = mk.sb("onec", [128, 1], F32)
    mk.op("pool", lambda e: e.memset(g.one_col[:], 1.0), writes=[g.one_col])


def phase_init_h(mk, g):
    mk.dma("sp", g.H[0:NCTX, :], g.ctx[:, :], reads=[g.ctx], writes=[g.H])
    for j in range(4):
        mk.dma("sp" if j % 2 == 0 else "act", g.H[NCTX + j * 1024:NCTX + (j + 1) * 1024, :],
               g.x[j * 1024:(j + 1) * 1024, :], reads=[g.x], writes=[g.H])


def phase_mod(mk, g, layer):
    nc = mk.nc
    if not hasattr(g, "M_all"):
        g.M_all = mk.sb("M_all", [128, 48, 2], F32)
        g.A1 = mk.sb("A1", [128, 8, 2], F32)
        g.A2 = mk.sb("A2", [128, 8, 2], F32)
    mk.begin_phase()
    c0 = mk.sb("c0", [128, 8], F32)
    c1 = mk.sb("c1", [128, 8], F32)
    sc = mk.sb("sc", [128, 8, 2], F32)
    bcol = mk.sb("bcol", [128, 48], F32)
    gm = mk.sb("gm", [128, 8], F32)
    gf = mk.sb("gf", [128, 8], F32)
    with nc.allow_non_contiguous_dma(reason="tiny column loads"):
        mk.dma("sp", c0[:], g.c[:].rearrange("(j p) -> p j", p=128), reads=[g.c], writes=[c0])
        mk.dma("sp", c1[:], g.c_ctx[:].rearrange("(j p) -> p j", p=128), reads=[g.c_ctx], writes=[c1])
        mk.dma("sp", bcol[:], g.b_mod[layer, :].rearrange("(j p) -> p j", p=128), reads=[g.b_mod], writes=[bcol])
        mk.dma("sp", gm[:], g.norm_mix_g[layer, :].rearrange("(j p) -> p j", p=128), reads=[g.norm_mix_g], writes=[gm])
        mk.dma("sp", gf[:], g.norm_ffn_g[layer, :].rearrange("(j p) -> p j", p=128), reads=[g.norm_ffn_g], writes=[gf])
    mk.op("act", lambda e: e.activation(sc[:, :, 0], c0[:], AF.Silu), reads=[c0], writes=[sc])
    mk.op("act", lambda e: e.activation(sc[:, :, 1], c1[:], AF.Silu), reads=[c1, sc], writes=[sc])
    pm = mk.ps("pm", [128, 256, 2], F32)
    wb = [mk.sb("wmodblk%d" % i, [128, 8, 512], F32) for i in range(2)]
    for cb in range(12):
        w = wb[cb % 2]
        mk.dma("sp" if cb % 2 == 0 else "act", w[:],
               g.w_mod[layer, :, cb * 512:(cb + 1) * 512].rearrange("(kc p) n -> p kc n", p=128),
               reads=[g.w_mod], writes=[w])
        for ft in range(4):
            j = cb * 4 + ft
            for kc in range(8):
                mk.mm(pm[:, j, :], w[:, kc, ft * 128:(ft + 1) * 128], sc[:, kc, :],
                      start=(kc == 0), stop=(kc == 7), reads=[w, sc], writes=[pm])
    for wv in range(2):
        mk.op("dve", lambda e: e.tensor_tensor(g.M_all[:, :, wv], pm[:, 0:48, wv], bcol[:], ALU.add),
              reads=[pm, bcol, g.M_all], writes=[g.M_all])
    for wv in range(2):
        mk.op("dve", lambda e: e.scalar_tensor_tensor(g.A1[:, :, wv], g.M_all[:, 8:16, wv], 1.0, gm[:],
                                                       ALU.add, ALU.mult),
              reads=[g.M_all, gm, g.A1], writes=[g.A1])
        mk.op("dve", lambda e: e.scalar_tensor_tensor(g.A2[:, :, wv], g.M_all[:, 32:40, wv], 1.0, gf[:],
                                                       ALU.add, ALU.mult),
              reads=[g.M_all, gf, g.A2], writes=[g.A2])
    with nc.allow_non_contiguous_dma(reason="small modulation table"):
        for wv in range(2):
            mk.dma("sp", g.MOD[wv, :].rearrange("(j p) -> p j", p=128), g.M_all[:, :, wv], reads=[g.M_all], writes=[g.MOD])
    mk.end_phase()


def norm_tile(mk, g, h_t, xn, ss, rstd, junk):
    mk.op("act", lambda e: e.activation(junk[:], h_t[:], AF.Square, accum_out=ss[:, 0:1]),
          reads=[h_t], writes=[junk, ss])
    mk.op("act", lambda e: e.activation(rstd[:], ss[:], AF.Sqrt, scale=1.0 / D, bias=g.eps_col[:, 0:1]),
          reads=[ss, g.eps_col], writes=[rstd])
    mk.op("dve", lambda e: e.reciprocal(rstd[:], rstd[:]), reads=[rstd], writes=[rstd])
    mk.op("dve", lambda e: e.tensor_scalar(xn[:], h_t[:], rstd[:, 0:1], None, ALU.mult),
          reads=[h_t, rstd], writes=[xn])


def transpose_mod_tile(mk, g, xn, pT, ut, A, Bsh, wv, boff):
    for kc in range(8):
        mk.tr(pT[:, kc, :], xn[:, kc * 128:(kc + 1) * 128], g.identb[:], reads=[xn, g.identb], writes=[pT])
    tmp = Bsh if Bsh is not None else ut
    mk.op("dve", lambda e: e.tensor_tensor(tmp[:], pT[:], A[:, :, wv].unsqueeze(2).broadcast_to([128, 8, 128]), ALU.mult),
          reads=[pT, A], writes=[tmp])
    mk.op("dve", lambda e: e.tensor_tensor(ut[:], tmp[:], g.M_all[:, boff:boff + 8, wv].unsqueeze(2).broadcast_to([128, 8, 128]), ALU.add),
          reads=[tmp, g.M_all, ut], writes=[ut])


def phase_norm1(mk, g, layer):
    mk.begin_phase()
    hts = [mk.sb("h_t%d" % i, [128, D], F32) for i in range(2)]
    xns = [mk.sb("xn%d" % i, [128, D], BF16) for i in range(2)]
    uts = [mk.sb("ut%d" % i, [128, 8, 128], BF16) for i in range(2)]
    pTs = [mk.ps("pT%d" % i, [128, 8, 128], BF16) for i in range(2)]
    junk = mk.sb("junk", [128, D], BF16)
    sss = [mk.sb("ss%d" % i, [128, 1], F32) for i in range(2)]
    rstds = [mk.sb("rstd%d" % i, [128, 1], F32) for i in range(2)]
    for i in range(NT):
        b = i % 2
        wv = 1 if i < 2 else 0
        mk.dma("sp", hts[b][:], g.H[i * 128:(i + 1) * 128, :], reads=[g.H], writes=[hts[b]])
        norm_tile(mk, g, hts[b], xns[b], sss[b], rstds[b], junk)
        transpose_mod_tile(mk, g, xns[b], pTs[b], uts[b], g.A1, None, wv, 0)
        mk.dma("act", g.UT[:, :, i * 128:(i + 1) * 128].rearrange("k p t -> p k t"), uts[b][:],
               reads=[uts[b]], writes=[g.UT])
    mk.end_phase()


def push_scope(mk):
    if not hasattr(mk, "scopes"):
        mk.scopes = []
    mk.scopes.append(mk.pstack)
    mk.pstack = ExitStack()


def pop_scope(mk):
    mk.barrier()
    mk.pstack.close()
    mk.pstack = mk.scopes.pop()


SCRATCH.update({
    "U8D": ([16, 128, 544], F32),
    "Y8D": ([16, 128, 544], F32),
    "YS5T": ([2, 128, T], BF16),
})

TWO_PI = 2.0 * math.pi
S5_OFF = 64.0 * math.pi
S5_TAUS = [float(t) for t in range(-7, 9)] + [8.0 * (2 ** k) for k in range(10)]


def tau_idx(t):
    return int(t) + 7


def s5_tables(mk):
    WZ = mk.sb("s5WZ", [128, 2, 16, 2, 64], F32)
    CO = mk.sb("s5CO", [128, 2, 8, 2, 128], F32)
    TOEP = mk.sb("s5TOEP", [128, 16, 128], F32)
    EPD = mk.sb("s5EPD", [128, 2, 8, 10, 3], F32)
    return WZ, CO, TOEP, EPD


def s5_setup_gen(mk, g, layer, WZ, CO, TOEP, EPD, pz, py):
    nc = mk.nc
    NTAU = len(S5_TAUS)
    lr = mk.sb("lr", [128, 2, 8], F32)
    li = mk.sb("li", [128, 2, 8], F32)
    ls = mk.sb("ls", [128, 2, 8], F32)
    BT = [mk.sb("BT%d" % i, [128, 8, 16], F32) for i in range(2)]
    CT = [mk.sb("CT%d" % i, [128, 8, 16], F32) for i in range(2)]
    with nc.allow_non_contiguous_dma(reason="small s5 parameter tables"):
        for (dst, src) in ((lr, g.s5_lam_re), (li, g.s5_lam_im)):
            for d in range(2):
                mk.dma("pool", dst[:, d, :], src[layer, d].rearrange("(pr hf) p -> (hf p) pr", hf=2), reads=[src], writes=[dst])
        for hf in range(2):
            for d in range(2):
                mk.dma("pool", ls[64 * hf:64 * hf + 64, d, :], g.s5_log_step[layer, d, hf::2].partition_broadcast(64),
                       reads=[g.s5_log_step], writes=[ls])
            for (dst, src) in ((BT[0], g.s5_b_re), (BT[1], g.s5_b_im)):
                mk.dma("pool", dst[64 * hf:64 * hf + 64, :, :],
                       src[layer].rearrange("(pr hf) p h -> hf p pr h", hf=2)[hf], reads=[src], writes=[dst])
            for (dst, src) in ((CT[0], g.s5_c_re), (CT[1], g.s5_c_im)):
                for pr in range(8):
                    mk.dma("pool", dst[64 * hf:64 * hf + 64, pr, :],
                           src[layer, 2 * pr + hf].rearrange("h p -> p h"), reads=[src], writes=[dst])
    dtv = mk.sb("dtv", [128, 16], F32)
    zr = mk.sb("zr", [128, 16], F32)
    zi = mk.sb("zi", [128, 16], F32)
    lrf = lr[:].rearrange("p d r -> p (d r)")
    lif = li[:].rearrange("p d r -> p (d r)")
    mk.op("act", lambda e: e.activation(dtv[:], ls[:].rearrange("p d r -> p (d r)"), AF.Exp), reads=[ls], writes=[dtv])
    mk.op("dve", lambda e: e.tensor_tensor(zr[:], lrf, dtv[:], ALU.mult), reads=[lr, dtv], writes=[zr])
    mk.op("dve", lambda e: e.tensor_tensor(zi[:], lif, dtv[:], ALU.mult), reads=[li, dtv], writes=[zi])
    tau = mk.sb("tau", [128, NTAU], F32)
    for i, tv in enumerate(S5_TAUS):
        mk.op("pool", lambda e: e.memset(tau[:, i:i + 1], tv), reads=[tau], writes=[tau])
    shp = [128, 16, NTAU]
    ARG = mk.sb("ARG", shp, F32)
    MAG = mk.sb("MAG", shp, F32)
    SA = mk.sb("SA", shp, F32)
    CA = mk.sb("CA", shp, F32)
    ER = mk.sb("ER", shp, F32)
    EI = mk.sb("EI", shp, F32)
    taub = tau[:].unsqueeze(1).broadcast_to(shp)
    mk.op("dve", lambda e: e.tensor_tensor(ARG[:], zi[:].unsqueeze(2).broadcast_to(shp), taub, ALU.mult),
          reads=[zi, tau], writes=[ARG])
    mk.op("dve", lambda e: e.tensor_tensor(MAG[:], zr[:].unsqueeze(2).broadcast_to(shp), taub, ALU.mult),
          reads=[zr, tau], writes=[MAG])
    mk.op("act", lambda e: e.activation(MAG[:], MAG[:], AF.Exp), reads=[MAG], writes=[MAG])
    KI = mk.sb("KI", shp, mybir.dt.int32)
    KF = mk.sb("KF", shp, F32)

    def range_reduce(dst, addc):
        mk.op("dve", lambda e: e.tensor_scalar(dst[:], ARG[:], addc, None, ALU.add), reads=[ARG], writes=[dst])
        mk.op("dve", lambda e: e.tensor_scalar(KF[:], dst[:], 1.0 / TWO_PI, None, ALU.mult), reads=[dst], writes=[KF])
        mk.op("dve", lambda e: e.tensor_copy(KI[:], KF[:]), reads=[KF], writes=[KI])
        mk.op("dve", lambda e: e.tensor_copy(KF[:], KI[:]), reads=[KI], writes=[KF])
        mk.op("dve", lambda e: e.scalar_tensor_tensor(dst[:], KF[:], -TWO_PI, dst[:], ALU.mult, ALU.add), reads=[KF, dst], writes=[dst])
        mk.op("dve", lambda e: e.tensor_scalar(KF[:], dst[:], math.pi, TWO_PI, ALU.is_gt, ALU.mult), reads=[dst], writes=[KF])
        mk.op("dve", lambda e: e.tensor_tensor(dst[:], dst[:], KF[:], ALU.subtract), reads=[KF, dst], writes=[dst])
        mk.op("dve", lambda e: e.tensor_scalar(KF[:], dst[:], -math.pi, TWO_PI, ALU.is_lt, ALU.mult), reads=[dst], writes=[KF])
        mk.op("dve", lambda e: e.tensor_tensor(dst[:], dst[:], KF[:], ALU.add), reads=[KF, dst], writes=[dst])
        mk.op("dve", lambda e: e.tensor_scalar(dst[:], dst[:], math.pi, -math.pi, ALU.min, ALU.max), reads=[dst], writes=[dst])

    range_reduce(SA, S5_OFF)
    range_reduce(CA, S5_OFF + 0.5 * math.pi)
    mk.op("act", lambda e: e.activation(SA[:], SA[:], AF.Sin), reads=[SA], writes=[SA])
    mk.op("act", lambda e: e.activation(CA[:], CA[:], AF.Sin), reads=[CA], writes=[CA])
    mk.op("dve", lambda e: e.tensor_tensor(ER[:], MAG[:], CA[:], ALU.mult), reads=[MAG, CA], writes=[ER])
    mk.op("dve", lambda e: e.tensor_tensor(EI[:], MAG[:], SA[:], ALU.mult), reads=[MAG, SA], writes=[EI])
    EPDv = EPD[:].rearrange("p d r k c -> p (d r) k c")
    mk.op("dve", lambda e: e.tensor_copy(EPDv[:, :, :, 0], ER[:, :, 16:26]), reads=[ER, EPD], writes=[EPD])
    mk.op("dve", lambda e: e.tensor_copy(EPDv[:, :, :, 1], EI[:, :, 16:26]), reads=[EI, EPD], writes=[EPD])
    mk.op("dve", lambda e: e.tensor_scalar(EPDv[:, :, :, 2], EI[:, :, 16:26], -1.0, None, ALU.mult), reads=[EI, EPD], writes=[EPD])
    i1 = tau_idx(1)
    nr = mk.sb("nr", [128, 16], F32)
    den = mk.sb("den", [128, 16], F32)
    t1 = mk.sb("t1", [128, 16], F32)
    t2 = mk.sb("t2", [128, 16], F32)
    fr = mk.sb("fr", [128, 16], F32)
    fi = mk.sb("fi", [128, 16], F32)
    mk.op("dve", lambda e: e.tensor_scalar(nr[:], ER[:, :, i1], -1.0, None, ALU.add), reads=[ER], writes=[nr])
    mk.op("dve", lambda e: e.tensor_tensor(den[:], lrf, lrf, ALU.mult), reads=[lr], writes=[den])
    mk.op("dve", lambda e: e.tensor_tensor(t1[:], lif, lif, ALU.mult), reads=[li], writes=[t1])
    mk.op("dve", lambda e: e.tensor_tensor(den[:], den[:], t1[:], ALU.add), reads=[den, t1], writes=[den])
    mk.op("dve", lambda e: e.reciprocal(den[:], den[:]), reads=[den], writes=[den])
    mk.op("dve", lambda e: e.tensor_tensor(t1[:], nr[:], lrf, ALU.mult), reads=[nr, lr], writes=[t1])
    mk.op("dve", lambda e: e.tensor_tensor(t2[:], EI[:, :, i1], lif, ALU.mult), reads=[EI, li], writes=[t2])
    mk.op("dve", lambda e: e.tensor_tensor(t1[:], t1[:], t2[:], ALU.add), reads=[t1, t2], writes=[t1])
    mk.op("dve", lambda e: e.tensor_tensor(fr[:], t1[:], den[:], ALU.mult), reads=[t1, den], writes=[fr])
    mk.op("dve", lambda e: e.tensor_tensor(t1[:], EI[:, :, i1], lrf, ALU.mult), reads=[EI, lr], writes=[t1])
    mk.op("dve", lambda e: e.tensor_tensor(t2[:], nr[:], lif, ALU.mult), reads=[nr, li], writes=[t2])
    mk.op("dve", lambda e: e.tensor_tensor(t1[:], t1[:], t2[:], ALU.subtract), reads=[t1, t2], writes=[t1])
    mk.op("dve", lambda e: e.tensor_tensor(fi[:], t1[:], den[:], ALU.mult), reads=[t1, den], writes=[fi])
    BBR = mk.sb("BBR", [128, 2, 8, 16], F32)
    BBI = mk.sb("BBI", [128, 2, 8, 16], F32)
    tb1 = mk.sb("tb1", [128, 8, 16], F32)
    tb2 = mk.sb("tb2", [128, 8, 16], F32)
    for d in range(2):
        frb = fr[:, d * 8:(d + 1) * 8].unsqueeze(2).broadcast_to([128, 8, 16])
        fib = fi[:, d * 8:(d + 1) * 8].unsqueeze(2).broadcast_to([128, 8, 16])
        mk.op("dve", lambda e: e.tensor_tensor(tb1[:], BT[0][:], frb, ALU.mult), reads=[BT[0], fr], writes=[tb1])
        mk.op("dve", lambda e: e.tensor_tensor(tb2[:], BT[1][:], fib, ALU.mult), reads=[BT[1], fi], writes=[tb2])
        mk.op("dve", lambda e: e.tensor_tensor(BBR[:, d], tb1[:], tb2[:], ALU.subtract), reads=[tb1, tb2, BBR], writes=[BBR])
        mk.op("dve", lambda e: e.tensor_tensor(tb1[:], BT[1][:], frb, ALU.mult), reads=[BT[1], fr], writes=[tb1])
        mk.op("dve", lambda e: e.tensor_tensor(tb2[:], BT[0][:], fib, ALU.mult), reads=[BT[0], fi], writes=[tb2])
        mk.op("dve", lambda e: e.tensor_tensor(BBI[:, d], tb1[:], tb2[:], ALU.add), reads=[tb1, tb2, BBI], writes=[BBI])
    WZT = mk.sb("WZT", [128, 2, 8, 2, 8, 16], F32)
    COM = mk.sb("COM", [128, 2, 8, 2, 8, 16], F32)
    COv = CO[:].rearrange("p d r i (t h) -> p d r i t h", h=16)
    ta = mk.sb("ta", [128, 8, 16], F32)
    tb = mk.sb("tb", [128, 8, 16], F32)
    sh3 = [128, 8, 16]

    def cplx(dst_re, dst_im, xr, xi, er, ei, conj_im, dsts):
        mk.op("dve", lambda e: e.tensor_tensor(ta[:], xr, er, ALU.mult), reads=[BBR, BBI, CT[0], CT[1], ER, EI], writes=[ta])
        mk.op("dve", lambda e: e.tensor_tensor(tb[:], xi, ei, ALU.mult), reads=[BBR, BBI, CT[0], CT[1], ER, EI], writes=[tb])
        mk.op("dve", lambda e: e.tensor_tensor(dst_re, ta[:], tb[:], ALU.subtract), reads=[ta, tb] + dsts, writes=dsts)
        mk.op("dve", lambda e: e.tensor_tensor(ta[:], xr, ei, ALU.mult), reads=[BBR, BBI, CT[0], CT[1], ER, EI], writes=[ta])
        mk.op("dve", lambda e: e.tensor_tensor(tb[:], xi, er, ALU.mult), reads=[BBR, BBI, CT[0], CT[1], ER, EI], writes=[tb])
        if conj_im:
            mk.op("dve", lambda e: e.scalar_tensor_tensor(dst_im, ta[:], -1.0, tb[:], ALU.mult, ALU.subtract),
                  reads=[ta, tb] + dsts, writes=dsts)
        else:
            mk.op("dve", lambda e: e.tensor_tensor(dst_im, ta[:], tb[:], ALU.add), reads=[ta, tb] + dsts, writes=dsts)

    for d in range(2):
        for pr in range(8):
            col = d * 8 + pr
            if d == 0:
                e_wz = slice(tau_idx(7), tau_idx(0) - 1 if tau_idx(0) - 1 >= 0 else None, -1)
                e_co = slice(tau_idx(1), tau_idx(8) + 1)
                e_cm = slice(tau_idx(-7), tau_idx(0) + 1)
            else:
                e_wz = slice(tau_idx(0), tau_idx(7) + 1)
                e_co = slice(tau_idx(8), tau_idx(1) - 1, -1)
                e_cm = slice(tau_idx(0), tau_idx(-7) - 1 if tau_idx(-7) - 1 >= 0 else None, -1)

            def eb(tab, sl):
                return tab[:, col, sl].unsqueeze(2).broadcast_to(sh3)

            xbr = BBR[:, d, pr, :].unsqueeze(1).broadcast_to(sh3)
            xbi = BBI[:, d, pr, :].unsqueeze(1).broadcast_to(sh3)
            cplx(WZT[:, d, pr, 0], WZT[:, d, pr, 1], xbr, xbi, eb(ER, e_wz), eb(EI, e_wz), False, [WZT])
            xcr = CT[0][:, pr, :].unsqueeze(1).broadcast_to(sh3)
            xci = CT[1][:, pr, :].unsqueeze(1).broadcast_to(sh3)
            cplx(COv[:, d, pr, 0], COv[:, d, pr, 1], xcr, xci, eb(ER, e_co), eb(EI, e_co), True, [CO])
            cplx(COM[:, d, pr, 0], COM[:, d, pr, 1], xcr, xci, eb(ER, e_cm), eb(EI, e_cm), True, [COM])
            yield
    maskF = mk.sb("maskF", [128, 8, 16], F32)
    maskB = mk.sb("maskB", [128, 8, 16], F32)
    mk.op("pool", lambda e: e.memset(maskF[:], 1.0), writes=[maskF])
    mk.op("pool", lambda e: e.memset(maskB[:], 1.0), writes=[maskB])
    mk.op("pool", lambda e: e.affine_select(out=maskF[:], in_=maskF[:], pattern=[[16, 8], [0, 16]],
                                             compare_op=ALU.is_ge, fill=0.0, base=15, channel_multiplier=-1),
          reads=[maskF], writes=[maskF])
    mk.op("pool", lambda e: e.affine_select(out=maskB[:], in_=maskB[:], pattern=[[-16, 8], [0, 16]],
                                             compare_op=ALU.is_ge, fill=0.0, base=0, channel_multiplier=1),
          reads=[maskB], writes=[maskB])
    mF = maskF[:].rearrange("p t h -> p (t h)")
    mB = maskB[:].rearrange("p t h -> p (t h)")
    ttmp = mk.sb("ttmp", [128, 128], F32)
    for gg in range(16):
        pr, hf = gg // 2, gg % 2
        rows = slice(64 * hf, 64 * hf + 64)
        pyb = py[gg % 2]
        for d in range(2):
            for ri in range(2):
                mk.op("pe", lambda e: e.transpose(pz[gg % 2][:, d, ri * 64:(ri + 1) * 64],
                                                    WZT[rows, d, pr, ri].rearrange("p s h -> p (s h)"),
                                                    g.identf[rows, rows]),
                      reads=[WZT, g.identf], writes=[pz[gg % 2]])
                mk.mm(pyb[:, d, 0:128], WZT[rows, d, pr, ri].rearrange("p s h -> p (s h)"),
                      COM[rows, d, pr, ri].rearrange("p t h -> p (t h)"), start=(ri == 0), stop=(ri == 1),
                      reads=[WZT, COM], writes=[pyb])
        mk.op("act", lambda e: e.activation(WZ[:, :, gg].rearrange("p d i c -> p d (i c)"), pz[gg % 2][:, :, 0:128], AF.Copy),
              reads=[pz[gg % 2], WZ], writes=[WZ])
        mk.op("dve", lambda e: e.tensor_tensor(ttmp[:], pyb[:, 0, 0:128], mF, ALU.mult), reads=[pyb, maskF], writes=[ttmp])
        mk.op("dve", lambda e: e.tensor_tensor(TOEP[:, gg, :], pyb[:, 1, 0:128], mB, ALU.mult), reads=[pyb, maskB, TOEP], writes=[TOEP])
        mk.op("dve", lambda e: e.tensor_tensor(TOEP[:, gg, :], TOEP[:, gg, :], ttmp[:], ALU.add), reads=[TOEP, ttmp], writes=[TOEP])
        yield


def s5_main(mk, g, layer, WZ, CO, TOEP, EPD, pz, py):
    nc = mk.nc
    UR = mk.sb("UR", [128, 2, 8, 544], F32)
    U8 = mk.sb("U8", [128, 16, 544], F32)
    Ws5 = mk.sb("Ws5", [128, 8, 256], BF16)
    mk.dma("pool", Ws5[:], g.w_in[layer, :, C_S5:C_S5 + 256].rearrange("(kc p) n -> p kc n", p=128),
           reads=[g.w_in], writes=[Ws5])
    ubs = [mk.sb("ub%d" % i, [128, 8, 512], BF16) for i in range(2)]
    nblk = 9
    for blk in range(nblk):
        N = 512 if blk < 8 else 256
        t0 = blk * 512
        ub = ubs[blk % 2]
        mk.dma("sp", ub[:, :, 0:N], g.UT[:, :, t0:t0 + N].rearrange("k p t -> p k t"), reads=[g.UT], writes=[ub])
        for mt in range(2):
            pp = pz[mt]
            for kc in range(8):
                mk.mm(pp[:, 0, 0:N], Ws5[:, kc, mt * 128:(mt + 1) * 128], ub[:, kc, 0:N],
                      start=(kc == 0), stop=(kc == 7), reads=[Ws5, ub], writes=[pp])
            c0 = blk * 64
            ncn = N // 8
            mk.op("act" if mt == 0 else "dve",
                  (lambda e: e.activation(UR[:, mt, :, c0:c0 + ncn].rearrange("p s c -> p c s"),
                                          pp[:, 0, 0:N].rearrange("p (c s) -> p c s", s=8), AF.Copy)) if mt == 0 else
                  (lambda e: e.tensor_copy(UR[:, mt, :, c0:c0 + ncn].rearrange("p s c -> p c s"),
                                           pp[:, 0, 0:N].rearrange("p (c s) -> p c s", s=8))),
                  reads=[pp, UR], writes=[UR])
    for gg in range(16):
        mt, gl = gg // 8, gg % 8
        mk.dma("sp" if gg % 2 == 0 else "pool", g.U8D[gg].rearrange("(s h) c -> h s c", h=16),
               UR[16 * gl:16 * gl + 16, mt, :, :], reads=[UR], writes=[g.U8D])
    mk.dma("sp", U8[:], g.U8D[:, :, :].rearrange("g p c -> p g c"), reads=[g.U8D], writes=[U8])

    ZSs = [[mk.sb("ZS%d_%d" % (i, st_), [128, 2, 544], F32) for i in range(2)] for st_ in range(2)]
    ZQs = [[mk.sb("ZQ%d_%d" % (i, st_), [128, 2, 544], F32) for i in range(2)] for st_ in range(2)]
    XS = [mk.sb("XS%d" % i, [128, 2, 545], F32) for i in range(2)]
    ystage = [mk.sb("yst%d" % i, [128, 544], F32) for i in range(2)]
    dtmp = mk.sb("dtmp", [128, 544], F32)
    mk.op("pool", lambda e: e.memset(XS[0][:], 0.0), writes=[XS[0]])
    mk.op("pool", lambda e: e.memset(XS[1][:], 0.0), writes=[XS[1]])
    SEGS = [(0, 32, 0, 256), (32, 256, 0, 0), (288, 256, 1, 0)]
    nzp_box = [0]

    def zstage(pr):
        ZS = ZSs[pr % 2]
        nzp = nzp_box[0]
        for d in range(2):
            for ri in range(2):
                pzz = pz[nzp % 2]
                nzp += 1
                for hf in range(2):
                    gg = 2 * pr + hf
                    for (c0, n, bk, pc) in SEGS:
                        mk.mm(pzz[64 * hf:64 * hf + 64, bk, pc:pc + n], WZ[:, d, gg, ri, :], U8[:, gg, c0:c0 + n],
                              start=True, stop=True, reads=[WZ, U8], writes=[pzz])
                if d == 0:
                    mk.op("act", lambda e: e.activation(ZS[d][:, ri, 0:32], pzz[:, 0, 256:288], AF.Copy), reads=[pzz, ZS[d]], writes=[ZS[d]])
                    mk.op("act", lambda e: e.activation(ZS[d][:, ri, 32:544].rearrange("p (b c) -> p b c", b=2), pzz[:, :, 0:256], AF.Copy),
                          reads=[pzz, ZS[d]], writes=[ZS[d]])
                else:
                    mk.op("act", lambda e: e.activation(ZS[d][:, ri, 512:544], pzz[:, 0, 256:288], AF.Copy), reads=[pzz, ZS[d]], writes=[ZS[d]])
                    mk.op("act", lambda e: e.activation(ZS[d][:, ri, 0:512].rearrange("p (b c) -> p b c", b=2), pzz[:, :, 0:256], AF.Copy),
                          reads=[pzz, ZS[d]], writes=[ZS[d]])

        nzp_box[0] = nzp

    def rest_stage(pr):
        ZS = ZSs[pr % 2]
        ZQ = ZQs[pr % 2]
        PQ = [[ZS[0], ZQ[0]], [ZS[1], ZQ[1]]]
        KP = {id(b): mk.sub(b, "keep") for b in (ZS[0], ZQ[0], ZS[1], ZQ[1])}
        for k in range(10):
            s = 2 ** k
            for d in range(2):
                P, Q = PQ[d]
                ar = EPD[:, d, pr, k, 0:1]
                ai = EPD[:, d, pr, k, 1:2]
                nai = EPD[:, d, pr, k, 2:3]
                if d == 0:
                    dst = slice(s, 544); src = slice(0, 544 - s); keep = slice(0, s)
                else:
                    dst = slice(0, 544 - s); src = slice(s, 544); keep = slice(544 - s, 544)
                for (qo, pi_, sc_, base, bb) in ((0, 0, ar, P, 0), (1, 1, ar, P, 1)):
                    mk.op("dve", lambda e: e.scalar_tensor_tensor(Q[:, qo, dst], P[:, pi_, src], sc_, base[:, bb, dst], ALU.mult, ALU.add),
                          reads=[P, KP[id(P)], EPD, Q], writes=[Q])
                mk.op("act", lambda e: e.activation(Q[:, :, keep], P[:, :, keep], AF.Copy), reads=[P, KP[id(P)], KP[id(Q)]], writes=[KP[id(Q)]])
            for d in range(2):
                P, Q = PQ[d]
                ai = EPD[:, d, pr, k, 1:2]
                nai = EPD[:, d, pr, k, 2:3]
                if d == 0:
                    dst = slice(s, 544); src = slice(0, 544 - s)
                else:
                    dst = slice(0, 544 - s); src = slice(s, 544)
                for (qo, pi_, sc_) in ((0, 1, nai), (1, 0, ai)):
                    mk.op("dve", lambda e: e.scalar_tensor_tensor(Q[:, qo, dst], P[:, pi_, src], sc_, Q[:, qo, dst], ALU.mult, ALU.add),
                          reads=[P, KP[id(P)], EPD, Q], writes=[Q])
                PQ[d] = [Q, P]
        for d in range(2):
            P = PQ[d][0]
            if d == 0:
                mk.op("act", lambda e: e.activation(XS[d][:, :, 1:545], P[:, :, :], AF.Copy), reads=[P, KP[id(P)], XS[d]], writes=[XS[d]])
            else:
                mk.op("act", lambda e: e.activation(XS[d][:, :, 0:544], P[:, :, :], AF.Copy), reads=[P, KP[id(P)], XS[d]], writes=[XS[d]])
        for hf in range(2):
            gg = 2 * pr + hf
            rows = slice(64 * hf, 64 * hf + 64)
            pyy = py[gg % 2]
            for (c0, n, bk, pc) in SEGS:
                out = pyy[:, bk, pc:pc + n]
                mk.mm(out, TOEP[:, gg, :], U8[:, gg, c0:c0 + n], start=True, stop=False, reads=[TOEP, U8], writes=[pyy])
                for ri in range(2):
                    mk.mm(out, CO[rows, 0, pr, ri, :], XS[0][rows, ri, c0:c0 + n], start=False, stop=False,
                          reads=[CO, XS[0]], writes=[pyy])
                j0 = (c0 - 32) if c0 >= 32 else (512 + c0)
                for ri in range(2):
                    mk.mm(out, CO[rows, 1, pr, ri, :], XS[1][rows, ri, j0 + 1:j0 + 1 + n], start=False, stop=(ri == 1),
                          reads=[CO, XS[1]], writes=[pyy])
            yst = ystage[gg % 2]
            mk.op("act", lambda e: e.activation(yst[:, 0:32], pyy[:, 0, 256:288], AF.Copy), reads=[pyy, yst], writes=[yst])
            mk.op("act", lambda e: e.activation(yst[:, 32:544].rearrange("p (b c) -> p b c", b=2), pyy[:, :, 0:256], AF.Copy),
                  reads=[pyy, yst], writes=[yst])
            mk.dma("sp", g.Y8D[gg], yst[:], reads=[yst], writes=[g.Y8D])

    zstage(0)
    for pr in range(8):
        if pr + 1 < 8:
            zstage(pr + 1)
        rest_stage(pr)
    YT = U8
    YTv = U8[:].rearrange("p (m t) c -> p m t c", m=2)
    for gg in range(16):
        mt, gl = gg // 8, gg % 8
        mk.dma("sp" if gg % 2 == 0 else "pool", YTv[16 * gl:16 * gl + 16, mt, :, :],
               g.Y8D[gg].rearrange("(t h) c -> h t c", h=16), reads=[g.Y8D, U8], writes=[U8])
    dcol = mk.sb("dcol", [128, 2], F32)
    bglu = mk.sb("bglu", [128, 2], F32)
    Wglu = mk.sb("Wglu", [128, 2, 256], BF16)
    with nc.allow_non_contiguous_dma(reason="small columns"):
        mk.dma("sp", dcol[:], g.s5_d[layer].rearrange("(m p) -> p m", p=128), reads=[g.s5_d], writes=[dcol])
        mk.dma("sp", bglu[:], g.s5_glu_b[layer].rearrange("(m p) -> p m", p=128), reads=[g.s5_glu_b], writes=[bglu])
    mk.dma("pool", Wglu[:], g.s5_glu_w[layer].rearrange("(m p) n -> p m n", p=128), reads=[g.s5_glu_w], writes=[Wglu])
    y1 = mk.sb("y1", [128, 2, 512], F32)
    sq = mk.sb("sq", [128, 2, 512], F32)
    yg = mk.sb("yg", [128, 2, 512], F32)
    ygb = mk.sb("ygb", [128, 2, 512], BF16)
    sig = mk.sb("sig", [128, 512], F32)
    yos = [mk.sb("yo%d" % i, [128, 2, 512], BF16) for i in range(2)]
    for blk in range(nblk):
        N = 512 if blk < 8 else 256
        t0 = blk * 512
        c0 = blk * 64
        ncn = N // 8
        yo = yos[blk % 2]
        for mt in range(2):
            uv = UR[:, mt, :, c0:c0 + ncn].rearrange("p s c -> p c s")
            yv = YTv[:, mt, :, c0:c0 + ncn].rearrange("p t c -> p c t")
            mk.op("dve", lambda e: e.scalar_tensor_tensor(y1[:, mt, 0:N].rearrange("p (c s) -> p c s", s=8), uv,
                                                           dcol[:, mt:mt + 1], yv, ALU.mult, ALU.add),
                  reads=[UR, U8, dcol, y1], writes=[y1])
        a = y1[:, :, 0:N]
        mk.op("act", lambda e: e.activation(sq[:, :, 0:N], a, AF.Square, scale=math.sqrt(0.044715)), reads=[y1], writes=[sq])
        mk.op("dve", lambda e: e.scalar_tensor_tensor(sq[:, :, 0:N], sq[:, :, 0:N], 1.0, a, ALU.add, ALU.mult), reads=[sq, y1], writes=[sq])
        mk.op("act", lambda e: e.activation(sq[:, :, 0:N], sq[:, :, 0:N], AF.Sigmoid, scale=1.5957691216057308), reads=[sq], writes=[sq])
        mk.op("dve", lambda e: e.tensor_tensor(yg[:, :, 0:N], sq[:, :, 0:N], a, ALU.mult), reads=[sq, y1], writes=[yg])
        mk.op("act", lambda e: e.activation(ygb[:, :, 0:N], yg[:, :, 0:N], AF.Copy), reads=[yg], writes=[ygb])
        for mo in range(2):
            pp = pz[mo]
            for mi in range(2):
                mk.mm(pp[:, 0, 0:N], Wglu[:, mi, mo * 128:(mo + 1) * 128], ygb[:, mi, 0:N], start=(mi == 0), stop=(mi == 1),
                      reads=[Wglu, ygb], writes=[pp])
            mk.op("act", lambda e: e.activation(sig[:, 0:N], pp[:, 0, 0:N], AF.Sigmoid, bias=bglu[:, mo:mo + 1]),
                  reads=[pp, bglu], writes=[sig])
            mk.op("dve", lambda e: e.tensor_tensor(yo[:, mo, 0:N], yg[:, mo, 0:N], sig[:, 0:N], ALU.mult),
                  reads=[yg, sig, yo], writes=[yo])
        mk.dma("sp", g.YS5T[:, :, t0:t0 + N].rearrange("m p t -> p m t"), yo[:, :, 0:N], reads=[yo], writes=[g.YS5T])


def run_gens(gens):
    alive = [True] * len(gens)
    while any(alive):
        for i_, gi in enumerate(gens):
            if alive[i_]:
                try:
                    next(gi)
                except StopIteration:
                    alive[i_] = False


def phase_s5(mk, g, layer):
    mk.begin_phase()
    WZ, CO, TOEP, EPD = s5_tables(mk)
    pz = [mk.ps("s5pz%d" % i, [128, 2, 512], F32) for i in range(2)]
    py = [mk.ps("s5py%d" % i, [128, 2, 512], F32) for i in range(2)]
    push_scope(mk)
    run_gens([s5_setup_gen(mk, g, layer, WZ, CO, TOEP, EPD, pz, py)])
    pop_scope(mk)
    push_scope(mk)
    s5_main(mk, g, layer, WZ, CO, TOEP, EPD, pz, py)
    pop_scope(mk)
    mk.end_phase()


def norm_chain(mk, g, scan):
    sfx = "_s" if scan else "_n"
    dst = g.UTH if scan else g.UT
    hts = [mk.sb("h_t%d" % i + sfx, [128, D], F32) for i in range(2)]
    xns = [mk.sb("xn%d" % i + sfx, [128, D], BF16) for i in range(2)]
    uts = [mk.sb("ut%d" % i + sfx, [128, 8, 128], BF16) for i in range(2)]
    pTs = [mk.ps("pT%d" % i + sfx, [128, 8, 128], BF16) for i in range(2)]
    junk = mk.sb("junk" + sfx, [128, D], BF16)
    utf = mk.sb("utf" + sfx, [128, 8, 128], F32)
    sss = [mk.sb("ss%d" % i + sfx, [128, 1], F32) for i in range(2)]
    rstds = [mk.sb("rstd%d" % i + sfx, [128, 1], F32) for i in range(2)]
    q1, q2 = ("sp", "pool")
    for i in range(NT):
        b = i % 2
        wv = 1 if i < 2 else 0
        if not scan:
            mk.dma(q1, hts[b][:], g.H[i * 128:(i + 1) * 128, :], reads=[g.H], writes=[hts[b]])
        else:
            po = 0
            for (r0, rs, n) in scan_rows(i):
                src = g.H[r0:r0 + n, :] if rs == 1 else g.H[r0:r0 + (n - 1) * rs + 1:rs, :]
                mk.dma(q1, hts[b][po:po + n, :], src, reads=[g.H], writes=[hts[b]])
                po += n
        norm_tile(mk, g, hts[b], xns[b], sss[b], rstds[b], junk)
        yield
        transpose_mod_tile(mk, g, xns[b], pTs[b], uts[b], g.A1, utf, wv, 0)
        mk.dma(q2, dst[:, :, i * 128:(i + 1) * 128].rearrange("k p t -> p k t"), uts[b][:],
               reads=[uts[b]], writes=[dst])
        yield


def phase_norm_s5(mk, g, layer):
    mk.begin_phase()
    WZ, CO, TOEP, EPD = s5_tables(mk)
    push_scope(mk)
    pzs = mk.ps("s5pzs", [128, 2, 512], F32)
    pys = mk.ps("s5pys", [128, 2, 512], F32)
    run_gens([norm_chain(mk, g, False), norm_chain(mk, g, True),
              s5_setup_gen(mk, g, layer, WZ, CO, TOEP, EPD, [pzs, pzs], [pys, pys])])
    pop_scope(mk)
    push_scope(mk)
    pz = [mk.ps("s5pz%d" % i, [128, 2, 512], F32) for i in range(2)]
    py = [mk.ps("s5py%d" % i, [128, 2, 512], F32) for i in range(2)]
    s5_main(mk, g, layer, WZ, CO, TOEP, EPD, pz, py)
    pop_scope(mk)
    mk.end_phase()


SCRATCH.update({
    "UTH": ([8, 128, T], BF16),
    "OFG": ([4, 128, T], F32),
    "OFH": ([2, 128, T], F32),
    "YGLAT": ([4, 128, T], BF16),
    "YHG": ([T, 256], BF16),
})


def scan_rows(tile_idx):
    if tile_idx < 2:
        return [(tile_idx * 128, 1, 128)]
    c0 = (tile_idx - 2) * 2
    return [(NCTX + c0, 64, 64), (NCTX + c0 + 1, 64, 64)]


def phase_norm1_scan(mk, g, layer):
    mk.begin_phase()
    hts = [mk.sb("h_t%d" % i, [128, D], F32) for i in range(2)]
    xns = [mk.sb("xn%d" % i, [128, D], BF16) for i in range(2)]
    uts = [mk.sb("ut%d" % i, [128, 8, 128], BF16) for i in range(2)]
    pTs = [mk.ps("pT%d" % i, [128, 8, 128], BF16) for i in range(2)]
    junk = mk.sb("junk", [128, D], BF16)
    sss = [mk.sb("ss%d" % i, [128, 1], F32) for i in range(2)]
    rstds = [mk.sb("rstd%d" % i, [128, 1], F32) for i in range(2)]
    for i in range(NT):
        b = i % 2
        wv = 1 if i < 2 else 0
        po = 0
        for (r0, rs, n) in scan_rows(i):
            if rs == 1:
                src = g.H[r0:r0 + n, :]
            else:
                src = g.H[r0:r0 + (n - 1) * rs + 1:rs, :]
            mk.dma("sp", hts[b][po:po + n, :], src, reads=[g.H], writes=[hts[b]])
            po += n
        norm_tile(mk, g, hts[b], xns[b], sss[b], rstds[b], junk)
        transpose_mod_tile(mk, g, xns[b], pTs[b], uts[b], g.A1, None, wv, 0)
        mk.dma("act", g.UTH[:, :, i * 128:(i + 1) * 128].rearrange("k p t -> p k t"), uts[b][:],
               reads=[uts[b]], writes=[g.UTH])
    mk.end_phase()


def phase_linattn(mk, g, layer, kind):
    nc = mk.nc
    gla = (kind == "gla")
    DV = 128 if gla else 64
    VW = 4 * DV
    esc = (-1.0 / 16.0) if gla else 1.0
    qscale = 0.125 if gla else 1.0
    UTsrc = g.UT if gla else g.UTH
    OF = g.OFG if gla else g.OFH
    NPO = 4 if gla else 2
    cq, ck, cv = (C_GQ, C_GK, C_GV) if gla else (C_HQ, None, C_HI)
    mk.begin_phase()
    ones32 = mk.sb("ones32", [128, 32], F32)
    mk.op("pool", lambda e: e.memset(ones32[:], 1.0), writes=[ones32])
    masks = []
    for d in range(2):
        m = mk.sb("mask%d" % d, [128, 128], F32)
        mk.op("pool", lambda e: e.memset(m[:], 1.0), writes=[m])
        if d == 0:
            mk.op("pool", lambda e: e.affine_select(out=m[:], in_=m[:], pattern=[[1, 128]], compare_op=ALU.is_ge,
                                                     fill=0.0, base=0, channel_multiplier=-1), reads=[m], writes=[m])
        else:
            mk.op("pool", lambda e: e.affine_select(out=m[:], in_=m[:], pattern=[[-1, 128]], compare_op=ALU.is_ge,
                                                     fill=0.0, base=0, channel_multiplier=1), reads=[m], writes=[m])
        for c in range(4):
            cs = slice(32 * c, 32 * c + 32)
            if d == 0:
                mk.op("pool", lambda e: e.affine_select(out=m[:, cs], in_=m[:, cs], pattern=[[0, 32]], compare_op=ALU.is_ge,
                                                         fill=0.0, base=-32 * c, channel_multiplier=1), reads=[m], writes=[m])
            else:
                mk.op("pool", lambda e: e.affine_select(out=m[:, cs], in_=m[:, cs], pattern=[[0, 32]], compare_op=ALU.is_ge,
                                                         fill=0.0, base=32 * c + 31, channel_multiplier=-1), reads=[m], writes=[m])
        masks.append(m)
    rowmask = mk.sb("rowmask", [128, 4], F32)
    mk.op("pool", lambda e: e.memset(rowmask[:], 1.0), writes=[rowmask])
    for c in range(4):
        mk.op("pool", lambda e: e.affine_select(out=rowmask[:, c:c + 1], in_=rowmask[:, c:c + 1], pattern=[[0, 1]],
                                                 compare_op=ALU.is_ge, fill=0.0, base=-32 * c, channel_multiplier=1),
              reads=[rowmask], writes=[rowmask])
        mk.op("pool", lambda e: e.affine_select(out=rowmask[:, c:c + 1], in_=rowmask[:, c:c + 1], pattern=[[0, 1]],
                                                 compare_op=ALU.is_ge, fill=0.0, base=32 * c + 31, channel_multiplier=-1),
              reads=[rowmask], writes=[rowmask])
    def loadw(name, c0, n):
        w = mk.sb(name, [128, 8, n], BF16)
        mk.dma("pool", w[:], g.w_in[layer, :, c0:c0 + n].rearrange("(kc p) n -> p kc n", p=128), reads=[g.w_in], writes=[w])
        return w
    Wq = loadw("Wq", cq, 256)
    Wv = loadw("Wv", cv, VW)
    if gla:
        Wk = loadw("Wk", ck, 256)
        Wr = loadw("Wr", C_GR, 512)
        Wdec = [loadw("Wc%d" % d, C_GC + 16 * d, 16) for d in range(2)]
        GU = [mk.sb("GU%d" % d, [16, 256], F32) for d in range(2)]
        nbcol = mk.sb("nbcol", [128, 2, 2], F32)
        gn = mk.sb("gncol", [128, 1], F32)
        with nc.allow_non_contiguous_dma(reason="small"):
            for d in range(2):
                mk.dma("sp", GU[d][:], g.gla_gate_up[layer, d], reads=[g.gla_gate_up], writes=[GU[d]])
                mk.dma("sp", nbcol[:, d, :], g.gla_gate_b[layer, d].rearrange("(m p) -> p m", p=128), reads=[g.gla_gate_b], writes=[nbcol])
            mk.dma("sp", gn[:], g.gla_norm_g[layer].rearrange("(p o) -> p o", o=1), reads=[g.gla_norm_g], writes=[gn])
        mk.op("dve", lambda e: e.tensor_scalar(nbcol[:], nbcol[:], -1.0, None, ALU.mult), reads=[nbcol], writes=[nbcol])
        ones_bf = mk.sb("ones_bf", [128, 128], BF16)
        mk.op("pool", lambda e: e.memset(ones_bf[:], 1.0), writes=[ones_bf])
    else:
        Wdec = [loadw("Wf%d" % d, C_HF + 256 * d, 256) for d in range(2)]
        Wog = loadw("Wog", C_HO, 256)
        lbc = mk.sb("lbc", [128, 2], F32)
        omlb = mk.sb("omlb", [128, 2], F32)
        l0 = mk.sb("l0", [128, 2], F32)
        gnrow = mk.sb("gnrow", [128, 4, 64], F32)
        with nc.allow_non_contiguous_dma(reason="small"):
            mk.dma("sp", lbc[:], g.hgrn_lower[1].rearrange("(m p) -> p m", p=128), reads=[g.hgrn_lower], writes=[lbc])
            mk.dma("sp", l0[:], g.hgrn_lower[0].rearrange("(m p) -> p m", p=128), reads=[g.hgrn_lower], writes=[l0])
            for h in range(4):
                mk.dma("sp", gnrow[:, h, :], g.hgrn_norm_g[layer].partition_broadcast(128), reads=[g.hgrn_norm_g], writes=[gnrow])
        if layer == 0:
            mk.op("dve", lambda e: e.memset(lbc[:], 0.0), reads=[lbc], writes=[lbc])
        else:
            mk.op("dve", lambda e: e.tensor_tensor(lbc[:], lbc[:], l0[:], ALU.subtract), reads=[lbc, l0], writes=[lbc])
            mk.op("act", lambda e: e.activation(lbc[:], lbc[:], AF.Sigmoid), reads=[lbc], writes=[lbc])
        mk.op("dve", lambda e: e.tensor_scalar(omlb[:], lbc[:], -1.0, 1.0, ALU.mult, ALU.add), reads=[lbc], writes=[omlb])

    pproj = [mk.ps("pproj%d" % i, [128, 512], F32) for i in range(2)]
    psc = mk.ps("psc", [128, 2, 512], F32)
    po = mk.ps("po", [128, 2, 512], F32)
    pds = mk.ps("pds", [128, 2, 256], F32)
    ptr = mk.ps("ptr", [128, 1024], BF16)
    pss = Buf(pds.t, "pss", psum=True)
    pss = pds
    pssv = pds[:].rearrange("p a b -> p (a b)")
    npj = [0]

    def nextp():
        npj[0] += 1
        return pproj[npj[0] % 2]

    ubs = [mk.sb("ub%d" % i, [128, 8, 512], BF16) for i in range(2)]
    qT = mk.sb("qT", [128, 2, 512], F32)
    kT = mk.sb("kT", [128, 2, 512], F32)
    LG = mk.sb("LG", [128, 2, 512], F32)
    vtok = mk.sb("vtok", [128, 4, VW], BF16)
    if gla:
        codeT = mk.sb("codeT", [16, 512], F32)
        rsil = mk.sb("rsil", [128, 4, 512], F32)
    else:
        sg = mk.sb("sg", [128, 2, 512], F32)
        ogs = mk.sb("ogs", [128, 4, 256], F32)
    cum = mk.sb("cum", [128, 2, 128], F32)
    eq = mk.sb("eq", [128, 2, 128], F32)
    ek = mk.sb("ek", [128, 2, 128], F32)
    elast = mk.sb("elast", [128, 2, 4], F32)
    qtil = mk.sb("qtil", [128, 2, 128], BF16)
    ktil = mk.sb("ktil", [128, 2, 128], F32)
    ktilb = mk.sb("ktilb", [128, 2, 128], BF16)
    khT = mk.sb("khT", [128, 2, 128], BF16)
    khat4 = mk.sb("khat4", [128, 4, 256], BF16)
    PT = mk.sb("PT", [128, 4, 128], BF16)
    S = [mk.sb("S%d" % i, [128, DV], F32) for i in range(2)]
    Sb = [mk.sb("Sb%d" % i, [128, DV], BF16) for i in range(2)]
    ost = [mk.sb("ost%d" % i, [128, NPO, 128], F32) for i in range(2)]
    ofl = [mk.sb("ofl%d" % i, [128, NPO, 128], F32) for i in range(2)]
    osum = mk.sb("osum", [128, NPO, 128], F32)
    if gla:
        osq = mk.sb("osq", [128, 4, 128], BF16)
        rstd = mk.sb("rstdg", [128, 512], F32)
        yst = [mk.sb("ystg%d" % i, [128, 4, 128], BF16) for i in range(2)]
    else:
        otok = mk.sb("otok", [128, 256], F32)
        junkh = mk.sb("junkh", [128, 64], F32)
        ssh = mk.sb("ssh", [128, 4], F32)
        rsth = mk.sb("rsth", [128, 4], F32)
        ptf = pds
        ptfv = pssv
        ysth = [mk.sb("ysth%d" % i, [128, 256], BF16) for i in range(2)]
    ntile = [0]

    def po_v():
        if gla:
            return po[:, :, 0:256].rearrange("p r (m t) -> p r m t", m=2)
        return None

    def hv(buf):
        return buf[:].rearrange("p (m r) t -> p r m t", r=2)

    def ost_v(par):
        return hv(ost[par]) if gla else None

    def ofl_v(par):
        return hv(ofl[par]) if gla else None

    def osum_v():
        return hv(osum) if gla else None


    blocks = [(0, 2)] + [(2 + 4 * b, 4) for b in range(8)]
    for d in range(2):
        last_pass = (d == 1)
        for mt in range(2):
            mk.op("pool", lambda e: e.memset(S[mt][:], 0.0), reads=[S[mt]], writes=[S[mt]])
            mk.op("pool", lambda e: e.memset(Sb[mt][:], 0.0), reads=[Sb[mt]], writes=[Sb[mt]])
        order = blocks if d == 0 else [blocks[0]] + blocks[:0:-1]
        for bi, (tile0, ntl) in enumerate(order):
            N = ntl * 128
            t0 = tile0 * 128
            ub = ubs[bi % 2]
            mk.dma("sp", ub[:, :, 0:N], UTsrc[:, :, t0:t0 + N].rearrange("k p t -> p k t"), reads=[UTsrc], writes=[ub])

            def proj(W, c0, m, evac):
                pp = nextp()
                for kc in range(8):
                    mk.mm(pp[0:m, 0:N], W[:, kc, c0:c0 + m], ub[:, kc, 0:N], start=(kc == 0), stop=(kc == 7),
                          reads=[W, ub], writes=[pp])
                evac(pp)

            for mt in range(2):
                if gla:
                    proj(Wq, mt * 128, 128, lambda pp: mk.op("act", lambda e: e.activation(qT[:, mt, 0:N], pp[:, 0:N], AF.Copy),
                                                              reads=[pp, qT], writes=[qT]))
                else:
                    proj(Wq, mt * 128, 128, lambda pp: mk.op("act", lambda e: e.activation(qT[:, mt, 0:N], pp[:, 0:N], AF.Silu),
                                                              reads=[pp, qT], writes=[qT]))
            if gla:
                for mt in range(2):
                    proj(Wk, mt * 128, 128, lambda pp: mk.op("dve", lambda e: e.tensor_copy(kT[:, mt, 0:N], pp[:, 0:N]),
                                                              reads=[pp, kT], writes=[kT]))
                proj(Wdec[d], 0, 16, lambda pp: mk.op("act", lambda e: e.activation(codeT[:, 0:N], pp[0:16, 0:N], AF.Copy),
                                                       reads=[pp], writes=[codeT]))
                for mt in range(2):
                    pp = nextp()
                    mk.mm(pp[:, 0:N], GU[d][:, mt * 128:(mt + 1) * 128], codeT[:, 0:N], start=True, stop=True,
                          reads=[GU[d], codeT], writes=[pp])
                    mk.op("act", lambda e: e.activation(LG[:, mt, 0:N], pp[:, 0:N], AF.Exp, scale=-1.0, bias=nbcol[:, d, mt:mt + 1]),
                          reads=[pp, nbcol, LG], writes=[LG])
                    mk.op("act", lambda e: e.activation(LG[:, mt, 0:N], LG[:, mt, 0:N], AF.Ln, bias=g.one_col[:, 0:1]),
                          reads=[LG, g.one_col], writes=[LG])
            else:
                for mt in range(2):
                    def ev(pp):
                        mk.op("act", lambda e: e.activation(sg[:, mt, 0:N], pp[:, 0:N], AF.Sigmoid), reads=[pp, sg], writes=[sg])
                        mk.op("act", lambda e: e.activation(LG[:, mt, 0:N], sg[:, mt, 0:N], AF.Ln, scale=omlb[:, mt:mt + 1],
                                                             bias=lbc[:, mt:mt + 1]), reads=[sg, omlb, lbc, LG], writes=[LG])
                        mk.op("dve", lambda e: e.tensor_scalar(kT[:, mt, 0:N], sg[:, mt, 0:N], -1.0, 1.0, ALU.mult, ALU.add),
                              reads=[sg, kT], writes=[kT])
                        mk.op("dve", lambda e: e.tensor_scalar(kT[:, mt, 0:N], kT[:, mt, 0:N], omlb[:, mt:mt + 1], None, ALU.mult),
                              reads=[kT, omlb], writes=[kT])
                    proj(Wdec[d], mt * 128, 128, ev)
            for tl in range(ntl):
                pp = nextp()
                for kc in range(8):
                    mk.mm(pp[:, 0:VW], ub[:, kc, tl * 128:(tl + 1) * 128], Wv[:, kc, :], start=(kc == 0), stop=(kc == 7),
                          reads=[Wv, ub], writes=[pp])
                mk.op("dve", lambda e: e.tensor_copy(vtok[:, tl, :], pp[:, 0:VW]), reads=[pp, vtok], writes=[vtok])
                if last_pass and not gla:
                    pp = nextp()
                    for kc in range(8):
                        mk.mm(pp[:, 0:256], ub[:, kc, tl * 128:(tl + 1) * 128], Wog[:, kc, :], start=(kc == 0), stop=(kc == 7),
                              reads=[Wog, ub], writes=[pp])
                    mk.op("act", lambda e: e.activation(ogs[:, tl, :], pp[:, 0:256], AF.Silu), reads=[pp, ogs], writes=[ogs])
            if last_pass and gla:
                for h in range(4):
                    proj(Wr, h * 128, 128, lambda pp: mk.op("act", lambda e: e.activation(rsil[:, h, 0:N], pp[:, 0:N], AF.Silu),
                                                             reads=[pp, rsil], writes=[rsil]))
            tls = list(range(ntl)) if d == 0 else list(range(ntl - 1, -1, -1))
            for tl in tls:
                ti = tile0 + tl
                tsl = slice(tl * 128, (tl + 1) * 128)
                ntile[0] += 1
                par = ntile[0] % 2
                if last_pass:
                    mk.dma("act", ofl[par][:], OF[:, :, ti * 128:(ti + 1) * 128].rearrange("m p t -> p m t"),
                           reads=[OF], writes=[ofl[par]])
                for mt in range(2):
                    for c in range(4):
                        lo, hi = 32 * c, 32 * c + 32
                        if d == 0:
                            osl = slice(lo, hi)
                            isl = slice(tl * 128 + lo, tl * 128 + hi)
                        else:
                            osl = slice(hi - 1, lo - 1 if lo > 0 else None, -1)
                            ilo, ihi = tl * 128 + lo, tl * 128 + hi
                            isl = slice(ihi - 1, ilo - 1 if ilo > 0 else None, -1)
                        mk.op("dve", lambda e: e.tensor_tensor_scan(cum[:, mt, osl], ones32[:, :], LG[:, mt, isl], 0.0,
                                                                     ALU.mult, ALU.add), reads=[LG, ones32, cum], writes=[cum])
                lidx = slice(31, 128, 32) if d == 0 else slice(0, 128, 32)
                mk.op("act", lambda e: e.activation(eq[:], cum[:], AF.Exp, scale=esc), reads=[cum], writes=[eq])
                mk.op("act", lambda e: e.activation(ek[:], cum[:], AF.Exp, scale=-esc), reads=[cum], writes=[ek])
                mk.op("act", lambda e: e.activation(elast[:], cum[:, :, lidx], AF.Exp, scale=esc), reads=[cum], writes=[elast])
                mk.op("dve", lambda e: e.scalar_tensor_tensor(qtil[:], qT[:, :, tsl], qscale, eq[:], ALU.mult, ALU.mult),
                      reads=[qT, eq], writes=[qtil])
                mk.op("dve", lambda e: e.tensor_tensor(ktil[:], kT[:, :, tsl], ek[:], ALU.mult), reads=[kT, ek], writes=[ktil])
                mk.op("pool", lambda e: e.tensor_copy(ktilb[:], ktil[:]), reads=[ktil], writes=[ktilb])
                mk.op("dve", lambda e: e.tensor_tensor(khT[:].rearrange("p m (c j) -> p m c j", j=32),
                                                        ktil[:].rearrange("p m (c j) -> p m c j", j=32),
                                                        elast[:].unsqueeze(3).broadcast_to([128, 2, 4, 32]), ALU.mult),
                      reads=[ktil, elast], writes=[khT])
                for mt in range(2):
                    mk.tr(ptr[:, mt * 128:(mt + 1) * 128], khT[:, mt, :], g.identb[:], reads=[khT, g.identb], writes=[ptr])
                for c in range(4):
                    if c % 2 == 0:
                        mk.op("act", lambda e: e.activation(khat4[:, c, :], ptr[:, 0:256], AF.Identity, scale=rowmask[:, c:c + 1]),
                              reads=[ptr, rowmask, khat4], writes=[khat4])
                    else:
                        mk.op("dve", lambda e: e.tensor_scalar(khat4[:, c, :], ptr[:, 0:256], rowmask[:, c:c + 1], None, ALU.mult),
                              reads=[ptr, rowmask, khat4], writes=[khat4])
                for h in range(4):
                    mt, rows = h // 2, slice(64 * (h % 2), 64 * (h % 2) + 64)
                    mk.mm(psc[:, h % 2, mt * 128:(mt + 1) * 128], ktilb[rows, mt, :], qtil[rows, mt, :], start=True, stop=True,
                          reads=[ktilb, qtil], writes=[psc])
                mk.op("dve", lambda e: e.tensor_tensor(PT[:].rearrange("p (m r) t -> p r m t", r=2),
                                                        psc[:, :, 0:256].rearrange("p r (m t) -> p r m t", m=2),
                                                        masks[d][:].unsqueeze(1).unsqueeze(1).broadcast_to([128, 2, 2, 128]), ALU.mult),
                      reads=[psc, masks[d]], writes=[PT])

                def po_ap(h, cols):
                    c0_ = (h // 2) * 128
                    csl = slice(c0_ + cols.start, c0_ + cols.stop)
                    if gla:
                        return po[:, h % 2, csl]
                    return po[64 * (h % 2):64 * (h % 2) + 64, h % 2, csl]

                chunks = list(range(4)) if d == 0 else [3, 2, 1, 0]
                for ci, c in enumerate(chunks):
                    cs = slice(32 * c, 32 * c + 32)
                    for h in range(4):
                        mt, rows = h // 2, slice(64 * (h % 2), 64 * (h % 2) + 64)
                        mk.mm(po_ap(h, cs), vtok[:, tl, h * DV:(h + 1) * DV], PT[:, h, cs], start=True, stop=False,
                              reads=[vtok, PT], writes=[po])
                        mk.mm(po_ap(h, cs), Sb[mt][rows, :], qtil[rows, mt, cs], start=False, stop=True,
                              reads=[Sb[mt], qtil], writes=[po])
                    for h in range(4):
                        mt, rows = h // 2, slice(64 * (h % 2), 64 * (h % 2) + 64)
                        mk.mm(pds[rows, mt, 0:DV], khat4[:, c, h * 64:(h + 1) * 64], vtok[:, tl, h * DV:(h + 1) * DV], start=True, stop=True,
                              reads=[khat4, vtok], writes=[pds])
                    for mt in range(2):
                        mk.op("dve", lambda e: e.scalar_tensor_tensor(S[mt][:], S[mt][:], elast[:, mt, c:c + 1], pds[:, mt, 0:DV],
                                                                       ALU.mult, ALU.add), reads=[S[mt], elast, pds], writes=[S[mt]])
                        mk.op("pool" if mt == 0 else "act",
                              (lambda e: e.tensor_copy(Sb[mt][:], S[mt][:])) if mt == 0 else
                              (lambda e: e.activation(Sb[mt][:], S[mt][:], AF.Copy)), reads=[S[mt]], writes=[Sb[mt]])
                if not last_pass:
                    if gla:
                        mk.op("act", lambda e: e.activation(ost_v(par), po_v(), AF.Copy), reads=[po], writes=[ost[par]])
                    else:
                        for r in range(2):
                            rr = slice(64 * r, 64 * r + 64)
                            mk.op("act", lambda e: e.activation(ost[par][rr, :, :], po[rr, r, 0:256].rearrange("p (m t) -> p m t", m=2), AF.Copy),
                                  reads=[po, ost[par]], writes=[ost[par]])
                    mk.dma("sp", OF[:, :, ti * 128:(ti + 1) * 128].rearrange("m p t -> p m t"), ost[par][:],
                           reads=[ost[par]], writes=[OF])
                    continue
                if gla:
                    mk.op("dve", lambda e: e.tensor_tensor(osum_v(), po_v(), ofl_v(par), ALU.add), reads=[po, ofl[par]], writes=[osum])
                else:
                    for r in range(2):
                        rr = slice(64 * r, 64 * r + 64)
                        mk.op("dve", lambda e: e.tensor_tensor(osum[rr, :, :], po[rr, r, 0:256].rearrange("p (m t) -> p m t", m=2),
                                                                ofl[par][rr, :, :], ALU.add), reads=[po, ofl[par], osum], writes=[osum])
                if gla:
                    mk.op("act", lambda e: e.activation(osq[:], osum[:], AF.Square), reads=[osum], writes=[osq])
                    mk.mm(pssv, ones_bf[:], osq[:].rearrange("p h t -> p (h t)"), start=True, stop=True,
                          reads=[ones_bf, osq], writes=[pss])
                    mk.op("act", lambda e: e.activation(rstd[:], pssv, AF.Sqrt, scale=1.0 / 128.0, bias=g.eps_col[:, 0:1]),
                          reads=[pss, g.eps_col], writes=[rstd])
                    mk.op("dve", lambda e: e.reciprocal(rstd[:], rstd[:]), reads=[rstd], writes=[rstd])
                    mk.op("dve", lambda e: e.scalar_tensor_tensor(osum[:], osum[:], gn[:, 0:1], rstd[:].rearrange("p (h t) -> p h t", h=4),
                                                                   ALU.mult, ALU.mult), reads=[osum, gn, rstd], writes=[osum])
                    mk.op("dve", lambda e: e.tensor_tensor(yst[par][:], osum[:], rsil[:, :, tsl], ALU.mult),
                          reads=[osum, rsil], writes=[yst[par]])
                    mk.dma("sp", g.YGLAT[:, :, ti * 128:(ti + 1) * 128].rearrange("m p t -> p m t"), yst[par][:],
                           reads=[yst[par]], writes=[g.YGLAT])
                else:
                    for mt in range(2):
                        mk.tr(ptfv[:, mt * 128:(mt + 1) * 128], osum[:, mt, :], g.identf[:], reads=[osum, g.identf], writes=[ptf])
                    mk.op("act", lambda e: e.activation(otok[:], ptfv[:, 0:256], AF.Copy), reads=[ptf], writes=[otok])
                    for h in range(4):
                        mk.op("act", lambda e: e.activation(junkh[:], otok[:, h * 64:(h + 1) * 64], AF.Square, accum_out=ssh[:, h:h + 1]),
                              reads=[otok, junkh, ssh], writes=[junkh, ssh])
                    mk.op("act", lambda e: e.activation(rsth[:], ssh[:], AF.Sqrt, scale=1.0 / 64.0, bias=g.eps_col[:, 0:1]),
                          reads=[ssh, g.eps_col], writes=[rsth])
                    mk.op("dve", lambda e: e.reciprocal(rsth[:], rsth[:]), reads=[rsth], writes=[rsth])
                    ov = otok[:].rearrange("p (h v) -> p h v", h=4)
                    mk.op("dve", lambda e: e.tensor_tensor(ov, ov, rsth[:].unsqueeze(2).broadcast_to([128, 4, 64]), ALU.mult),
                          reads=[otok, rsth], writes=[otok])
                    mk.op("dve", lambda e: e.tensor_tensor(ov, ov, gnrow[:], ALU.mult), reads=[otok, gnrow], writes=[otok])
                    mk.op("dve", lambda e: e.tensor_tensor(ysth[par][:], otok[:], ogs[:, tl, :], ALU.mult),
                          reads=[otok, ogs], writes=[ysth[par]])
                    pofs = 0
                    for (r0, rs, n) in scan_rows(ti):
                        dst = g.YHG[r0:r0 + n, :] if rs == 1 else g.YHG[r0:r0 + (n - 1) * rs + 1:rs, :]
                        mk.dma("sp", dst, ysth[par][pofs:pofs + n, :], reads=[ysth[par]], writes=[g.YHG])
                        pofs += n
    mk.end_phase()


SCRATCH.update({
    "VT": ([8, 128, T], BF16),
    "GATESD": ([128, NT, 16], F32),
})

BIG = 1.0e30


def phase_merge(mk, g, layer):
    nc = mk.nc
    last = (layer == 1)
    if not hasattr(g, "gates"):
        g.gates = mk.sb("gates", [128, NT, 16], F32)
    mk.begin_phase()

    def loadw(name, src_ap, kc, n):
        w = mk.sb(name, [128, kc, n], BF16)
        mk.dma("pool", w[:], src_ap.rearrange("(kc p) n -> p kc n", p=128), reads=[g.w_in], writes=[w])
        return w
    Wg = mk.sb("Wg", [128, 8, 3072], BF16)
    for br in range(3):
        mk.dma("pool", Wg[:, :, br * 1024:(br + 1) * 1024],
               g.w_in[layer, :, C_GATE + br * 1024:C_GATE + (br + 1) * 1024].rearrange("(kc p) n -> p kc n", p=128),
               reads=[g.w_in], writes=[Wg])
    Wb = [loadw("Wb0", g.w_branch_s5[layer], 2, 1024), loadw("Wb1", g.w_branch_gla[layer], 4, 1024),
          loadw("Wb2", g.w_branch_hgrn[layer], 2, 1024)]
    Wout = loadw("Wout", g.w_out[layer], 8, 1024)
    RW = mk.sb("RW", [128, 8, 16], F32)
    rb = mk.sb("rb", [128, 16], F32)
    G2row = mk.sb("G2row", [128, 1024], F32)
    with nc.allow_non_contiguous_dma(reason="small"):
        mk.dma("sp", RW[:], g.router_w[:, :].rearrange("(kc p) e -> p kc e", p=128), reads=[g.router_w], writes=[RW])
        mk.dma("sp", rb[:], g.router_b[:].partition_broadcast(128), reads=[g.router_b], writes=[rb])
    rot = [mk.ps("rot%d" % i, [128, 512], F32) for i in range(2)]
    pmix = mk.ps("pmix", [128, 2, 512], F32)
    pT32 = mk.ps("pT32", [128, 8, 128], F32)
    plg = mk.ps("plg", [128, 512], F32)
    ptb = mk.ps("ptb", [128, 1024], BF16)
    nrot = [0]

    def nextrot():
        nrot[0] += 1
        return rot[nrot[0] % 2]

    ubs = [mk.sb("ub%d" % i, [128, 8, 512], BF16) for i in range(2)]
    ys5 = mk.sb("ys5", [128, 2, 512], BF16)
    ygl = mk.sb("ygl", [128, 4, 512], BF16)
    yhtok = mk.sb("yhtok", [128, 4, 256], BF16)
    yhT = mk.sb("yhT", [128, 2, 512], BF16)
    sig = [mk.sb("sig%d" % i, [128, 512], F32) for i in range(2)]
    acc = mk.sb("acc", [128, 512], F32)
    tmpm = mk.sb("tmpm", [128, 512], F32)
    mergedT = mk.sb("mergedT", [128, 8, 512], BF16)
    hts = [mk.sb("h_t%d" % i, [128, D], F32) for i in range(2)]
    tmph = mk.sb("tmph", [128, D], F32)
    xnf = mk.sb("xnf", [128, D], F32)
    junk = mk.sb("junk", [128, D], BF16)
    ss = mk.sb("ss", [128, 1], F32)
    rstd = mk.sb("rstd", [128, 1], F32)
    vts = [mk.sb("vt%d" % i, [128, 8, 128], BF16) for i in range(2)]
    vT32 = mk.sb("vT32", [128, 8, 128], F32)
    LGT = mk.sb("LGT", [128, NT, 16], F32)
    R = {k: mk.sb("r_" + k, [128, 16], F32) for k in ("ex", "pr", "sel", "msk", "m1", "m2", "w")}
    r4 = {k: mk.sb("r4_" + k, [128, 4], F32) for k in ("a", "gs", "inb", "t2")}
    r1 = {k: mk.sb("r1_" + k, [128, 1], F32) for k in ("mx", "sum", "best", "top1", "top2", "ws")}

    blocks = [(0, 2)] + [(2 + 4 * b, 4) for b in range(8)]
    if last:
        blocks = blocks[1:]
    cur_w = [None]
    nt_ = [0]
    mergedTs = [mergedT, mk.sb("mergedT2", [128, 8, 512], BF16)]

    def dtloop(bi, tile0, ntl):
            N = ntl * 128
            t0 = tile0 * 128
            wv = 1 if tile0 < 2 else 0
            ub = ubs[bi % 2]
            mk.dma("sp", ub[:, :, 0:N], g.UT[:, :, t0:t0 + N].rearrange("k p t -> p k t"), reads=[g.UT], writes=[ub])
            mk.dma("pool", ys5[:, :, 0:N], g.YS5T[:, :, t0:t0 + N].rearrange("m p t -> p m t"), reads=[g.YS5T], writes=[ys5])
            mk.dma("pool", ygl[:, :, 0:N], g.YGLAT[:, :, t0:t0 + N].rearrange("m p t -> p m t"), reads=[g.YGLAT], writes=[ygl])
            mk.dma("sp", yhtok[:, 0:ntl, :], g.YHG[t0:t0 + N, :].rearrange("(a p) c -> p a c", p=128), reads=[g.YHG], writes=[yhtok])
            for tl in range(ntl):
                for m in range(2):
                    mk.tr(ptb[:, m * 128:(m + 1) * 128], yhtok[:, tl, m * 128:(m + 1) * 128], g.identb[:],
                          reads=[yhtok, g.identb], writes=[ptb])
                mk.op("act", lambda e: e.activation(yhT[:, :, tl * 128:(tl + 1) * 128], ptb[:, 0:256].rearrange("p (m t) -> p m t", m=2), AF.Copy),
                      reads=[ptb, yhT], writes=[yhT])
            ybr = [(ys5, 2), (ygl, 4), (yhT, 2)]
            for dt in range(8):
                for br in range(3):
                    pg = nextrot()
                    for kc in range(8):
                        mk.mm(pg[:, 0:N], Wg[:, kc, br * 1024 + dt * 128:br * 1024 + (dt + 1) * 128], ub[:, kc, 0:N],
                              start=(kc == 0), stop=(kc == 7), reads=[Wg, ub], writes=[pg])
                    sg = sig[br % 2]
                    mk.op("act", lambda e: e.activation(sg[:, 0:N], pg[:, 0:N], AF.Sigmoid), reads=[pg], writes=[sg])
                    pp = nextrot()
                    yb, nk = ybr[br]
                    for kc in range(nk):
                        mk.mm(pp[:, 0:N], Wb[br][:, kc, dt * 128:(dt + 1) * 128], yb[:, kc, 0:N],
                              start=(kc == 0), stop=(kc == nk - 1), reads=[Wb[br], yb], writes=[pp])
                    if br == 0:
                        mk.op("dve", lambda e: e.tensor_tensor(acc[:, 0:N], pp[:, 0:N], sg[:, 0:N], ALU.mult), reads=[pp, sg], writes=[acc])
                    elif br == 1:
                        mk.op("dve", lambda e: e.tensor_tensor(tmpm[:, 0:N], pp[:, 0:N], sg[:, 0:N], ALU.mult), reads=[pp, sg], writes=[tmpm])
                        mk.op("dve", lambda e: e.tensor_tensor(acc[:, 0:N], acc[:, 0:N], tmpm[:, 0:N], ALU.add), reads=[acc, tmpm], writes=[acc])
                    else:
                        mk.op("dve", lambda e: e.tensor_tensor(tmpm[:, 0:N], pp[:, 0:N], sg[:, 0:N], ALU.mult), reads=[pp, sg], writes=[tmpm])
                        mk.op("dve", lambda e: e.tensor_tensor(mergedTs[bi % 2][:, dt, 0:N], acc[:, 0:N], tmpm[:, 0:N], ALU.add),
                              reads=[acc, tmpm, mergedTs[bi % 2]], writes=[mergedTs[bi % 2]])
                    yield


    def tails(bi, tile0, ntl):
            N = ntl * 128
            t0 = tile0 * 128
            wv = 1 if tile0 < 2 else 0
            if cur_w[0] != wv:
                cur_w[0] = wv
                mk.dma("sp", G2row[:], g.MOD[wv, 2048:3072].partition_broadcast(128), reads=[g.MOD], writes=[G2row])
            for tl in range(ntl):
                ti = tile0 + tl
                nt_[0] += 1
                par = nt_[0] % 2
                ht = hts[par]
                mk.dma("sp", ht[:], g.H[ti * 128:(ti + 1) * 128, :], reads=[g.H], writes=[ht])
                for hf in range(2):
                    for kc in range(8):
                        mk.mm(pmix[:, hf, :], mergedTs[bi % 2][:, kc, tl * 128:(tl + 1) * 128], Wout[:, kc, hf * 512:(hf + 1) * 512],
                              start=(kc == 0), stop=(kc == 7), reads=[mergedTs[bi % 2], Wout], writes=[pmix])
                mk.op("dve", lambda e: e.tensor_tensor(tmph[:], pmix[:].rearrange("p a b -> p (a b)"), G2row[:], ALU.mult),
                      reads=[pmix, G2row], writes=[tmph])
                mk.op("dve", lambda e: e.tensor_tensor(ht[:], ht[:], tmph[:], ALU.add), reads=[ht, tmph], writes=[ht])
                mk.dma("sp", g.H[ti * 128:(ti + 1) * 128, :], ht[:], reads=[ht], writes=[g.H])
                yield
                mk.op("act", lambda e: e.activation(junk[:], ht[:], AF.Square, accum_out=ss[:, 0:1]), reads=[ht], writes=[junk, ss])
                mk.op("act", lambda e: e.activation(rstd[:], ss[:], AF.Sqrt, scale=1.0 / D, bias=g.eps_col[:, 0:1]),
                      reads=[ss, g.eps_col], writes=[rstd])
                mk.op("dve", lambda e: e.reciprocal(rstd[:], rstd[:]), reads=[rstd], writes=[rstd])
                mk.op("dve", lambda e: e.tensor_scalar(xnf[:], ht[:], rstd[:, 0:1], None, ALU.mult), reads=[ht, rstd], writes=[xnf])
                for kc in range(8):
                    mk.tr(pT32[:, kc, :], xnf[:, kc * 128:(kc + 1) * 128], g.identf[:], reads=[xnf, g.identf], writes=[pT32])
                yield
                vt = vts[par]
                mk.op("dve", lambda e: e.tensor_tensor(vT32[:], pT32[:], g.A2[:, :, wv].unsqueeze(2).broadcast_to([128, 8, 128]), ALU.mult),
                      reads=[pT32, g.A2], writes=[vT32])
                mk.op("dve", lambda e: e.tensor_tensor(vT32[:], vT32[:], g.M_all[:, 24:32, wv].unsqueeze(2).broadcast_to([128, 8, 128]), ALU.add),
                      reads=[vT32, g.M_all], writes=[vT32])
                mk.op("act", lambda e: e.activation(vt[:], vT32[:], AF.Copy), reads=[vT32], writes=[vt])
                mk.dma("pool", g.VT[:, :, ti * 128:(ti + 1) * 128].rearrange("k p t -> p k t"), vt[:], reads=[vt], writes=[g.VT])
                yield
                for kc in range(8):
                    mk.mm(plg[:, 0:16], vT32[:, kc, :], RW[:, kc, :], start=(kc == 0), stop=(kc == 7), reads=[vT32, RW], writes=[plg])
                mk.op("act", lambda e: e.activation(LGT[:, ti, :], plg[:, 0:16], AF.Copy), reads=[plg, LGT], writes=[LGT])
                yield


    def run_gens(gens):
        alive = [True] * len(gens)
        while any(alive):
            for i_, gi in enumerate(gens):
                if alive[i_]:
                    try:
                        next(gi)
                    except StopIteration:
                        alive[i_] = False

    run_gens([dtloop(0, *blocks[0])])
    for bi in range(len(blocks)):
        gl = [tails(bi, *blocks[bi])]
        if bi + 1 < len(blocks):
            gl.append(dtloop(bi + 1, *blocks[bi + 1]))
        run_gens(gl)
    V = "dve"
    tsel = slice(2, NT) if last else slice(0, NT)
    ntv = NT - 2 if last else NT
    L3 = LGT[:, tsel, :]
    sh = [128, ntv, 16]
    sh4 = [128, ntv, 4]
    Rb = {k: mk.sb("rb_" + k, [128, NT, 16], F32) for k in ("ex", "pr", "sel", "msk", "m1", "m2")}
    Rg = {k: mk.sb("rg_" + k, [128, NT, 4], F32) for k in ("a", "gs", "inb", "t2")}
    Rs = {k: mk.sb("rs_" + k, [128, NT], F32) for k in ("mx", "sum", "best", "top1", "top2", "ws")}
    X = lambda k: Rb[k][:, tsel, :]
    X4 = lambda k: Rg[k][:, tsel, :]
    X1 = lambda k: Rs[k][:, tsel]
    B16 = lambda k: Rs[k][:, tsel].unsqueeze(2).broadcast_to(sh)
    mk.op(V, lambda e: e.tensor_reduce(X1("mx"), L3, AX.X, ALU.max), reads=[LGT], writes=[Rs["mx"]])
    mk.op(V, lambda e: e.tensor_tensor(X("ex"), L3, B16("mx"), ALU.subtract), reads=[LGT, Rs["mx"]], writes=[Rb["ex"]])
    mk.op("act", lambda e: e.activation(X("ex"), X("ex"), AF.Exp), reads=[Rb["ex"]], writes=[Rb["ex"]])
    mk.op(V, lambda e: e.tensor_reduce(X1("sum"), X("ex"), AX.X, ALU.add), reads=[Rb["ex"]], writes=[Rs["sum"]])
    mk.op(V, lambda e: e.reciprocal(X1("sum"), X1("sum")), reads=[Rs["sum"]], writes=[Rs["sum"]])
    mk.op(V, lambda e: e.tensor_tensor(X("pr"), X("ex"), B16("sum"), ALU.mult), reads=[Rb["ex"], Rs["sum"]], writes=[Rb["pr"]])
    mk.op(V, lambda e: e.tensor_tensor(X("sel"), X("pr"), rb[:].unsqueeze(1).broadcast_to(sh), ALU.add), reads=[Rb["pr"], rb], writes=[Rb["sel"]])
    selg = lambda j: Rb["sel"][:, tsel, :].rearrange("p t (g j) -> p t g j", j=4)[:, :, :, j]
    first = True
    for (a_, b_) in ((0, 1), (0, 2), (0, 3), (1, 2), (1, 3), (2, 3)):
        if first:
            mk.op(V, lambda e: e.tensor_tensor(X4("gs"), selg(a_), selg(b_), ALU.add), reads=[Rb["sel"]], writes=[Rg["gs"]])
            first = False
        else:
            mk.op(V, lambda e: e.tensor_tensor(X4("a"), selg(a_), selg(b_), ALU.add), reads=[Rb["sel"]], writes=[Rg["a"]])
            mk.op(V, lambda e: e.tensor_tensor(X4("gs"), X4("gs"), X4("a"), ALU.max), reads=[Rg["gs"], Rg["a"]], writes=[Rg["gs"]])
    mk.op(V, lambda e: e.tensor_reduce(X1("best"), X4("gs"), AX.X, ALU.max), reads=[Rg["gs"]], writes=[Rs["best"]])
    mk.op(V, lambda e: e.tensor_tensor(X4("inb"), X4("gs"), Rs["best"][:, tsel].unsqueeze(2).broadcast_to(sh4), ALU.is_equal),
          reads=[Rg["gs"], Rs["best"]], writes=[Rg["inb"]])
    mk.op(V, lambda e: e.tensor_scalar(X4("t2"), X4("inb"), BIG, -BIG, ALU.mult, ALU.add), reads=[Rg["inb"]], writes=[Rg["t2"]])
    for j in range(4):
        mskj = Rb["msk"][:, tsel, :].rearrange("p t (g j) -> p t g j", j=4)[:, :, :, j]
        mk.op(V, lambda e: e.tensor_tensor(mskj, selg(j), X4("inb"), ALU.mult), reads=[Rb["sel"], Rg["inb"], Rb["msk"]], writes=[Rb["msk"]])
        mk.op(V, lambda e: e.tensor_tensor(mskj, mskj, X4("t2"), ALU.add), reads=[Rb["msk"], Rg["t2"]], writes=[Rb["msk"]])
    mk.op(V, lambda e: e.tensor_reduce(X1("top1"), X("msk"), AX.X, ALU.max), reads=[Rb["msk"]], writes=[Rs["top1"]])
    mk.op(V, lambda e: e.tensor_tensor(X("m1"), X("msk"), B16("top1"), ALU.is_equal), reads=[Rb["msk"], Rs["top1"]], writes=[Rb["m1"]])
    mk.op(V, lambda e: e.scalar_tensor_tensor(X("msk"), X("m1"), -BIG, X("msk"), ALU.mult, ALU.add), reads=[Rb["m1"], Rb["msk"]], writes=[Rb["msk"]])
    mk.op(V, lambda e: e.tensor_reduce(X1("top2"), X("msk"), AX.X, ALU.max), reads=[Rb["msk"]], writes=[Rs["top2"]])
    mk.op(V, lambda e: e.tensor_tensor(X("m2"), X("msk"), B16("top2"), ALU.is_equal), reads=[Rb["msk"], Rs["top2"]], writes=[Rb["m2"]])
    mk.op(V, lambda e: e.tensor_tensor(X("m1"), X("m1"), X("m2"), ALU.add), reads=[Rb["m1"], Rb["m2"]], writes=[Rb["m1"]])
    mk.op(V, lambda e: e.tensor_tensor(X("ex"), X("pr"), X("m1"), ALU.mult), reads=[Rb["pr"], Rb["m1"]], writes=[Rb["ex"]])
    mk.op(V, lambda e: e.tensor_reduce(X1("ws"), X("ex"), AX.X, ALU.add), reads=[Rb["ex"]], writes=[Rs["ws"]])
    mk.op(V, lambda e: e.reciprocal(X1("ws"), X1("ws")), reads=[Rs["ws"]], writes=[Rs["ws"]])
    mk.op(V, lambda e: e.tensor_tensor(g.gates[:, tsel, :], X("ex"), B16("ws"), ALU.mult), reads=[Rb["ex"], Rs["ws"], g.gates], writes=[g.gates])
    if getattr(g, 'debug_gates', False):
        mk.dma("sp", g.GATESD[:, :, :], g.gates[:], reads=[g.gates], writes=[g.GATESD])
    mk.end_phase()


def phase_moe(mk, g, layer):
    nc = mk.nc
    last = (layer == 1)
    mk.begin_phase()
    tiles = list(range(2, NT)) if last else list(range(NT))
    nsb = 4
    per = (len(tiles) + nsb - 1) // nsb
    sbs = [tiles[i * per:(i + 1) * per] for i in range(nsb)]
    sbs = [x for x in sbs if x]
    MAXT = max(len(x) for x in sbs)
    accs = [mk.sb("macc%d" % i, [128, MAXT, D], F32) for i in range(2)]
    vts = [mk.sb("mvt%d" % i, [128, 8, MAXT * 128], BF16) for i in range(2)]

    def load_vt(si):
        sb_ = sbs[si]
        mk.dma("sp", vts[si % 2][:, :, 0:len(sb_) * 128], g.VT[:, :, sb_[0] * 128:sb_[0] * 128 + len(sb_) * 128].rearrange("k p t -> p k t"),
               reads=[g.VT], writes=[vts[si % 2]])
    load_vt(0)
    Wup = [mk.sb("Wup%d" % i, [128, 8, 1024], BF16) for i in range(2)]
    Wdn = [mk.sb("Wdn%d" % i, [128, 4, 1024], BF16) for i in range(2)]
    sgs = [mk.sb("msg%d" % i, [128, 512], F32) for i in range(2)]
    hid = [mk.sb("hid%d" % i, [128, 4, 512], BF16) for i in range(2)]
    hts = [mk.sb("mh%d" % i, [128, D], F32) for i in range(2)]
    tmph = mk.sb("mtmp", [128, D], F32)
    G5row = mk.sb("G5row", [128, D], F32)
    pup = [mk.ps("pup%d" % i, [128, 512], F32) for i in range(4)]
    pdn = [mk.ps("pdn%d" % i, [128, 2, 512], F32) for i in range(2)]
    if last:
        fng = mk.sb("fng", [128, D], F32)
        mk.dma("sp", fng[:], g.final_norm_g[:].partition_broadcast(128), reads=[g.final_norm_g], writes=[fng])
        junk = mk.sb("mjunk", [128, D], BF16)
        ss = mk.sb("mss", [128, 1], F32)
        rstd = mk.sb("mrstd", [128, 1], F32)
    cur_w = [None]
    nw = 0
    nup = 0
    nh = 0
    ndn = 0
    def tail_fn(sb, acc):
        for j, ti in enumerate(sb):
            wv = 1 if ti < 2 else 0
            if cur_w[0] != wv:
                cur_w[0] = wv
                mk.dma("sp", G5row[:], g.MOD[wv, 5120:6144].partition_broadcast(128), reads=[g.MOD], writes=[G5row])
            ht = hts[j % 2]
            mk.dma("sp", ht[:], g.H[ti * 128:(ti + 1) * 128, :], reads=[g.H], writes=[ht])
            mk.op("pool", lambda e: e.tensor_tensor(tmph[:], acc[:, j, :], G5row[:], ALU.mult), reads=[acc, G5row], writes=[tmph])
            mk.op("pool", lambda e: e.tensor_tensor(ht[:], ht[:], tmph[:], ALU.add), reads=[ht, tmph], writes=[ht])
            if not last:
                mk.dma("sp", g.H[ti * 128:(ti + 1) * 128, :], ht[:], reads=[ht], writes=[g.H])
            else:
                mk.op("act", lambda e: e.activation(junk[:], ht[:], AF.Square, accum_out=ss[:, 0:1]), reads=[ht], writes=[junk, ss])
                mk.op("act", lambda e: e.activation(rstd[:], ss[:], AF.Sqrt, scale=1.0 / D, bias=g.eps_col[:, 0:1]),
                      reads=[ss, g.eps_col], writes=[rstd])
                mk.op("dve", lambda e: e.reciprocal(rstd[:], rstd[:]), reads=[rstd], writes=[rstd])
                mk.op("dve", lambda e: e.scalar_tensor_tensor(ht[:], ht[:], rstd[:, 0:1], fng[:], ALU.mult, ALU.mult),
                      reads=[ht, rstd, fng], writes=[ht])
                r0 = ti * 128 - NCTX
                mk.dma("sp", g.OUT[r0:r0 + 128, :], ht[:], reads=[ht], writes=[g.OUT])


    items = []
    for si, sb in enumerate(sbs):
        nts = len(sb)
        subs = []
        o = 0
        while o < nts:
            n = min(4, nts - o)
            subs.append((o, n))
            o += n
        for e_ in range(16):
            for k_, (o, n) in enumerate(subs):
                items.append((si, e_, k_, o, n))
    st = {"nw": 0, "nup": 0, "nh": 0, "ndn": 0, "w": {}}

    def up_stage(it):
        si, e_, k_, o, n = it
        sb = sbs[si]
        vt = vts[si % 2]
        if e_ == 0 and k_ == 0 and si + 1 < len(sbs):
            load_vt(si + 1)
        if k_ == 0:
            if e_ == 2 and si > 0:
                tail_fn(sbs[si - 1], accs[(si - 1) % 2])
            wu = Wup[st["nw"] % 2]
            wd = Wdn[st["nw"] % 2]
            st["nw"] += 1
            st["w"][(si, e_)] = (wu, wd)
            mk.dma("pool", wu[:], g.moe_w_up[layer, e_].rearrange("(kc p) n -> p kc n", p=128), reads=[g.moe_w_up], writes=[wu])
            mk.dma("pool", wd[:], g.moe_w_down[layer, e_].rearrange("(kc p) n -> p kc n", p=128), reads=[g.moe_w_down], writes=[wd])
        wu, wd = st["w"][(si, e_)]
        N = n * 128
        c0 = o * 128
        hd = hid[st["nh"] % 2]
        st["nh"] += 1
        for ft in range(4):
            pg = pup[st["nup"] % 4]
            pu = pup[(st["nup"] + 1) % 4]
            st["nup"] += 2
            for kc in range(8):
                mk.mm(pg[:, 0:N], wu[:, kc, ft * 128:(ft + 1) * 128], vt[:, kc, c0:c0 + N], start=(kc == 0), stop=(kc == 7),
                      reads=[wu, vt], writes=[pg])
            for kc in range(8):
                mk.mm(pu[:, 0:N], wu[:, kc, 512 + ft * 128:512 + (ft + 1) * 128], vt[:, kc, c0:c0 + N], start=(kc == 0), stop=(kc == 7),
                      reads=[wu, vt], writes=[pu])
            sg = sgs[ft % 2]
            mk.op("act", lambda e: e.activation(sg[:, 0:N], pg[:, 0:N], AF.Silu), reads=[pg], writes=[sg])
            mk.op("dve", lambda e: e.tensor_tensor(hd[:, ft, 0:N], pu[:, 0:N], sg[:, 0:N], ALU.mult), reads=[pu, sg, hd], writes=[hd])
        return hd

    def down_stage(it, hd):
        si, e_, k_, o, n = it
        sb = sbs[si]
        acc = accs[si % 2]
        wu, wd = st["w"][(si, e_)]
        for tl in range(n):
            ti = sb[o + tl]
            pd = pdn[st["ndn"] % 2]
            st["ndn"] += 1
            for hf in range(2):
                for ft in range(4):
                    mk.mm(pd[:, hf, :], hd[:, ft, tl * 128:(tl + 1) * 128], wd[:, ft, hf * 512:(hf + 1) * 512],
                          start=(ft == 0), stop=(ft == 3), reads=[hd, wd], writes=[pd])
            pdv = pd[:].rearrange("p a b -> p (a b)")
            if e_ == 0:
                mk.op("dve", lambda e: e.tensor_scalar(acc[:, o + tl, :], pdv, g.gates[:, ti, e_:e_ + 1], None, ALU.mult),
                      reads=[pd, g.gates, acc], writes=[acc])
            else:
                mk.op("dve", lambda e: e.scalar_tensor_tensor(acc[:, o + tl, :], pdv, g.gates[:, ti, e_:e_ + 1], acc[:, o + tl, :],
                                                               ALU.mult, ALU.add), reads=[pd, g.gates, acc], writes=[acc])

    hd_cur = up_stage(items[0])
    for k in range(len(items)):
        hd_next = up_stage(items[k + 1]) if k + 1 < len(items) else None
        down_stage(items[k], hd_cur)
        hd_cur = hd_next
    tail_fn(sbs[-1], accs[(len(sbs) - 1) % 2])
    mk.end_phase()


def build_program():
    nc = bass.Bass("TRN2", target_bir_lowering=False)
    with ExitStack() as st:
        mk = MK(nc, st)
        g = G()
        declare(mk, g)
        st.enter_context(nc.Block())
        setup_consts(mk, g)
        phase_init_h(mk, g)
        for layer in range(2):
            phase_mod(mk, g, layer)
            phase_norm_s5(mk, g, layer)
            phase_linattn2(mk, g, layer, "gla")
            phase_linattn2(mk, g, layer, "hgrn")
            phase_merge(mk, g, layer)
            phase_moe(mk, g, layer)
        mk.finish([g.OUT], "sp")
        for e in ("act", "pool", "dve", "pe"):
            mk.finish([g.OUT], e)
    return nc


_NC_CACHE = {}


def kernel(**inputs):
    n = 8
    if "nc" not in _NC_CACHE:
        _NC_CACHE["nc"] = build_program()
    nc = _NC_CACHE["nc"]
    params = {k: np.ascontiguousarray(np.asarray(inputs[k], dtype=np.float32)) for k in PARAM_SHAPES}
    x = np.asarray(inputs["x"], dtype=np.float32)
    ctx = np.asarray(inputs["ctx"], dtype=np.float32)
    c = np.asarray(inputs["c"], dtype=np.float32)
    c_ctx = np.ascontiguousarray(np.asarray(inputs["c_ctx"], dtype=np.float32))
    in_maps = []
    for b in range(n):
        m = dict(params)
        m["x"] = np.ascontiguousarray(x[b])
        m["ctx"] = np.ascontiguousarray(ctx[b])
        m["c"] = np.ascontiguousarray(c[b])
        m["c_ctx"] = c_ctx
        in_maps.append(m)
    res = run_bass_kernel_spmd(nc, in_maps, core_ids=list(range(n)))
    out = np.stack([np.asarray(res.results[b]["out"], dtype=np.float32) for b in range(n)], axis=0)
    return out


SCRATCH.update({
    "OBG": ([4, 128, T], F32),
    "OBH": ([2, 128, T], F32),
})


def phase_linattn2(mk, g, layer, kind):
    nc = mk.nc
    gla = (kind == "gla")
    DV = 128 if gla else 64
    VW = 4 * DV
    esc = (-1.0 / 16.0) if gla else 1.0
    qscale = 0.125 if gla else 1.0
    UTsrc = g.UT if gla else g.UTH
    OFs = [g.OFG, g.OBG] if gla else [g.OFH, g.OBH]
    NPO = 4 if gla else 2
    cq, ck, cv = (C_GQ, C_GK, C_GV) if gla else (C_HQ, None, C_HI)
    mk.begin_phase()
    rst = mk.sb("rst", [128, 128], F32)
    mk.op("pool", lambda e: e.memset(rst[:], 1.0), writes=[rst])
    for c in range(4):
        mk.op("pool", lambda e: e.memset(rst[:, 32 * c:32 * c + 1], 0.0), reads=[rst], writes=[rst])
    masks = []
    for d in range(2):
        m = mk.sb("mask%d" % d, [128, 128], F32)
        mk.op("pool", lambda e: e.memset(m[:], 1.0), writes=[m])
        if d == 0:
            mk.op("pool", lambda e: e.affine_select(out=m[:], in_=m[:], pattern=[[1, 128]], compare_op=ALU.is_ge,
                                                     fill=0.0, base=0, channel_multiplier=-1), reads=[m], writes=[m])
        else:
            mk.op("pool", lambda e: e.affine_select(out=m[:], in_=m[:], pattern=[[-1, 128]], compare_op=ALU.is_ge,
                                                     fill=0.0, base=0, channel_multiplier=1), reads=[m], writes=[m])
        for c in range(4):
            cs = slice(32 * c, 32 * c + 32)
            if d == 0:
                mk.op("pool", lambda e: e.affine_select(out=m[:, cs], in_=m[:, cs], pattern=[[0, 32]], compare_op=ALU.is_ge,
                                                         fill=0.0, base=-32 * c, channel_multiplier=1), reads=[m], writes=[m])
            else:
                mk.op("pool", lambda e: e.affine_select(out=m[:, cs], in_=m[:, cs], pattern=[[0, 32]], compare_op=ALU.is_ge,
                                                         fill=0.0, base=32 * c + 31, channel_multiplier=-1), reads=[m], writes=[m])
        masks.append(m)
    rowmask = mk.sb("rowmask", [128, 4], F32)
    mk.op("pool", lambda e: e.memset(rowmask[:], 1.0), writes=[rowmask])
    for c in range(4):
        mk.op("pool", lambda e: e.affine_select(out=rowmask[:, c:c + 1], in_=rowmask[:, c:c + 1], pattern=[[0, 1]],
                                                 compare_op=ALU.is_ge, fill=0.0, base=-32 * c, channel_multiplier=1),
              reads=[rowmask], writes=[rowmask])
        mk.op("pool", lambda e: e.affine_select(out=rowmask[:, c:c + 1], in_=rowmask[:, c:c + 1], pattern=[[0, 1]],
                                                 compare_op=ALU.is_ge, fill=0.0, base=32 * c + 31, channel_multiplier=-1),
              reads=[rowmask], writes=[rowmask])
    rowsel = mk.sb("rowsel", [128, 2], F32)
    mk.op("pool", lambda e: e.memset(rowsel[:], 1.0), writes=[rowsel])
    mk.op("pool", lambda e: e.affine_select(out=rowsel[:, 0:1], in_=rowsel[:, 0:1], pattern=[[0, 1]], compare_op=ALU.is_ge,
                                             fill=0.0, base=63, channel_multiplier=-1), reads=[rowsel], writes=[rowsel])
    mk.op("pool", lambda e: e.affine_select(out=rowsel[:, 1:2], in_=rowsel[:, 1:2], pattern=[[0, 1]], compare_op=ALU.is_ge,
                                             fill=0.0, base=-64, channel_multiplier=1), reads=[rowsel], writes=[rowsel])

    lnsel = mk.sb("lnsel", [128, 2], F32)
    mk.op("dve", lambda e: e.tensor_scalar(lnsel[:], rowsel[:], -1.0, 30000.0, ALU.add, ALU.mult), reads=[rowsel], writes=[lnsel])

    def loadw(name, c0, n):
        w = mk.sb(name, [128, 8, n], BF16)
        mk.dma("pool", w[:], g.w_in[layer, :, c0:c0 + n].rearrange("(kc p) n -> p kc n", p=128), reads=[g.w_in], writes=[w])
        return w
    Wq = loadw("Wq", cq, 256)
    Wv = loadw("Wv", cv, VW)
    if gla:
        Wk = loadw("Wk", ck, 256)
        Wdec = [loadw("Wc%d" % d, C_GC + 16 * d, 16) for d in range(2)]
        GU = [mk.sb("GU%d" % d, [16, 256], F32) for d in range(2)]
        nbcol = mk.sb("nbcol", [128, 2, 2], F32)
        with nc.allow_non_contiguous_dma(reason="small"):
            for d in range(2):
                mk.dma("sp", GU[d][:], g.gla_gate_up[layer, d], reads=[g.gla_gate_up], writes=[GU[d]])
                mk.dma("sp", nbcol[:, d, :], g.gla_gate_b[layer, d].rearrange("(m p) -> p m", p=128), reads=[g.gla_gate_b], writes=[nbcol])
        mk.op("dve", lambda e: e.tensor_scalar(nbcol[:], nbcol[:], -1.0, None, ALU.mult), reads=[nbcol], writes=[nbcol])
    else:
        Wdec = [loadw("Wf%d" % d, C_HF + 256 * d, 256) for d in range(2)]
        lbc = mk.sb("lbc", [128, 2], F32)
        omlb = mk.sb("omlb", [128, 2], F32)
        l0 = mk.sb("l0", [128, 2], F32)
        with nc.allow_non_contiguous_dma(reason="small"):
            mk.dma("sp", lbc[:], g.hgrn_lower[1].rearrange("(m p) -> p m", p=128), reads=[g.hgrn_lower], writes=[lbc])
            mk.dma("sp", l0[:], g.hgrn_lower[0].rearrange("(m p) -> p m", p=128), reads=[g.hgrn_lower], writes=[l0])
        if layer == 0:
            mk.op("dve", lambda e: e.memset(lbc[:], 0.0), reads=[lbc], writes=[lbc])
        else:
            mk.op("dve", lambda e: e.tensor_tensor(lbc[:], lbc[:], l0[:], ALU.subtract), reads=[lbc, l0], writes=[lbc])
            mk.op("act", lambda e: e.activation(lbc[:], lbc[:], AF.Sigmoid), reads=[lbc], writes=[lbc])
        mk.op("dve", lambda e: e.tensor_scalar(omlb[:], lbc[:], -1.0, 1.0, ALU.mult, ALU.add), reads=[lbc], writes=[omlb])
        nomlb = mk.sb("nomlb", [128, 2], F32)
        mk.op("dve", lambda e: e.tensor_scalar(nomlb[:], omlb[:], -1.0, None, ALU.mult), reads=[omlb], writes=[nomlb])

    blocks = [(0, 2)] + [(2 + 4 * b, 4) for b in range(8)]

    def chain(d):
        sfx = "_%d" % d
        pA = mk.ps("pA" + sfx, [128, 512], F32)
        pB = mk.ps("pB" + sfx, [128, 512], F32)
        pds = mk.ps("pds" + sfx, [128, 2, 256], F32)
        ptr = mk.ps("ptr" + sfx, [128, 1024], BF16)
        pscv = pA[:].rearrange("p (h t) -> p h t", h=4)
        rotl = [pA, pB]
        npj = [0]

        def nextp():
            npj[0] += 1
            return rotl[npj[0] % 2]

        ubs = [mk.sb("ub%d" % i + sfx, [128, 8, 512], BF16) for i in range(2)]
        qT = mk.sb("qT" + sfx, [128, 2, 512], F32)
        kT = mk.sb("kT" + sfx, [128, 2, 512], F32)
        LG = mk.sb("LG" + sfx, [128, 2, 512], F32)
        vtok = mk.sb("vtok" + sfx, [128, 4, VW], BF16)
        if gla:
            codeT = mk.sb("codeT" + sfx, [16, 512], F32)
        else:
            sg = mk.sb("sg" + sfx, [128, 2, 512], F32)
        cum = mk.sb("cum" + sfx, [128, 2, 128], F32)
        eq = mk.sb("eq" + sfx, [128, 2, 128], F32)
        eqm = mk.sb("eqm" + sfx, [128, 2, 2, 128], F32)
        ek = mk.sb("ek" + sfx, [128, 2, 128], F32)
        elast = mk.sb("elast" + sfx, [128, 2, 4], F32)
        qtil4 = mk.sb("qtil4" + sfx, [128, 4, 128], F32)
        ktil = mk.sb("ktil" + sfx, [128, 2, 128], F32)
        ktilb = mk.sb("ktilb" + sfx, [128, 2, 128], BF16)
        khT = mk.sb("khT" + sfx, [128, 2, 128], BF16)
        khat4 = mk.sb("khat4" + sfx, [128, 4, 256], BF16)
        PT = mk.sb("PT" + sfx, [128, 4, 128], BF16)
        S = [mk.sb("S%d" % i + sfx, [128, DV], F32) for i in range(2)]
        Sb4 = [[mk.sb("Sb4_%d_%d" % (pp_, i) + sfx, [128, 5, DV], F32) for i in range(2)] for pp_ in range(2)]
        SbT = [[[mk.sub(Sb4[pp_][i], "slot") for _ in range(5)] for i in range(2)] for pp_ in range(2)]
        ptrF = ptr[:].bitcast(F32).rearrange("p (m x) -> p m x", m=2)
        ost = [mk.sb("ost%d" % i + sfx, [128, NPO, 128], F32) for i in range(2)]
        for mt in range(2):
            mk.op("pool", lambda e: e.memset(S[mt][:], 0.0), reads=[S[mt]], writes=[S[mt]])
            for pp_ in range(2):
                mk.op("pool", lambda e: e.memset(Sb4[pp_][mt][:], 0.0), reads=SbT[pp_][mt], writes=SbT[pp_][mt])
        ntile = 0
        order = blocks if d == 0 else [blocks[0]] + blocks[:0:-1]
        for bi, (tile0, ntl) in enumerate(order):
            N = ntl * 128
            t0 = tile0 * 128
            ub = ubs[bi % 2]
            if bi == 0:
                mk.dma("sp", ub[:, :, 0:N], UTsrc[:, :, t0:t0 + N].rearrange("k p t -> p k t"), reads=[UTsrc], writes=[ub])

            def proj(W, c0, m, evac):
                pp = nextp()
                for kc in range(8):
                    mk.mm(pp[0:m, 0:N], W[:, kc, c0:c0 + m], ub[:, kc, 0:N], start=(kc == 0), stop=(kc == 7),
                          reads=[W, ub], writes=[pp])
                evac(pp)

            for mt in range(2):
                fq = AF.Copy if gla else AF.Silu
                proj(Wq, mt * 128, 128, lambda pp: mk.op("act", lambda e: e.activation(qT[:, mt, 0:N], pp[:, 0:N], fq),
                                                          reads=[pp, qT], writes=[qT]))
                yield
            if gla:
                for mt in range(2):
                    proj(Wk, mt * 128, 128, lambda pp: mk.op("dve", lambda e: e.tensor_copy(kT[:, mt, 0:N], pp[:, 0:N]),
                                                              reads=[pp, kT], writes=[kT]))
                    yield
                proj(Wdec[d], 0, 16, lambda pp: mk.op("act", lambda e: e.activation(codeT[:, 0:N], pp[0:16, 0:N], AF.Copy),
                                                       reads=[pp], writes=[codeT]))
                for mt in range(2):
                    pp = nextp()
                    mk.mm(pp[:, 0:N], GU[d][:, mt * 128:(mt + 1) * 128], codeT[:, 0:N], start=True, stop=True,
                          reads=[GU[d], codeT], writes=[pp])
                    mk.op("act", lambda e: e.activation(LG[:, mt, 0:N], pp[:, 0:N], AF.Exp, scale=-1.0, bias=nbcol[:, d, mt:mt + 1]),
                          reads=[pp, nbcol, LG], writes=[LG])
                    mk.op("act", lambda e: e.activation(LG[:, mt, 0:N], LG[:, mt, 0:N], AF.Ln, bias=g.one_col[:, 0:1]),
                          reads=[LG, g.one_col], writes=[LG])
                    yield
            else:
                for mt in range(2):
                    def ev(pp):
                        mk.op("act", lambda e: e.activation(sg[:, mt, 0:N], pp[:, 0:N], AF.Sigmoid), reads=[pp, sg], writes=[sg])
                        mk.op("act", lambda e: e.activation(LG[:, mt, 0:N], sg[:, mt, 0:N], AF.Ln, scale=omlb[:, mt:mt + 1],
                                                             bias=lbc[:, mt:mt + 1]), reads=[sg, omlb, lbc, LG], writes=[LG])
                        mk.op("act", lambda e: e.activation(kT[:, mt, 0:N], sg[:, mt, 0:N], AF.Identity, scale=nomlb[:, mt:mt + 1],
                                                             bias=omlb[:, mt:mt + 1]), reads=[sg, kT, omlb, nomlb], writes=[kT])
                    proj(Wdec[d], mt * 128, 128, ev)
                    yield
            for tl in range(ntl):
                pp = nextp()
                for kc in range(8):
                    mk.mm(pp[:, 0:VW], ub[:, kc, tl * 128:(tl + 1) * 128], Wv[:, kc, :], start=(kc == 0), stop=(kc == 7),
                          reads=[Wv, ub], writes=[pp])
                mk.op("dve", lambda e: e.tensor_copy(vtok[:, tl, :], pp[:, 0:VW]), reads=[pp, vtok], writes=[vtok])
                yield
            if bi + 1 < len(order):
                tile0n, ntln = order[bi + 1]
                mk.dma("sp", ubs[(bi + 1) % 2][:, :, 0:ntln * 128],
                       UTsrc[:, :, tile0n * 128:(tile0n + ntln) * 128].rearrange("k p t -> p k t"), reads=[UTsrc], writes=[ubs[(bi + 1) % 2]])
            tls = list(range(ntl)) if d == 0 else list(range(ntl - 1, -1, -1))
            for tl in tls:
                ti = tile0 + tl
                tsl = slice(tl * 128, (tl + 1) * 128)
                ntile += 1
                par = ntile % 2
                for mt in range(2):
                    if d == 0:
                        osl = slice(0, 128)
                        isl = slice(tl * 128, tl * 128 + 128)
                    else:
                        osl = slice(127, None, -1)
                        ilo, ihi = tl * 128, tl * 128 + 128
                        isl = slice(ihi - 1, ilo - 1 if ilo > 0 else None, -1)
                    mk.op("dve", lambda e: e.tensor_tensor_scan(cum[:, mt, osl], rst[:, :], LG[:, mt, isl], 0.0,
                                                                 ALU.mult, ALU.add), reads=[LG, rst, cum], writes=[cum])
                yield
                lidx = slice(31, 128, 32) if d == 0 else slice(0, 128, 32)
                for r in range(2):
                    mk.op("act", lambda e: e.activation(eqm[:, r], cum[:], AF.Exp, scale=esc, bias=lnsel[:, r:r + 1]),
                          reads=[cum, lnsel, eqm], writes=[eqm])
                mk.op("act", lambda e: e.activation(ek[:], cum[:], AF.Exp, scale=-esc), reads=[cum], writes=[ek])
                mk.op("act", lambda e: e.activation(elast[:], cum[:, :, lidx], AF.Exp, scale=esc), reads=[cum], writes=[elast])
                for r in range(2):
                    mk.op("dve", lambda e: e.scalar_tensor_tensor(qtil4[:].rearrange("p (m r) t -> p r m t", r=2)[:, r],
                                                                   qT[:, :, tsl], qscale, eqm[:, r], ALU.mult, ALU.mult),
                          reads=[qT, eqm, qtil4], writes=[qtil4])
                mk.op("dve", lambda e: e.tensor_tensor(ktil[:], kT[:, :, tsl], ek[:], ALU.mult), reads=[kT, ek], writes=[ktil])
                mk.op("dve", lambda e: e.tensor_tensor(khT[:].rearrange("p m (c j) -> p m c j", j=32),
                                                        ktil[:].rearrange("p m (c j) -> p m c j", j=32),
                                                        elast[:].unsqueeze(3).broadcast_to([128, 2, 4, 32]), ALU.mult),
                      reads=[ktil, elast], writes=[khT])
                yield
                for mt in range(2):
                    mk.tr(ptr[:, mt * 128:(mt + 1) * 128], khT[:, mt, :], g.identb[:], reads=[khT, g.identb], writes=[ptr])
                for c in range(4):
                    if c % 2 == 0:
                        mk.op("act", lambda e: e.activation(khat4[:, c, :], ptr[:, 0:256], AF.Identity, scale=rowmask[:, c:c + 1]),
                              reads=[ptr, rowmask, khat4], writes=[khat4])
                    else:
                        mk.op("pool", lambda e: e.tensor_scalar(khat4[:, c, :], ptr[:, 0:256], rowmask[:, c:c + 1], None, ALU.mult),
                              reads=[ptr, rowmask, khat4], writes=[khat4]) if False else \
                            mk.op("dve", lambda e: e.tensor_scalar(khat4[:, c, :], ptr[:, 0:256], rowmask[:, c:c + 1], None, ALU.mult),
                                  reads=[ptr, rowmask, khat4], writes=[khat4])
                for h in range(4):
                    mk.mm(pscv[:, h, :], ktil[:, h // 2, :], qtil4[:, h, :], start=True, stop=True,
                          reads=[ktil, qtil4], writes=[pA])
                mk.op("dve", lambda e: e.tensor_tensor(PT[:], pscv, masks[d][:].unsqueeze(1).broadcast_to([128, 4, 128]), ALU.mult),
                      reads=[pA, masks[d]], writes=[PT])
                yield

                def po_ap(h, cols):
                    if gla:
                        return pB[:, h * 128 + cols.start:h * 128 + cols.stop]
                    mt_ = h // 2
                    return pB[64 * (h % 2):64 * (h % 2) + 64, mt_ * 128 + cols.start:mt_ * 128 + cols.stop]

                chunks = list(range(4)) if d == 0 else [3, 2, 1, 0]
                cur = ntile % 2
                prv = 1 - cur
                for ci, c in enumerate(chunks):
                    bank, bv = (pds, pds) if ci < 2 else (ptr, ptrF)
                    sl = ci % 2
                    for h in range(4):
                        mt, rows = h // 2, slice(64 * (h % 2), 64 * (h % 2) + 64)
                        mk.mm(bv[rows, mt, sl * 128:sl * 128 + DV], khat4[:, c, h * 64:(h + 1) * 64], vtok[:, tl, h * DV:(h + 1) * DV],
                              start=True, stop=True, reads=[khat4, vtok], writes=[bank])
                for ci, c in enumerate(chunks):
                    bank, bv = (pds, pds) if ci < 2 else (ptr, ptrF)
                    sl = ci % 2
                    for mt in range(2):
                        if ci == 0:
                            sin_ap, sin_buf = Sb4[prv][mt][:, 4, :], SbT[prv][mt][4]
                        else:
                            sin_ap, sin_buf = Sb4[cur][mt][:, ci, :], SbT[cur][mt][ci]
                        mk.op("dve", lambda e: e.scalar_tensor_tensor(Sb4[cur][mt][:, ci + 1, :], sin_ap, elast[:, mt, c:c + 1],
                                                                       bv[:, mt, sl * 128:sl * 128 + DV], ALU.mult, ALU.add),
                              reads=[sin_buf, elast, bank, SbT[cur][mt][ci + 1]], writes=[SbT[cur][mt][ci + 1]])
                yield
                for ci, c in enumerate(chunks):
                    cs = slice(32 * c, 32 * c + 32)
                    for h in range(4):
                        mt = h // 2
                        if ci == 0:
                            st_ap, st_buf = Sb4[prv][mt][:, 4, :], SbT[prv][mt][4]
                        else:
                            st_ap, st_buf = Sb4[cur][mt][:, ci, :], SbT[cur][mt][ci]
                        mk.mm(po_ap(h, cs), vtok[:, tl, h * DV:(h + 1) * DV], PT[:, h, cs], start=True, stop=False,
                              reads=[vtok, PT], writes=[pB])
                        mk.mm(po_ap(h, cs), st_ap, qtil4[:, h, cs], start=False, stop=True,
                              reads=[st_buf, qtil4], writes=[pB])
                yield
                if gla:
                    mk.op("act", lambda e: e.activation(ost[par][:], pB[:].rearrange("p (h t) -> p h t", h=4), AF.Copy), reads=[pB], writes=[ost[par]])
                else:
                    mk.op("act", lambda e: e.activation(ost[par][:], pB[:, 0:256].rearrange("p (m t) -> p m t", m=2), AF.Copy),
                          reads=[pB], writes=[ost[par]])
                mk.dma("pool", OFs[d][:, :, ti * 128:(ti + 1) * 128].rearrange("m p t -> p m t"), ost[par][:],
                       reads=[ost[par]], writes=[OFs[d]])
                yield

    push_scope(mk)
    gens = [chain(0), chain(1)]
    alive = [True, True]
    while any(alive):
        for i, gi in enumerate(gens):
            if alive[i]:
                try:
                    next(gi)
                except StopIteration:
                    alive[i] = False
    pop_scope(mk)

    push_scope(mk)
    pf = [mk.ps("pf%d" % i, [128, 512], F32) for i in range(2)]
    pss = mk.ps("pssf", [128, 512], F32)
    ubs = [mk.sb("fub%d" % i, [128, 8, 512], BF16) for i in range(2)]
    ofl = [mk.sb("fof%d" % i, [128, NPO, 512], F32) for i in range(2)]
    obl = [mk.sb("fob%d" % i, [128, NPO, 512], F32) for i in range(2)]
    if gla:
        Wr = loadw("Wr", C_GR, 512)
        gn = mk.sb("gncol", [128, 1], F32)
        with nc.allow_non_contiguous_dma(reason="small"):
            mk.dma("sp", gn[:], g.gla_norm_g[layer].rearrange("(p o) -> p o", o=1), reads=[g.gla_norm_g], writes=[gn])
        ones_bf = mk.sb("ones_bf", [128, 128], BF16)
        mk.op("pool", lambda e: e.memset(ones_bf[:], 1.0), writes=[ones_bf])
        rsil = mk.sb("rsil", [128, 4, 512], F32)
        osq = mk.sb("osq", [128, 512], BF16)
        rstd = mk.sb("rstdg", [128, 512], F32)
        ysts = [mk.sb("ystg%d" % i, [128, 4, 512], BF16) for i in range(2)]
    else:
        Wog = loadw("Wog", C_HO, 256)
        gnrow = mk.sb("gnrow", [128, 4, 64], F32)
        for h in range(4):
            mk.dma("sp", gnrow[:, h, :], g.hgrn_norm_g[layer].partition_broadcast(128), reads=[g.hgrn_norm_g], writes=[gnrow])
        ogs = mk.sb("ogs", [128, 256], F32)
        otok = mk.sb("otok", [128, 256], F32)
        junkh = mk.sb("junkh", [128, 64], F32)
        ssh = mk.sb("ssh", [128, 4], F32)
        rsth = mk.sb("rsth", [128, 4], F32)
        ysth = [mk.sb("ysth%d" % i, [128, 256], BF16) for i in range(2)]
    npf = 0
    for bi, (tile0, ntl) in enumerate(blocks):
        N = ntl * 128
        t0 = tile0 * 128
        ub = ubs[bi % 2]
        of_, ob_ = ofl[bi % 2], obl[bi % 2]
        mk.dma("sp", ub[:, :, 0:N], UTsrc[:, :, t0:t0 + N].rearrange("k p t -> p k t"), reads=[UTsrc], writes=[ub])
        mk.dma("pool", of_[:, :, 0:N], OFs[0][:, :, t0:t0 + N].rearrange("m p t -> p m t"), reads=[OFs[0]], writes=[of_])
        mk.dma("pool", ob_[:, :, 0:N], OFs[1][:, :, t0:t0 + N].rearrange("m p t -> p m t"), reads=[OFs[1]], writes=[ob_])
        mk.op("dve", lambda e: e.tensor_tensor(of_[:, :, 0:N], of_[:, :, 0:N], ob_[:, :, 0:N], ALU.add), reads=[of_, ob_], writes=[of_])
        if gla:
            yst = ysts[bi % 2]
            for h in range(4):
                pp = pf[npf % 2]
                npf += 1
                for kc in range(8):
                    mk.mm(pp[:, 0:N], Wr[:, kc, h * 128:(h + 1) * 128], ub[:, kc, 0:N], start=(kc == 0), stop=(kc == 7),
                          reads=[Wr, ub], writes=[pp])
                mk.op("act", lambda e: e.activation(rsil[:, h, 0:N], pp[:, 0:N], AF.Silu), reads=[pp, rsil], writes=[rsil])
                mk.op("act", lambda e: e.activation(osq[:, 0:N], of_[:, h, 0:N], AF.Square), reads=[of_], writes=[osq])
                mk.mm(pss[:, 0:N], ones_bf[:], osq[:, 0:N], start=True, stop=True, reads=[ones_bf, osq], writes=[pss])
                mk.op("act", lambda e: e.activation(rstd[:, 0:N], pss[:, 0:N], AF.Sqrt, scale=1.0 / 128.0, bias=g.eps_col[:, 0:1]),
                      reads=[pss, g.eps_col], writes=[rstd])
                mk.op("dve", lambda e: e.reciprocal(rstd[:, 0:N], rstd[:, 0:N]), reads=[rstd], writes=[rstd])
                mk.op("dve", lambda e: e.scalar_tensor_tensor(of_[:, h, 0:N], of_[:, h, 0:N], gn[:, 0:1], rstd[:, 0:N],
                                                               ALU.mult, ALU.mult), reads=[of_, gn, rstd], writes=[of_])
                mk.op("dve", lambda e: e.tensor_tensor(yst[:, h, 0:N], of_[:, h, 0:N], rsil[:, h, 0:N], ALU.mult),
                      reads=[of_, rsil, yst], writes=[yst])
            mk.dma("sp", g.YGLAT[:, :, t0:t0 + N].rearrange("m p t -> p m t"), yst[:, :, 0:N], reads=[yst], writes=[g.YGLAT])
        else:
            for tl in range(ntl):
                ti = tile0 + tl
                pp = pf[npf % 2]
                npf += 1
                for kc in range(8):
                    mk.mm(pp[:, 0:256], ub[:, kc, tl * 128:(tl + 1) * 128], Wog[:, kc, :], start=(kc == 0), stop=(kc == 7),
                          reads=[Wog, ub], writes=[pp])
                mk.op("act", lambda e: e.activation(ogs[:], pp[:, 0:256], AF.Silu), reads=[pp], writes=[ogs])
                for mt in range(2):
                    mk.tr(pss[:, mt * 128:(mt + 1) * 128], of_[:, mt, tl * 128:(tl + 1) * 128], g.identf[:], reads=[of_, g.identf], writes=[pss])
                mk.op("act", lambda e: e.activation(otok[:], pss[:, 0:256], AF.Copy), reads=[pss], writes=[otok])
                for h in range(4):
                    mk.op("act", lambda e: e.activation(junkh[:], otok[:, h * 64:(h + 1) * 64], AF.Square, accum_out=ssh[:, h:h + 1]),
                          reads=[otok, junkh, ssh], writes=[junkh, ssh])
                mk.op("act", lambda e: e.activation(rsth[:], ssh[:], AF.Sqrt, scale=1.0 / 64.0, bias=g.eps_col[:, 0:1]),
                      reads=[ssh, g.eps_col], writes=[rsth])
                mk.op("dve", lambda e: e.reciprocal(rsth[:], rsth[:]), reads=[rsth], writes=[rsth])
                ov = otok[:].rearrange("p (h v) -> p h v", h=4)
                mk.op("dve", lambda e: e.tensor_tensor(ov, ov, rsth[:].unsqueeze(2).broadcast_to([128, 4, 64]), ALU.mult),
                      reads=[otok, rsth], writes=[otok])
                mk.op("dve", lambda e: e.tensor_tensor(ov, ov, gnrow[:], ALU.mult), reads=[otok, gnrow], writes=[otok])
                yh = ysth[ti % 2]
                mk.op("dve", lambda e: e.tensor_tensor(yh[:], otok[:], ogs[:], ALU.mult), reads=[otok, ogs], writes=[yh])
                pofs = 0
                for (r0, rs, n) in scan_rows(ti):
                    dst = g.YHG[r0:r0 + n, :] if rs == 1 else g.YHG[r0:r0 + (n - 1) * rs + 1:rs, :]
                    mk.dma("sp", dst, yh[pofs:pofs + n, :], reads=[yh], writes=[g.YHG])
                    pofs += n
    pop_scope(mk)
    mk.end_phase()


def phase_norm1_both(mk, g, layer):
    mk.begin_phase()

    def chain(scan):
        sfx = "_s" if scan else "_n"
        dst = g.UTH if scan else g.UT
        hts = [mk.sb("h_t%d" % i + sfx, [128, D], F32) for i in range(2)]
        xns = [mk.sb("xn%d" % i + sfx, [128, D], BF16) for i in range(2)]
        uts = [mk.sb("ut%d" % i + sfx, [128, 8, 128], BF16) for i in range(2)]
        pTs = [mk.ps("pT%d" % i + sfx, [128, 8, 128], BF16) for i in range(2)]
        junk = mk.sb("junk" + sfx, [128, D], BF16)
        sss = [mk.sb("ss%d" % i + sfx, [128, 1], F32) for i in range(2)]
        rstds = [mk.sb("rstd%d" % i + sfx, [128, 1], F32) for i in range(2)]
        q1, q2 = ("sp", "act") if not scan else ("act", "sp")
        for i in range(NT):
            b = i % 2
            wv = 1 if i < 2 else 0
            if not scan:
                mk.dma(q1, hts[b][:], g.H[i * 128:(i + 1) * 128, :], reads=[g.H], writes=[hts[b]])
            else:
                po = 0
                for (r0, rs, n) in scan_rows(i):
                    src = g.H[r0:r0 + n, :] if rs == 1 else g.H[r0:r0 + (n - 1) * rs + 1:rs, :]
                    mk.dma(q1, hts[b][po:po + n, :], src, reads=[g.H], writes=[hts[b]])
                    po += n
            norm_tile(mk, g, hts[b], xns[b], sss[b], rstds[b], junk)
            yield
            transpose_mod_tile(mk, g, xns[b], pTs[b], uts[b], g.A1, None, wv, 0)
            mk.dma(q2, dst[:, :, i * 128:(i + 1) * 128].rearrange("k p t -> p k t"), uts[b][:],
                   reads=[uts[b]], writes=[dst])
            yield

    gens = [chain(False), chain(True)]
    alive = [True, True]
    while any(alive):
        for i, gi in enumerate(gens):
            if alive[i]:
                try:
                    next(gi)
                except StopIteration:
                    alive[i] = False
    mk.end_phase()
```
